# Optimizing a Trainium2 kernel written in Bass

```python
import math
import jax, jax.numpy as jnp
from jax import lax
import numpy as np

D_MODEL = 1024
BATCH = 16
SEQ = 2048
DEPTH = 4

N_MIXERS = 2
N_HEADS = 16
HEAD_DIM = 64
KV_RANK = 256
IDX_HEADS = 8
IDX_DIM = 64
INDEX_TOPK = 256
Q_BLOCK = 128
KV_HEADS = 2
WINDOW = 128
N_BUCKETS = 32
MAX_DISTANCE = 128
D_FF = -(-8 * D_MODEL // (3 * 256)) * 256
N_A = (DEPTH + 1) // 2
N_B = DEPTH // 2
RMS_EPS = 1e-6
NEG = -1e30

A_SPLITS = (N_HEADS * HEAD_DIM,
            N_HEADS * HEAD_DIM + KV_RANK,
            N_HEADS * HEAD_DIM + KV_RANK + IDX_HEADS * IDX_DIM,
            N_HEADS * HEAD_DIM + KV_RANK + IDX_HEADS * IDX_DIM + IDX_DIM)
A_IN = A_SPLITS[-1] + IDX_HEADS
B_IN = (N_HEADS + 2 * KV_HEADS) * HEAD_DIM

kernel_name = 'hybrid_dsa_swa_sink_adaln_trunk'


def rms_norm(x, g):
    xf = x.astype(jnp.float32)
    y = xf * lax.rsqrt(jnp.mean(xf * xf, axis=-1, keepdims=True) + RMS_EPS)
    return (y * g.astype(jnp.float32)).astype(x.dtype)


def layer_norm(x, g, b):
    xf = x.astype(jnp.float32)
    mu = jnp.mean(xf, axis=-1, keepdims=True)
    var = jnp.mean(jnp.square(xf - mu), axis=-1, keepdims=True)
    y = (xf - mu) * lax.rsqrt(var + RMS_EPS)
    return (y * g.astype(jnp.float32) + b.astype(jnp.float32)).astype(x.dtype)


def t5_bucket(dist):
    max_exact = N_BUCKETS // 2
    d = jnp.maximum(dist, 0)
    large = max_exact + (jnp.log(jnp.maximum(d, 1).astype(jnp.float32) / max_exact)
                         / math.log(MAX_DISTANCE / max_exact)
                         * (N_BUCKETS - max_exact)).astype(jnp.int32)
    large = jnp.minimum(large, N_BUCKETS - 1)
    return jnp.where(d < max_exact, d, large)


def modulate(h, shift, scale):
    return h * (1 + scale[:, None, :]) + shift[:, None, :]


def dsa_mla_mixer(h, w_in, kv_norm_g, w_uk, w_uv, idx_k_g, idx_k_b, w_out, rel_bias):
    B, L, _ = h.shape
    proj = h @ w_in
    q, ckv, qi, ki, wi = jnp.split(proj, A_SPLITS, axis=-1)
    q = q.reshape(B, L, N_HEADS, HEAD_DIM)
    ckv = rms_norm(ckv, kv_norm_g)
    qi = qi.reshape(B, L, IDX_HEADS, IDX_DIM)
    ki = layer_norm(ki, idx_k_g, idx_k_b)
    wi = wi * (IDX_HEADS ** -0.5 * IDX_DIM ** -0.5)
    topk = min(INDEX_TOPK, L // 4)
    nblk = L // Q_BLOCK
    key_pos = jnp.arange(L)

    def blocks(a):
        return a.reshape(B, nblk, Q_BLOCK, *a.shape[2:]).swapaxes(0, 1)

    def one_block(args):
        q_b, qi_b, wi_b, t_b = args
        rel = jax.nn.relu(jnp.einsum('bqhd,bsd->bqhs', qi_b, ki).astype(jnp.float32))
        score = jnp.einsum('bqhs,bqh->bqs', rel, wi_b.astype(jnp.float32))
        causal = key_pos[None, :] <= t_b[:, None]
        score = jnp.where(causal[None], score, NEG)
        _, sel = lax.top_k(score, topk)
        valid = sel <= t_b[None, :, None]
        kv_sel = jax.vmap(lambda cb, ib: cb[ib])(ckv, sel)
        q_abs = jnp.einsum('bqhd,hrd->bqhr', q_b, w_uk)
        logits = jnp.einsum('bqhr,bqkr->bqhk', q_abs, kv_sel).astype(jnp.float32) * HEAD_DIM ** -0.5
        bias = rel_bias[t5_bucket(t_b[None, :, None] - sel)]
        logits = logits + bias.astype(jnp.float32).transpose(0, 1, 3, 2)
        logits = jnp.where(valid[:, :, None, :], logits, NEG)
        p = jax.nn.softmax(logits, axis=-1).astype(kv_sel.dtype)
        return jnp.einsum('bqhk,bqkr->bqhr', p, kv_sel)

    t_blocks = jnp.arange(L).reshape(nblk, Q_BLOCK)
    o_lat = lax.map(one_block, (blocks(q), blocks(qi), blocks(wi), t_blocks))
    o_lat = o_lat.swapaxes(0, 1).reshape(B, L, N_HEADS, KV_RANK)
    o = jnp.einsum('blhr,hrd->blhd', o_lat, w_uv)
    return o.reshape(B, L, N_HEADS * HEAD_DIM) @ w_out


def swa_sink_mixer(h, w_in, b_in, sinks, w_out, b_out, rel_bias):
    B, L, _ = h.shape
    W = WINDOW
    nb = L // W
    G = N_HEADS // KV_HEADS
    proj = h @ w_in + b_in
    q, k, v = jnp.split(proj, (N_HEADS * HEAD_DIM, (N_HEADS + KV_HEADS) * HEAD_DIM), axis=-1)
    q = q.reshape(B, nb, W, KV_HEADS, G, HEAD_DIM)
    k = k.reshape(B, nb, W, KV_HEADS, HEAD_DIM)
    v = v.reshape(B, nb, W, KV_HEADS, HEAD_DIM)

    def with_prev(a):
        prev = jnp.pad(a[:, :-1], ((0, 0), (1, 0), (0, 0), (0, 0), (0, 0)))
        return jnp.concatenate([prev, a], axis=2)

    kb, vb = with_prev(k), with_prev(v)
    logits = jnp.einsum('bnqkgd,bnskd->bnkgqs', q, kb).astype(jnp.float32) * HEAD_DIM ** -0.5
    qpos = jnp.arange(W)
    kpos = jnp.arange(2 * W)
    dist = qpos[:, None] + W - kpos[None, :]
    allowed = (dist >= 0) & (dist < WINDOW)
    blk_valid = (jnp.arange(nb)[:, None] > 0) | (kpos[None, :] >= W)
    mask = allowed[None] & blk_valid[:, None, :]
    bias = rel_bias[t5_bucket(dist)].transpose(2, 0, 1).reshape(KV_HEADS, G, W, 2 * W)
    logits = logits + bias.astype(jnp.float32)[None, None]
    logits = jnp.where(mask[None, :, None, None], logits, NEG)
    sink = jnp.broadcast_to(sinks.astype(jnp.float32).reshape(KV_HEADS, G, 1, 1),
                            logits.shape[:-1] + (1,))
    p = jax.nn.softmax(jnp.concatenate([logits, sink], axis=-1), axis=-1)[..., :-1]
    o = jnp.einsum('bnkgqs,bnskd->bnqkgd', p.astype(vb.dtype), vb)
    return o.reshape(B, L, N_HEADS * HEAD_DIM) @ w_out + b_out


def swiglu(h, w1, w3, w2):
    return (jax.nn.silu(h @ w1) * (h @ w3)) @ w2


def setup_inputs(seed: int = 0) -> dict:
    key = jax.random.key(seed)
    ks = jax.random.split(key, 24)
    f32 = jnp.float32
    D = D_MODEL
    HD = N_HEADS * HEAD_DIM

    def nrm(k, shape, scale):
        return jax.random.normal(k, shape, f32) * scale

    return {
        'x': nrm(ks[0], (BATCH, SEQ, D), 1.0),
        'c': nrm(ks[1], (BATCH, D), 1.0),
        'rel_bias': nrm(ks[2], (N_BUCKETS, N_HEADS), 0.5),
        'w_ada': nrm(ks[3], (DEPTH, D, 6 * D), 0.5 * D ** -0.5),
        'b_ada': nrm(ks[4], (DEPTH, 6 * D), 0.02),
        'norm_mix_g': 1.0 + nrm(ks[5], (DEPTH, D), 0.02),
        'norm_ffn_g': 1.0 + nrm(ks[6], (DEPTH, D), 0.02),
        'a_w_in': nrm(ks[7], (N_A, D, A_IN), D ** -0.5),
        'a_kv_norm_g': 1.0 + nrm(ks[8], (N_A, KV_RANK), 0.02),
        'a_w_uk': nrm(ks[9], (N_A, N_HEADS, KV_RANK, HEAD_DIM), KV_RANK ** -0.5),
        'a_w_uv': nrm(ks[10], (N_A, N_HEADS, KV_RANK, HEAD_DIM), KV_RANK ** -0.5),
        'a_idx_k_g': 1.0 + nrm(ks[11], (N_A, IDX_DIM), 0.02),
        'a_idx_k_b': nrm(ks[12], (N_A, IDX_DIM), 0.02),
        'a_w_out': nrm(ks[13], (N_A, HD, D), HD ** -0.5),
        'b_w_in': nrm(ks[14], (N_B, D, B_IN), D ** -0.5),
        'b_b_in': nrm(ks[15], (N_B, B_IN), 0.02),
        'b_sinks': nrm(ks[16], (N_B, N_HEADS), 0.5),
        'b_w_out': nrm(ks[17], (N_B, HD, D), HD ** -0.5),
        'b_b_out': nrm(ks[18], (N_B, D), 0.02),
        'ffn_w1': nrm(ks[19], (DEPTH, D, D_FF), D ** -0.5),
        'ffn_w3': nrm(ks[20], (DEPTH, D, D_FF), D ** -0.5),
        'ffn_w2': nrm(ks[21], (DEPTH, D_FF, D), D_FF ** -0.5),
        'norm_final_g': 1.0 + nrm(ks[22], (D,), 0.02),
    }


def reference(x, c, rel_bias, w_ada, b_ada, norm_mix_g, norm_ffn_g,
              a_w_in, a_kv_norm_g, a_w_uk, a_w_uv, a_idx_k_g, a_idx_k_b, a_w_out,
              b_w_in, b_b_in, b_sinks, b_w_out, b_b_out,
              ffn_w1, ffn_w3, ffn_w2, norm_final_g):
    cs = jax.nn.silu(c)
    for i in range(DEPTH):
        mod = cs @ w_ada[i] + b_ada[i]
        sh1, sc1, g1, sh2, sc2, g2 = jnp.split(mod, 6, axis=-1)
        h = modulate(rms_norm(x, norm_mix_g[i]), sh1, sc1)
        j = i // N_MIXERS
        if i % N_MIXERS == 0:
            y = dsa_mla_mixer(h, a_w_in[j], a_kv_norm_g[j], a_w_uk[j], a_w_uv[j],
                              a_idx_k_g[j], a_idx_k_b[j], a_w_out[j], rel_bias)
        else:
            y = swa_sink_mixer(h, b_w_in[j], b_b_in[j], b_sinks[j], b_w_out[j],
                               b_b_out[j], rel_bias)
        x = x + g1[:, None, :] * y
        h = modulate(rms_norm(x, norm_ffn_g[i]), sh2, sc2)
        x = x + g2[:, None, :] * swiglu(h, ffn_w1[i], ffn_w3[i], ffn_w2[i])
    return rms_norm(x, norm_final_g)
```

```python
import contextlib
import math
import os

import numpy as np
import concourse.bass as bass
import concourse.mybir as mybir
from concourse.bass_utils import run_bass_kernel_spmd

F32 = mybir.dt.float32
BF16 = mybir.dt.bfloat16
AF = mybir.ActivationFunctionType
ALU = mybir.AluOpType

D = 1024
SEQ = 2048
DEPTH = 4
NH = 16
DFF = 2816
NFC = DFF // 128
NTILE = SEQ // 128
NCORES = 8
A_IN = 1864
B_IN = 1280
NEG_BIG = -1.0e30
REPL = -3.0e38

ENGS = ('pe', 'act', 'dve', 'pool', 'sp')
NDMASEM = 12
EPOCH = 24000


class Op:
    __slots__ = ('eng', 'fn', 'deps', 'signal', 'is_dma', 'dsem', 'dval', 'cnt', 'epoch')

    def __init__(self, eng, fn, is_dma):
        self.eng = eng
        self.fn = fn
        self.deps = ()
        self.signal = False
        self.is_dma = is_dma
        self.dsem = None
        self.dval = 0
        self.cnt = 0
        self.epoch = 0


class Sched:
    def __init__(self, nc):
        self.nc = nc
        self.ops = {e: [] for e in ENGS}
        self.res = {}
        self.dma_hist = {e: [] for e in ENGS}
        self.pending_barrier = {e: None for e in ENGS}
        self.dma_since_barrier = []

    def barrier(self):
        deps = set(self.dma_since_barrier)
        for e in ENGS:
            if self.ops[e]:
                last = self.ops[e][-1]
                deps.add(last)
        self.dma_since_barrier = []
        for e in ENGS:
            old = self.pending_barrier[e]
            self.pending_barrier[e] = set(deps) | (old or set())

    def add(self, eng, fn, reads=(), writes=(), dma=False):
        op = Op(eng, fn, dma)
        deps = set()
        res = self.res
        for k in reads:
            st = res.get(k)
            if st is not None and st[0] is not None:
                deps.add(st[0])
        for k in writes:
            st = res.get(k)
            if st is not None:
                if st[0] is not None:
                    deps.add(st[0])
                deps.update(st[1].values())
                deps.update(st[2])
        for k in reads:
            st = res.get(k)
            if st is None:
                st = res[k] = [None, {}, []]
            if dma:
                st[2].append(op)
            else:
                st[1][eng] = op
        for k in writes:
            res[k] = [op, {}, []]
        pb = self.pending_barrier[eng]
        if pb is not None:
            deps.update(pb)
            self.pending_barrier[eng] = None
        if dma:
            h = self.dma_hist[eng]
            k = len(h)
            op.dsem = k % NDMASEM
            op.dval = 16 * (k // NDMASEM + 1)
            if k >= NDMASEM:
                deps.add(h[k - NDMASEM])
            h.append(op)
            self.dma_since_barrier.append(op)
        deps.discard(op)
        op.deps = deps
        for p in deps:
            if not p.is_dma:
                if not (p.eng == 'pe' and eng == 'pe'):
                    p.signal = True
        self.ops[eng].append(op)
        return op

    def finalize(self, final_waits=()):
        nc = self.nc
        nepoch = {}
        for e in ENGS:
            c = 0
            ep = 0
            for op in self.ops[e]:
                if op.signal and not op.is_dma:
                    if c >= EPOCH:
                        ep += 1
                        c = 0
                    c += 1
                    op.cnt = c
                    op.epoch = ep
            nepoch[e] = ep + 1
        with contextlib.ExitStack() as es:
            csem = {}
            for e in ENGS:
                for ep in range(nepoch[e]):
                    csem[(e, ep)] = es.enter_context(nc.semaphore(f"c_{e}_{ep}"))
            dsem = {}
            for e in ENGS:
                if self.dma_hist[e]:
                    for i in range(NDMASEM):
                        dsem[(e, i)] = es.enter_context(nc.semaphore(f"d_{e}_{i}"))
            block = es.enter_context(nc.Block())
            getter = {'pe': block.tensor, 'act': block.scalar, 'dve': block.vector,
                      'pool': block.gpsimd, 'sp': block.sync}
            finals = list(final_waits)

            def make(e):
                def body(engobj):
                    waited = {}
                    for op in self.ops[e]:
                        for p in op.deps:
                            if p.is_dma:
                                key = ('d', p.eng, p.dsem)
                                if waited.get(key, 0) >= p.dval:
                                    continue
                                waited[key] = p.dval
                                engobj.wait_ge(dsem[(p.eng, p.dsem)], p.dval)
                            else:
                                if p.eng == 'pe' and e == 'pe':
                                    continue
                                key = ('c', p.eng)
                                val = (p.epoch, p.cnt)
                                if waited.get(key, (-1, 0)) >= val:
                                    continue
                                waited[key] = val
                                engobj.wait_ge(csem[(p.eng, p.epoch)], p.cnt)
                        ins = op.fn(engobj)
                        if op.is_dma:
                            ins.then_inc(dsem[(e, op.dsem)], 16)
                        elif op.signal:
                            ins.then_inc(csem[(e, op.epoch)], 1)
                    if e == 'sp':
                        for p in finals:
                            engobj.wait_ge(dsem[(p.eng, p.dsem)], p.dval)
                return body

            for e in ENGS:
                if self.ops[e] or e == 'sp':
                    getter[e](make(e))


class Arena:
    def __init__(self, tensor, nwords):
        self.t = tensor
        self.n = nwords
        self.top = 0
        self.peak = 0

    def alloc(self, free_shape, dtype):
        n = int(np.prod(free_shape))
        words = n if dtype == F32 else (n + 1) // 2
        words = (words + 7) // 8 * 8
        off = self.top
        self.top += words
        self.peak = max(self.peak, self.top)
        assert self.top <= self.n, f"arena overflow {self.top} > {self.n}"
        ap = self.t[:, off:off + words]
        if dtype != F32:
            ap = ap.bitcast(dtype)
        ap = ap[:, 0:n]
        if len(free_shape) == 2:
            ap = ap.rearrange("p (a b) -> p a b", a=free_shape[0])
        elif len(free_shape) == 3:
            ap = ap.rearrange("p (a b c) -> p a b c", a=free_shape[0], b=free_shape[1])
        elif len(free_shape) == 4:
            ap = ap.rearrange("p (a b c d) -> p a b c d", a=free_shape[0], b=free_shape[1], c=free_shape[2])
        return ap

    def mark(self):
        return self.top

    def release(self, m):
        self.top = m


def _smalls_layout(nseq):
    items = [('c', 8 * nseq), ('bada', 4 * 48), ('gmix', 32), ('gffn', 32), ('gfin', 8),
             ('kvg', 4), ('ikg', 2), ('ikb', 2), ('bq', 16), ('bk2', 4), ('bout', 16),
             ('sinks', 32), ('rb31', 16), ('bv2', 512)]
    off = {}
    o = 0
    for k, n in items:
        off[k] = (o, n)
        o += n
    return off, o


ARENA_WORDS = 53000


def build(layers, nseq, final_norm):
    DBG = os.environ.get('KDBG', '').split(',')
    nc = bass.Bass("TRN2", target_bir_lowering=False)
    SM, NS = _smalls_layout(nseq)
    dtn = nc.dram_tensor
    xin = dtn("xin", [nseq, D, SEQ], F32, kind="ExternalInput").ap()
    xout = dtn("xout", [nseq, D, SEQ], F32, kind="ExternalOutput").ap()
    smalls_d = dtn("smalls", [128, NS], F32, kind="ExternalInput").ap()
    consts_d = dtn("consts", [128, 5 * 128], F32, kind="ExternalInput").ap()
    tb_d = dtn("tbias", [2, 128, NH * 128], F32, kind="ExternalInput").ap()
    w_ada = dtn("w_ada", [DEPTH, D, 6 * D], F32, kind="ExternalInput").ap()
    a_w_in = dtn("a_w_in", [2, D, A_IN], F32, kind="ExternalInput").ap()
    a_w_ukT = dtn("a_w_ukT", [2, 128, 8 * 256], F32, kind="ExternalInput").ap()
    a_w_uv = dtn("a_w_uv", [2, NH, 256, 64], F32, kind="ExternalInput").ap()
    a_w_out = dtn("a_w_out", [2, D, D], F32, kind="ExternalInput").ap()
    b_w_in = dtn("b_w_in", [2, D, B_IN], F32, kind="ExternalInput").ap()
    b_w_out = dtn("b_w_out", [2, D, D], F32, kind="ExternalInput").ap()
    ffn_w1 = dtn("ffn_w1", [DEPTH, D, DFF], F32, kind="ExternalInput").ap()
    ffn_w3 = dtn("ffn_w3", [DEPTH, D, DFF], F32, kind="ExternalInput").ap()
    ffn_w2 = dtn("ffn_w2", [DEPTH, DFF, D], F32, kind="ExternalInput").ap()
    maskD = dtn("maskD", [NTILE, 128, NTILE * 128], BF16).ap()

    es = contextlib.ExitStack()
    arena_t = es.enter_context(nc.sbuf_tensor("arena", [128, ARENA_WORDS], F32))
    PSB = [es.enter_context(nc.psum_tensor(f"ps{i}", [128, 512], F32)) for i in range(8)]
    S = Sched(nc)
    AR = Arena(arena_t, ARENA_WORDS)

    def ps(i):
        return PSB[i][:, :]

    def psk(i):
        return ('ps', i)

    def MM(out, lhsT, rhs, start, stop, rd, wr):
        S.add('pe', lambda e: e.matmul(out, lhsT=lhsT, rhs=rhs, start=start, stop=stop), reads=rd, writes=wr)

    def ACT(out, in_, func, rd, wr, bias=None, scale=None):
        kw = {}
        if bias is not None:
            kw['bias'] = bias
        if scale is not None:
            kw['scale'] = scale
        S.add('act', lambda e: e.activation(out=out, in_=in_, func=func, **kw), reads=rd, writes=wr)

    def TS(out, in0, s1, s2, op0, op1, rd, wr, eng='dve'):
        if op1 is None:
            S.add(eng, lambda e: e.tensor_scalar(out=out, in0=in0, scalar1=s1, scalar2=None, op0=op0), reads=rd, writes=wr)
        else:
            S.add(eng, lambda e: e.tensor_scalar(out=out, in0=in0, scalar1=s1, scalar2=s2, op0=op0, op1=op1), reads=rd, writes=wr)

    def TT(out, in0, in1, op, rd, wr, eng='dve'):
        S.add(eng, lambda e: e.tensor_tensor(out=out, in0=in0, in1=in1, op=op), reads=rd, writes=wr)

    def STT(out, in0, scalar, in1, op0, op1, rd, wr):
        S.add('dve', lambda e: e.scalar_tensor_tensor(out=out, in0=in0, scalar=scalar, in1=in1, op0=op0, op1=op1), reads=rd, writes=wr)

    def CP(out, in_, rd, wr, eng='dve'):
        if eng == 'act':
            S.add(eng, lambda e: e.activation(out=out, in_=in_, func=AF.Identity), reads=rd, writes=wr)
        else:
            S.add(eng, lambda e: e.tensor_copy(out=out, in_=in_), reads=rd, writes=wr)

    def RECIP(out, in_, rd, wr):
        S.add('dve', lambda e: e.reciprocal(out=out, in_=in_), reads=rd, writes=wr)

    def MEMSET(ap, val, wr, eng='dve'):
        S.add(eng, lambda e: e.memset(ap, val), writes=wr)

    def DMA(q, out, in_, rd, wr):
        return S.add(q, lambda e: e.dma_start(out=out, in_=in_), reads=rd, writes=wr, dma=True)

    xT = AR.alloc((8, SEQ), F32)
    hT = AR.alloc((8, SEQ), BF16)
    smalls = AR.alloc((NS,), F32)
    cst = AR.alloc((5, 128), F32)
    ident_bf = AR.alloc((128,), BF16)
    ones_bf = AR.alloc((128,), BF16)
    ones_f = AR.alloc((128,), F32)
    Tcur = AR.alloc((NH, 128), F32)
    Tprev = AR.alloc((NH, 128), F32)
    modT = AR.alloc((DEPTH, 48, nseq), F32)
    aT = AR.alloc((DEPTH, 2, 8, nseq), F32)
    gbo = AR.alloc((DEPTH, 8, nseq), F32)
    esink = AR.alloc((2, NH), F32)
    cs_bf = AR.alloc((8, nseq), BF16)
    cols = AR.alloc((8,), F32)
    m8 = AR.alloc((8,), F32)
    PBASE = AR.mark()

    def sm(name):
        o, n = SM[name]
        return smalls[:, o:o + n]

    DMA('sp', smalls, smalls_d, [], ['smalls'])
    DMA('sp', cst, consts_d.rearrange("p (a b) -> p a b", a=5), [], ['cst'])
    DMA('sp', Tcur, tb_d[0].rearrange("p (a b) -> p a b", a=NH), [], ['Tcur'])
    DMA('sp', Tprev, tb_d[1].rearrange("p (a b) -> p a b", a=NH), [], ['Tprev'])
    MEMSET(ones_bf, 1.0, ['ones_bf'])
    MEMSET(ones_f, 1.0, ['ones_f'])
    MEMSET(cols[:, 0:1], 1e-6, ['cols'])
    CP(ident_bf, cst[:, 0, :], ['cst'], ['ident_bf'])
    ident_f = cst[:, 0, :]
    onesblk = cst[:, 1, :]
    causal_add = cst[:, 2, :]
    prev_add = cst[:, 3, :]
    negtri = cst[:, 4, :]
    eps_col = cols[:, 0:1]
    rb31 = sm('rb31')
    rb31_b = rb31.unsqueeze(2).to_broadcast([128, NH, 128])
    TT(Tcur, Tcur, rb31_b, ALU.subtract, ['Tcur', 'smalls'], ['Tcur'])
    TT(Tprev, Tprev, rb31_b, ALU.subtract, ['Tprev', 'smalls'], ['Tprev'])
    TT(Tcur, Tcur, causal_add.unsqueeze(1).to_broadcast([128, NH, 128]), ALU.add, ['Tcur', 'cst'], ['Tcur'])
    sinks = sm('sinks').rearrange("p (a b) -> p a b", a=2)
    TT(esink, sinks, rb31.unsqueeze(1).to_broadcast([128, 2, NH]), ALU.subtract, ['smalls'], ['esink'])
    ACT(esink, esink, AF.Exp, ['esink'], ['esink'])
    cview = sm('c').rearrange("p (a b) -> p a b", a=8)
    ACT(cs_bf, cview, AF.Silu, ['smalls'], ['cs_bf'])

    class Slots:
        def __init__(self, n, tag):
            self.bufs = [AR.alloc((8 * 512,), BF16) for _ in range(n)]
            self.n = n
            self.i = 0
            self.tag = tag

        def next(self):
            k = self.i % self.n
            self.i += 1
            return self.bufs[k], (self.tag, k)

    def load_w(slots, src, kc, ncol):
        buf, key = slots.next()
        v = buf[:, 0:kc * ncol].rearrange("p (a b) -> p a b", a=kc)
        DMA('pool', v, src, [], [key])
        return v, key

    def wrows(w2d):
        return w2d.rearrange("(c p) n -> p c n", p=128)

    S.barrier()
    m0 = AR.mark()
    sl = Slots(2, 'wsl')
    bada = sm('bada').rearrange("p (a b) -> p a b", a=4)
    gmix = sm('gmix').rearrange("p (a b) -> p a b", a=4)
    gffn = sm('gffn').rearrange("p (a b) -> p a b", a=4)
    bout = sm('bout').rearrange("p (a b) -> p a b", a=2)
    for l in layers:
        wv = wrows(w_ada[l])
        pm = PSB[0][:, 0:48 * nseq]
        for blk in range(12):
            v, key = load_w(sl, wv[:, :, blk * 512:(blk + 1) * 512], 8, 512)
            for fcl in range(4):
                ch = blk * 4 + fcl
                for kc in range(8):
                    MM(pm[:, ch * nseq:(ch + 1) * nseq], v[:, kc, fcl * 128:(fcl + 1) * 128], cs_bf[:, kc, :],
                       kc == 0, kc == 7, [key, 'cs_bf'], [psk(0)])
        TT(modT[:, l], pm.rearrange("p (a b) -> p a b", a=48), bada[:, l, :].unsqueeze(2).to_broadcast([128, 48, nseq]),
           ALU.add, [psk(0), 'smalls'], ['modT'])
        for which, (g, sc0) in enumerate(((gmix, 8), (gffn, 32))):
            TS(aT[:, l, which], modT[:, l, sc0:sc0 + 8, :], 1.0, None, ALU.add, None, ['modT'], ['aT'])
            TT(aT[:, l, which], aT[:, l, which], g[:, l, :].unsqueeze(2).to_broadcast([128, 8, nseq]), ALU.mult,
               ['aT', 'smalls'], ['aT'])
        if l % 2 == 1:
            TT(gbo[:, l], modT[:, l, 16:24, :], bout[:, l // 2, :].unsqueeze(2).to_broadcast([128, 8, nseq]), ALU.mult,
               ['modT', 'smalls'], ['gbo'])
    AR.release(m0)

    def norm_mod(a_of_c, b_of_c, out_fn):
        m = AR.mark()
        sq = [AR.alloc((512,), F32) for _ in range(2)]
        tmp = [AR.alloc((512,), F32) for _ in range(2)]
        sd = AR.alloc((512,), F32)
        rstd = AR.alloc((512,), F32)
        for tg in range(4):
            tsl = slice(tg * 512, (tg + 1) * 512)
            for c in range(8):
                ACT(sq[c % 2], xT[:, c, tsl], AF.Square, [('x', c, tg)], [('sq', c % 2)])
                MM(ps(0), ones_f, sq[c % 2], c == 0, c == 7, [('sq', c % 2), 'ones_f'], [psk(0)])
            ACT(sd, ps(0), AF.Sqrt, [psk(0), 'cols'], ['sd'], bias=eps_col, scale=1.0 / D)
            RECIP(rstd, sd, ['sd'], ['rstd'])
            for c in range(8):
                TT(tmp[c % 2], xT[:, c, tsl], rstd, ALU.mult, [('x', c, tg), 'rstd'], [('ntmp', c % 2)])
                out_fn(c, tg, tmp[c % 2], ('ntmp', c % 2))
        AR.release(m)

    def norm_to_h(l, which, s):
        sh0 = 0 if which == 0 else 24

        def out_fn(c, tg, t_ap, t_key):
            ACT(hT[:, c, tg * 512:(tg + 1) * 512], t_ap, AF.Identity, [t_key, 'aT', 'modT'], [('h', c, tg)],
                bias=modT[:, l, sh0 + c, s:s + 1], scale=aT[:, l, which, c, s:s + 1])
        norm_mod(None, None, out_fn)

    def ffn(l, s):
        S.barrier()
        m = AR.mark()
        uT = AR.alloc((NFC, 1024), BF16)
        sl = Slots(4, 'wsl')
        sg = [AR.alloc((512,), F32) for _ in range(2)]
        w1v = wrows(ffn_w1[l])
        w3v = wrows(ffn_w3[l])
        w2v = ffn_w2[l].rearrange("(f p) n -> p f n", p=128)
        cnt = 0
        for th in range(2):
            for fb in range(6):
                nfc = 4 if fb < 5 else 2
                ncol = nfc * 128
                va, ka = load_w(sl, w1v[:, :, fb * 512:fb * 512 + ncol], 8, ncol)
                vb, kb = load_w(sl, w3v[:, :, fb * 512:fb * 512 + ncol], 8, ncol)
                for fcl in range(nfc):
                    fc = fb * 4 + fcl
                    for t2 in range(2):
                        tg = th * 2 + t2
                        b1 = (cnt % 2) * 2
                        b3 = b1 + 1
                        for kc in range(8):
                            MM(ps(b1), va[:, kc, fcl * 128:(fcl + 1) * 128], hT[:, kc, tg * 512:(tg + 1) * 512],
                               kc == 0, kc == 7, [ka, ('h', kc, tg)], [psk(b1)])
                        for kc in range(8):
                            MM(ps(b3), vb[:, kc, fcl * 128:(fcl + 1) * 128], hT[:, kc, tg * 512:(tg + 1) * 512],
                               kc == 0, kc == 7, [kb, ('h', kc, tg)], [psk(b3)])
                        ACT(sg[cnt % 2], ps(b1), AF.Silu, [psk(b1)], [('sg', cnt % 2)])
                        TT(uT[:, fc, t2 * 512:(t2 + 1) * 512], sg[cnt % 2], ps(b3), ALU.mult,
                           [('sg', cnt % 2), psk(b3)], [('u', fc, t2)])
                        cnt += 1
            for t2 in range(2):
                tg = th * 2 + t2
                for fcb in range(6):
                    nfc = 4 if fcb < 5 else 2
                    buf, key = sl.next()
                    v = buf[:, 0:nfc * 1024].rearrange("p (a b) -> p a b", a=nfc)
                    DMA('pool', v, w2v[:, fcb * 4:fcb * 4 + nfc, :], [], [key])
                    for fcl in range(nfc):
                        fc = fcb * 4 + fcl
                        for fo in range(8):
                            MM(ps(fo), v[:, fcl, fo * 128:(fo + 1) * 128], uT[:, fc, t2 * 512:(t2 + 1) * 512],
                               fc == 0, fc == NFC - 1, [key, ('u', fc, t2)], [psk(fo)])
                for fo in range(8):
                    xs = xT[:, fo, tg * 512:(tg + 1) * 512]
                    STT(xs, ps(fo), modT[:, l, 40 + fo, s:s + 1], xs, ALU.mult, ALU.add,
                        [psk(fo), 'modT', ('x', fo, tg)], [('x', fo, tg)])
        AR.release(m)

    def out_proj(w_out2d, l, s, has_bias):
        S.barrier()
        m = AR.mark()
        sl = Slots(2, 'wsl')
        wv = wrows(w_out2d)
        for half in range(2):
            v, key = load_w(sl, wv[:, :, half * 512:(half + 1) * 512], 8, 512)
            for fl in range(4):
                fo = half * 4 + fl
                for tg in range(4):
                    b = (fl * 4 + tg) % 4
                    for kc in range(8):
                        MM(ps(b), v[:, kc, fl * 128:(fl + 1) * 128], hT[:, kc, tg * 512:(tg + 1) * 512],
                           kc == 0, kc == 7, [key, ('h', kc, tg)], [psk(b)])
                    xs = xT[:, fo, tg * 512:(tg + 1) * 512]
                    STT(xs, ps(b), modT[:, l, 16 + fo, s:s + 1], xs, ALU.mult, ALU.add,
                        [psk(b), 'modT', ('x', fo, tg)], [('x', fo, tg)])
                    if has_bias:
                        TS(xs, xs, gbo[:, l, fo, s:s + 1], None, ALU.add, None, [('x', fo, tg), 'gbo'], [('x', fo, tg)])
        AR.release(m)

    def mixer_b(l, s):
        j = l // 2
        S.barrier()
        m = AR.mark()
        qT = AR.alloc((8, SEQ), BF16)
        kT2 = AR.alloc((2, SEQ), BF16)
        v2 = AR.alloc((NTILE, 256), BF16)
        bq = sm('bq').rearrange("p (a b) -> p a b", a=2)
        bk2 = sm('bk2').rearrange("p (a b) -> p a b", a=2)
        bv2 = sm('bv2').rearrange("p (a b) -> p a b", a=2)
        m1 = AR.mark()
        sl = Slots(2, 'wsl')
        wv = wrows(b_w_in[j])
        for half in range(2):
            v, key = load_w(sl, wv[:, :, half * 512:(half + 1) * 512], 8, 512)
            for fl in range(4):
                c = half * 4 + fl
                for tg in range(4):
                    b = (fl * 4 + tg) % 4
                    for kc in range(8):
                        MM(ps(b), v[:, kc, fl * 128:(fl + 1) * 128], hT[:, kc, tg * 512:(tg + 1) * 512],
                           kc == 0, kc == 7, [key, ('h', kc, tg)], [psk(b)])
                    ACT(qT[:, c, tg * 512:(tg + 1) * 512], ps(b), AF.Identity, [psk(b), 'smalls'], [('q', c, tg)],
                        bias=bq[:, j, c:c + 1])
        buf, key = sl.next()
        v = buf[:, :].rearrange("p (a b) -> p a b", a=8)
        for kvh in range(2):
            for dup in range(2):
                DMA('pool', v[:, :, kvh * 128 + dup * 64:kvh * 128 + dup * 64 + 64],
                    wv[:, :, 1024 + kvh * 64:1024 + kvh * 64 + 64], [], [key])
                DMA('pool', v[:, :, 256 + kvh * 128 + dup * 64:256 + kvh * 128 + dup * 64 + 64],
                    wv[:, :, 1152 + kvh * 64:1152 + kvh * 64 + 64], [], [key])
        for kvh in range(2):
            for tg in range(4):
                b = tg % 4
                for kc in range(8):
                    MM(ps(b), v[:, kc, kvh * 128:(kvh + 1) * 128], hT[:, kc, tg * 512:(tg + 1) * 512],
                       kc == 0, kc == 7, [key, ('h', kc, tg)], [psk(b)])
                ACT(kT2[:, kvh, tg * 512:(tg + 1) * 512], ps(b), AF.Identity, [psk(b), 'smalls'], [('k2', kvh, tg)],
                    bias=bk2[:, j, kvh:kvh + 1])
        for st in range(NTILE):
            b = st % 4
            for kc in range(8):
                MM(PSB[b][:, 0:256], hT[:, kc, st * 128:(st + 1) * 128], v[:, kc, 256:512],
                   kc == 0, kc == 7, [key, ('h', kc, st // 4)], [psk(b)])
            TT(v2[:, st, :], PSB[b][:, 0:256], bv2[:, j, :], ALU.add, [psk(b), 'smalls'], [('v2', st)])
        S.barrier()
        AR.release(m1)
        tmpb = [AR.alloc((4, 128), F32) for _ in range(2)]
        pb = [AR.alloc((512,), BF16) for _ in range(4)]
        lnd = AR.alloc((512,), F32)
        rec = AR.alloc((4, 128), F32)
        it = 0
        pi = 0
        for n in range(NTILE):
            tsl = slice(n * 128, (n + 1) * 128)
            for kvh in range(2):
                for par in range(2):
                    psl = slice(par * 64, (par + 1) * 64)
                    kts = ([n - 1] if n > 0 else []) + [n]
                    plist = []
                    for kt in kts:
                        bl = it % 2
                        MM(ps(bl), kT2[psl, kvh, kt * 128:(kt + 1) * 128], qT[psl, 4 * kvh:4 * kvh + 4, tsl],
                           True, True, [('k2', kvh, kt // 4), ('q', 4 * kvh, n // 4), ('q', 4 * kvh + 1, n // 4),
                                        ('q', 4 * kvh + 2, n // 4), ('q', 4 * kvh + 3, n // 4)], [psk(bl)])
                        tb = tmpb[it % 2]
                        tkey = ('tmpb', it % 2)
                        T = Tcur if kt == n else Tprev
                        hs = 8 * kvh + par
                        Tsel = T[:, hs:hs + 7:2, :]
                        STT(tb, PSB[bl][:, :].rearrange("p (a b) -> p a b", a=4), 0.125, Tsel, ALU.mult, ALU.add,
                            [psk(bl), 'Tcur', 'Tprev'], [tkey])
                        if kt != n:
                            TT(tb, tb, prev_add.unsqueeze(1).to_broadcast([128, 4, 128]), ALU.add, [tkey, 'cst'], [tkey])
                        p_ap = pb[pi % 4]
                        pkey = ('pb', pi % 4)
                        pi += 1
                        ACT(p_ap, tb.rearrange("p a b -> p (a b)"), AF.Exp, [tkey], [pkey])
                        plist.append((p_ap, pkey, kt))
                        it += 1
                    for ii, (p_ap, pkey, kt) in enumerate(plist):
                        MM(ps(2), ones_bf, p_ap, ii == 0, ii == len(plist) - 1, [pkey, 'ones_bf'], [psk(2)])
                    for ii, (p_ap, pkey, kt) in enumerate(plist):
                        MM(ps(3), v2[:, kt, kvh * 128:(kvh + 1) * 128], p_ap, ii == 0, ii == len(plist) - 1,
                           [pkey, ('v2', kt)], [psk(3)])
                    hs = 8 * kvh + par
                    es_b = esink[:, j, hs:hs + 7:2].unsqueeze(2).to_broadcast([128, 4, 128])
                    TT(rec, PSB[2][:, :].rearrange("p (a b) -> p a b", a=4), es_b, ALU.add, [psk(2), 'esink'], ['rec'])
                    ACT(lnd, rec.rearrange("p a b -> p (a b)"), AF.Ln, ['rec'], ['lnd'])
                    ACT(rec.rearrange("p a b -> p (a b)"), lnd, AF.Exp, ['lnd'], ['rec'], scale=-1.0)
                    TT(hT[psl, 4 * kvh:4 * kvh + 4, tsl], PSB[3][psl, :].rearrange("p (a b) -> p a b", a=4), rec[psl],
                       ALU.mult, [psk(3), 'rec'], [('h', 4 * kvh + cc, n // 4) for cc in range(4)])
        AR.release(m)
        out_proj(b_w_out[j], l, s, True)

    def mixer_a(l, s):
        j = l // 2
        S.barrier()
        m = AR.mark()
        wv = wrows(a_w_in[j])
        kvg = sm('kvg').rearrange("p (a b) -> p a b", a=2)
        ikg = sm('ikg')
        ikb = sm('ikb')
        qiT = AR.alloc((4, SEQ), BF16)
        kiT = AR.alloc((SEQ,), BF16)
        wi = AR.alloc((NTILE, 8), F32)
        m1 = AR.mark()
        sl = Slots(2, 'wsl')
        v, key = load_w(sl, wv[:, :, 1280:1792], 8, 512)
        for c in range(4 if 'noqi' not in DBG else 0):
            for tg in range(4):
                b = tg % 4
                for kc in range(8):
                    MM(ps(b), v[:, kc, c * 128:(c + 1) * 128], hT[:, kc, tg * 512:(tg + 1) * 512],
                       kc == 0, kc == 7, [key, ('h', kc, tg)], [psk(b)])
                CP(qiT[:, c, tg * 512:(tg + 1) * 512], ps(b), [psk(b)], [('qi', c, tg)], eng='act' if tg % 2 else 'dve')
        buf, key = sl.next()
        v = buf[:, 0:8 * 256].rearrange("p (a b) -> p a b", a=8)
        DMA('pool', v[:, :, 0:64], wv[:, :, 1792:1856], [], [key])
        DMA('pool', v[:, :, 64:128], wv[:, :, 1792:1856], [], [key])
        DMA('pool', v[:, :, 128:192], wv[:, :, 1800:1864], [], [key])
        kraw = AR.alloc((512,), F32)
        kcen = AR.alloc((512,), F32)
        ksq = AR.alloc((512,), F32)
        ksd = AR.alloc((512,), F32)
        krs = AR.alloc((512,), F32)
        for tg in range(4 if 'noki' not in DBG else 0):
            tsl = slice(tg * 512, (tg + 1) * 512)
            for kc in range(8):
                MM(ps(0), v[:, kc, 0:128], hT[:, kc, tsl], kc == 0, kc == 7, [key, ('h', kc, tg)], [psk(0)])
            CP(kraw, ps(0), [psk(0)], ['kraw'])
            MM(ps(1), onesblk, kraw, True, True, ['kraw', 'cst'], [psk(1)])
            TT(kcen, kraw, ps(1), ALU.subtract, ['kraw', psk(1)], ['kcen'])
            ACT(ksq, kcen, AF.Square, ['kcen'], ['ksq'])
            MM(ps(1), onesblk, ksq, True, True, ['ksq', 'cst'], [psk(1)])
            ACT(ksd, ps(1), AF.Sqrt, [psk(1), 'cols'], ['ksd'], bias=eps_col)
            RECIP(krs, ksd, ['ksd'], ['krs'])
            TT(kcen, kcen, krs, ALU.mult, ['kcen', 'krs'], ['kcen'])
            ACT(kiT[:, tsl], kcen, AF.Identity, ['kcen', 'smalls'], [('ki', tg)],
                bias=ikb[:, j:j + 1], scale=ikg[:, j:j + 1])
        for st in range(NTILE if 'nowi' not in DBG else 0):
            b = 2 + st % 2
            for kc in range(8):
                MM(PSB[b][:, 0:8], hT[:, kc, st * 128:(st + 1) * 128], v[:, kc, 184:192],
                   kc == 0, kc == 7, [key, ('h', kc, st // 4)], [psk(b)])
            TS(wi[:, st, :], PSB[b][:, 0:8], 8 ** -0.5 * 64 ** -0.5, None, ALU.mult, None, [psk(b)], [('wi', st)])
        S.barrier()
        AR.release(m1)
        score = AR.alloc((SEQ,), F32)
        mask01 = AR.alloc((SEQ,), BF16)
        rl = [AR.alloc((512,), F32) for _ in range(2)]
        mst = [AR.alloc((NTILE, 128), BF16) for _ in range(2)]
        ri = 0
        tgi = 0
        for i in range(2, NTILE if 'nop1' not in DBG else 0):
            n = 128 * (i + 1)
            nkb = (n + 511) // 512
            for h in range(8):
                psl = slice((h % 2) * 64, (h % 2) * 64 + 64)
                for kb in range(nkb):
                    w = min(512, n - kb * 512)
                    bl = ri % 2
                    MM(PSB[bl][:, 0:w], qiT[psl, h // 2, i * 128:(i + 1) * 128], kiT[psl, kb * 512:kb * 512 + w],
                       True, True, [('qi', h // 2, i // 4), ('ki', kb)], [psk(bl)])
                    r_ap = rl[ri % 2][:, 0:w]
                    rkey = ('rl', ri % 2)
                    ACT(r_ap, PSB[bl][:, 0:w], AF.Relu, [psk(bl)], [rkey])
                    sc_ap = score[:, kb * 512:kb * 512 + w]
                    if h == 0:
                        TS(sc_ap, r_ap, wi[:, i, 0:1], None, ALU.mult, None, [rkey, ('wi', i)], [('score', kb)])
                    else:
                        STT(sc_ap, r_ap, wi[:, i, h:h + 1], sc_ap, ALU.mult, ALU.add,
                            [rkey, ('wi', i), ('score', kb)], [('score', kb)])
                    ri += 1
            allk = [('score', kb) for kb in range(nkb)]
            dsl = slice(i * 128, (i + 1) * 128)
            TT(score[:, dsl], score[:, dsl], negtri, ALU.add, allk + ['cst'], allk)
            for it in range(32 if 'notopk' not in DBG else 0):
                S.add('dve', lambda e, n=n: e.max(out=m8, in_=score[:, 0:n]), reads=allk, writes=['m8'])
                S.add('dve', lambda e, n=n: e.match_replace(out=score[:, 0:n], in_to_replace=m8, in_values=score[:, 0:n],
                                                            imm_value=REPL), reads=allk + ['m8'], writes=allk)
            TS(mask01[:, 0:n], score[:, 0:n], -2.0e38, None, ALU.is_le, None, allk, ['mask01'])
            ms = mst[i % 2]
            mkey = ('mst', i % 2)
            for g in range((i + 1 + 7) // 8):
                j0 = 8 * g
                ng = min(8, i + 1 - j0)
                bank = 6 + (tgi % 2)
                tgi += 1
                pbb = PSB[bank][:, :].bitcast(BF16)
                for jl in range(ng):
                    jj = j0 + jl
                    S.add('pe', lambda e, jj=jj, jl=jl, pbb=pbb: e.transpose(pbb[:, jl * 128:(jl + 1) * 128], mask01[:, jj * 128:(jj + 1) * 128], ident_bf),
                          reads=['mask01', 'ident_bf'], writes=[psk(bank)])
                CP(ms[:, j0:j0 + ng, :].rearrange("p a b -> p (a b)"), pbb[:, 0:ng * 128], [psk(bank)], [mkey], eng='act')
            DMA('sp', maskD[i][:, 0:n], ms[:, 0:i + 1, :].rearrange("p a b -> p (a b)"), [mkey], [('maskD', i)])
        S.barrier()
        AR.release(m)

        m = AR.mark()
        qT = AR.alloc((8, SEQ), BF16)
        ckvT = AR.alloc((2, SEQ), BF16)
        ckv = AR.alloc((NTILE, 256), BF16)
        m1 = AR.mark()
        sl = Slots(2, 'wsl')
        for half in range(2 if 'noq' not in DBG else 0):
            v, key = load_w(sl, wv[:, :, half * 512:(half + 1) * 512], 8, 512)
            for fl in range(4):
                c = half * 4 + fl
                for tg in range(4):
                    b = (fl * 4 + tg) % 4
                    for kc in range(8):
                        MM(ps(b), v[:, kc, fl * 128:(fl + 1) * 128], hT[:, kc, tg * 512:(tg + 1) * 512],
                           kc == 0, kc == 7, [key, ('h', kc, tg)], [psk(b)])
                    CP(qT[:, c, tg * 512:(tg + 1) * 512], ps(b), [psk(b)], [('q', c, tg)], eng='act' if tg % 2 else 'dve')
        v, key = load_w(sl, wv[:, :, 1024:1280], 8, 256)
        craw = AR.alloc((2, 512), F32)
        csq = AR.alloc((512,), F32)
        csd = AR.alloc((512,), F32)
        crs = AR.alloc((512,), F32)
        for tg in range(4 if 'nockv' not in DBG else 0):
            tsl = slice(tg * 512, (tg + 1) * 512)
            for rc in range(2):
                for kc in range(8):
                    MM(ps(rc), v[:, kc, rc * 128:(rc + 1) * 128], hT[:, kc, tsl], kc == 0, kc == 7,
                       [key, ('h', kc, tg)], [psk(rc)])
                CP(craw[:, rc, :], ps(rc), [psk(rc)], [('craw', rc)])
                ACT(csq, craw[:, rc, :], AF.Square, [('craw', rc)], ['csq'])
                MM(ps(2), ones_f, csq, rc == 0, rc == 1, ['csq', 'ones_f'], [psk(2)])
            ACT(csd, ps(2), AF.Sqrt, [psk(2), 'cols'], ['csd'], bias=eps_col, scale=1.0 / 256)
            RECIP(crs, csd, ['csd'], ['crs'])
            for rc in range(2):
                STT(ckvT[:, rc, tsl], craw[:, rc, :], kvg[:, j, rc:rc + 1], crs, ALU.mult, ALU.mult,
                    [('craw', rc), 'crs', 'smalls'], [('ckvT', rc, tg)])
        for sg4 in range(NTILE // 4 if 'notr' not in DBG else 0):
            bank = 6 + (sg4 % 2)
            pbb = PSB[bank][:, :].bitcast(BF16)
            for sl4 in range(4):
                st = sg4 * 4 + sl4
                for rc in range(2):
                    k = sl4 * 2 + rc
                    S.add('pe', lambda e, st=st, rc=rc, k=k, pbb=pbb: e.transpose(pbb[:, k * 128:(k + 1) * 128], ckvT[:, rc, st * 128:(st + 1) * 128], ident_bf),
                          reads=[('ckvT', rc, st // 4), 'ident_bf'], writes=[psk(bank)])
            CP(ckv[:, sg4 * 4:sg4 * 4 + 4, :].rearrange("p a b -> p (a b)"), pbb[:, :], [psk(bank)],
               [('ckv', sg4 * 4 + q) for q in range(4)], eng='act' if sg4 % 2 else 'dve')
        S.barrier()
        AR.release(m1)
        wuk = AR.alloc((8, 256), BF16)
        wuv = AR.alloc((2, NH, 128), BF16)
        if 'nouk' not in DBG:
            DMA('pool', wuk, a_w_ukT[j].rearrange("p (a b) -> p a b", a=8), [], ['wuk'])
        if 'nomemset' not in DBG:
            MEMSET(wuv, 0.0, ['wuv'])
        uvv = a_w_uv[j].rearrange("h (rc p) d -> p rc h d", p=128)
        for rc in range(2 if 'nouv' not in DBG else 0):
            for par in range(2):
                S.add('pool', lambda e, rc=rc, par=par: e.dma_start(out=wuv[:, rc, par:NH:2, par * 64:par * 64 + 64],
                                                                    in_=uvv[:, rc, par:NH:2, :]),
                      reads=[], writes=['wuv'], dma=True)
        qa = [AR.alloc((2, 512), BF16) for _ in range(2)]
        mT = [AR.alloc((NTILE, 128), BF16) for _ in range(1)]
        pb = [AR.alloc((4, 128), BF16) for _ in range(4)]
        olat = [AR.alloc((2, 512), BF16) for _ in range(1)]
        rec = AR.alloc((512,), F32)
        lnd = AR.alloc((512,), F32)
        tmpb = [AR.alloc((4, 128), F32) for _ in range(2)]
        pi = 0
        ti = 0
        gi = 0
        for i in range(NTILE if 'nop2' not in DBG else 0):
            tsl = slice(i * 128, (i + 1) * 128)
            mt = mT[0]
            mtk = ('mT', 0)
            if i >= 2 and 'nop1' not in DBG:
                DMA('sp', mt[:, 0:i + 1, :].rearrange("p a b -> p (a b)"), maskD[i][:, 0:128 * (i + 1)], [('maskD', i)], [mtk])
            for hg in range(4):
                qa_ap = qa[gi % 2]
                qak = ('qa', gi % 2)
                ol = olat[0]
                olk = ('olat', 0)
                gi += 1
                qa4 = qa_ap.rearrange("p r (a b) -> p r a b", a=4)
                for rc in range(2):
                    for hl in range(4):
                        h = 4 * hg + hl
                        psl = slice((h % 2) * 64, (h % 2) * 64 + 64)
                        bq_ = 5 + (hl % 2)
                        MM(PSB[bq_][:, (hl // 2) * 128:(hl // 2) * 128 + 128], wuk[psl, h // 2, rc * 128:(rc + 1) * 128],
                           qT[psl, h // 2, tsl], True, True, ['wuk', ('q', h // 2, i // 4)], [psk(bq_)])
                    CP(qa4[:, rc, 0:4:2, :], PSB[5][:, 0:256].rearrange("p (a b) -> p a b", a=2), [psk(5)], [qak], eng='dve')
                    CP(qa4[:, rc, 1:4:2, :], PSB[6][:, 0:256].rearrange("p (a b) -> p a b", a=2), [psk(6)], [qak], eng='act')
                for jj in range(i + 1):
                    bl = pi % 2
                    for rc in range(2):
                        MM(ps(bl), ckvT[:, rc, jj * 128:(jj + 1) * 128], qa_ap[:, rc, :], rc == 0, rc == 1,
                           [('ckvT', rc, jj // 4), qak], [psk(bl)])
                    p_ap = pb[pi % 4]
                    pkey = ('pb', pi % 4)
                    pi += 1
                    if jj >= i - 1:
                        T = Tcur if jj == i else Tprev
                        tb = tmpb[ti % 2]
                        tkey = ('tmpb', ti % 2)
                        ti += 1
                        STT(tb, PSB[bl][:, :].rearrange("p (a b) -> p a b", a=4), 0.125, T[:, 4 * hg:4 * hg + 4, :],
                            ALU.mult, ALU.add, [psk(bl), 'Tcur', 'Tprev'], [tkey])
                        ACT(p_ap, tb, AF.Exp, [tkey], [pkey])
                    else:
                        ACT(p_ap, PSB[bl][:, :].rearrange("p (a b) -> p a b", a=4), AF.Exp, [psk(bl)], [pkey], scale=0.125)
                    if i >= 2 and 'nop1' not in DBG:
                        TT(p_ap, p_ap, mt[:, jj, :].unsqueeze(1).to_broadcast([128, 4, 128]), ALU.mult,
                           [pkey, mtk], [pkey])
                    p2 = p_ap.rearrange("p a b -> p (a b)")
                    MM(ps(2), ones_bf, p2, jj == 0, jj == i, [pkey, 'ones_bf'], [psk(2)])
                    for rc in range(2):
                        MM(ps(3 + rc), ckv[:, jj, rc * 128:(rc + 1) * 128], p2, jj == 0, jj == i,
                           [pkey, ('ckv', jj)], [psk(3 + rc)])
                ACT(lnd, ps(2), AF.Ln, [psk(2)], ['lnd'])
                ACT(rec, lnd, AF.Exp, ['lnd'], ['rec'], scale=-1.0)
                for rc in range(2):
                    CP(ol[:, rc, :], ps(3 + rc), [psk(3 + rc)], [olk], eng='act' if rc else 'dve')
                for pr in range(2):
                    c = 2 * hg + pr
                    bo = 5 + pr
                    k = 0
                    for par in range(2):
                        hl = 2 * pr + par
                        h = 4 * hg + hl
                        for rc in range(2):
                            MM(PSB[bo][:, 0:128], wuv[:, rc, h, :], ol[:, rc, hl * 128:(hl + 1) * 128], k == 0, k == 3,
                               ['wuv', olk], [psk(bo)])
                            k += 1
                    for par in range(2):
                        hl = 2 * pr + par
                        psl = slice(par * 64, par * 64 + 64)
                        TT(hT[psl, c, tsl], PSB[bo][psl, 0:128], rec[psl, hl * 128:(hl + 1) * 128], ALU.mult,
                           [psk(bo), 'rec'], [('h', c, i // 4)])
        AR.release(m)
        out_proj(a_w_out[j], l, s, False)

    finals = []
    for s in range(nseq):
        S.barrier()
        for c in range(8):
            DMA('sp', xT[:, c, :], xin[s, c * 128:(c + 1) * 128, :], [], [('x', c, tg) for tg in range(4)])
        for l in layers:
            S.barrier()
            norm_to_h(l, 0, s)
            if 'noA' in DBG:
                pass
            elif l % 2 == 0:
                mixer_a(l, s)
            else:
                mixer_b(l, s)
            S.barrier()
            norm_to_h(l, 1, s)
            ffn(l, s)
        S.barrier()
        if final_norm:
            m = AR.mark()
            ost = [AR.alloc((512,), F32) for _ in range(2)]
            gfin = sm('gfin')
            cnt = [0]

            def out_fn(c, tg, t_ap, t_key):
                k = cnt[0] % 2
                cnt[0] += 1
                TS(ost[k], t_ap, gfin[:, c:c + 1], None, ALU.mult, None, [t_key, 'smalls'], [('ost', k)], eng='dve')
                finals.append(DMA('sp', xout[s, c * 128:(c + 1) * 128, tg * 512:(tg + 1) * 512], ost[k], [('ost', k)], []))
            norm_mod(None, None, out_fn)
            AR.release(m)
        else:
            for c in range(8):
                finals.append(DMA('sp', xout[s, c * 128:(c + 1) * 128, :], xT[:, c, :], [('x', c, tg) for tg in range(4)], []))
    S.finalize(final_waits=finals)
    es.close()
    return nc


def _t5_bucket_np(dist):
    d = np.maximum(dist, 0)
    large = 16 + (np.log(np.maximum(d, 1).astype(np.float32) / 16) / math.log(128 / 16) * 16).astype(np.int32)
    large = np.minimum(large, 31)
    return np.where(d < 16, d, large)


def _consts():
    c = np.zeros((128, 5, 128), np.float32)
    idx = np.arange(128)
    c[:, 0, :] = np.eye(128, dtype=np.float32)
    blk = (idx[:, None] // 64) == (idx[None, :] // 64)
    c[:, 1, :] = blk.astype(np.float32) / 64.0
    c[:, 2, :] = np.where(idx[:, None] <= idx[None, :], 0.0, -30000.0)
    c[:, 3, :] = np.where(idx[:, None] > idx[None, :], 0.0, -30000.0)
    c[:, 4, :] = np.where(idx[None, :] <= idx[:, None], 0.0, NEG_BIG)
    return c.reshape(128, 5 * 128)


def _tbias(rel_bias):
    s = np.arange(128)[:, None]
    t = np.arange(128)[None, :]
    out = np.zeros((2, 128, NH, 128), np.float32)
    for k, off in enumerate((0, 128)):
        b = _t5_bucket_np(t - s + off)
        g = rel_bias[b]
        out[k] = np.transpose(g, (0, 2, 1))
    return out.reshape(2, 128, NH * 128)


def _fm(v):
    v = np.asarray(v, np.float32)
    lead = v.shape[:-1]
    r = v.reshape(lead + (v.shape[-1] // 128, 128))
    return np.moveaxis(r, -1, 0)


def _smalls(inp, core, nseq):
    SM, NS = _smalls_layout(nseq)
    sm = np.zeros((128, NS), np.float32)

    def put(name, arr):
        o, n = SM[name]
        sm[:, o:o + n] = np.asarray(arr, np.float32).reshape(128, n)
    c = inp['c'][core * nseq:(core + 1) * nseq]
    put('c', np.transpose(_fm(c), (0, 2, 1)))
    put('bada', _fm(inp['b_ada']))
    put('gmix', _fm(inp['norm_mix_g']))
    put('gffn', _fm(inp['norm_ffn_g']))
    put('gfin', _fm(inp['norm_final_g']))
    put('kvg', _fm(inp['a_kv_norm_g']))
    put('ikg', np.concatenate([inp['a_idx_k_g'].T, inp['a_idx_k_g'].T], 0))
    put('ikb', np.concatenate([inp['a_idx_k_b'].T, inp['a_idx_k_b'].T], 0))
    bin_ = inp['b_b_in']
    put('bq', _fm(bin_[:, 0:1024]))
    bk = bin_[:, 1024:1152].reshape(2, 2, 64)
    put('bk2', np.concatenate([np.transpose(bk, (2, 0, 1))] * 2, 0))
    put('bout', _fm(inp['b_b_out']))
    put('sinks', np.broadcast_to(inp['b_sinks'].reshape(1, 32), (128, 32)))
    put('rb31', np.broadcast_to(inp['rel_bias'][31].reshape(1, 16), (128, 16)))
    bv = bin_[:, 1152:1280].reshape(2, 2, 1, 64)
    bv2 = np.broadcast_to(bv, (2, 2, 2, 64)).reshape(1, 512)
    put('bv2', np.broadcast_to(bv2, (128, 512)))
    return sm


_NC_CACHE = {}


def _get_nc(layers, nseq, final_norm):
    key = (tuple(layers), nseq, final_norm)
    if key not in _NC_CACHE:
        _NC_CACHE[key] = build(list(layers), nseq, final_norm)
    return _NC_CACHE[key]


FUSED = False


def kernel(**inp):
    inp = {k: np.asarray(v) for k, v in inp.items()}
    nseq = 2
    x = inp['x']
    xT = np.ascontiguousarray(np.transpose(x, (0, 2, 1)))
    consts = _consts()
    tb = _tbias(inp['rel_bias'])
    ukT = np.ascontiguousarray(
        np.transpose(inp['a_w_uk'].reshape(2, 8, 2, 256, 64), (0, 2, 4, 1, 3))).reshape(2, 128, 8 * 256)
    shared = {
        'consts': consts, 'tbias': tb, 'w_ada': inp['w_ada'], 'a_w_in': inp['a_w_in'], 'a_w_ukT': ukT,
        'a_w_uv': inp['a_w_uv'], 'a_w_out': inp['a_w_out'], 'b_w_in': inp['b_w_in'], 'b_w_out': inp['b_w_out'],
        'ffn_w1': inp['ffn_w1'], 'ffn_w3': inp['ffn_w3'], 'ffn_w2': inp['ffn_w2'],
    }
    smalls = [_smalls(inp, c, nseq) for c in range(NCORES)]
    cur = [xT[c * nseq:(c + 1) * nseq] for c in range(NCORES)]
    plan = [([0, 1, 2, 3], True)] if FUSED else [([0], False), ([1], False), ([2], False), ([3], True)]
    for layers, fin in plan:
        nc = _get_nc(layers, nseq, fin)
        in_maps = [dict(shared, xin=np.ascontiguousarray(cur[c]), smalls=smalls[c]) for c in range(NCORES)]
        res = run_bass_kernel_spmd(nc, in_maps, core_ids=list(range(NCORES)))
        cur = [res.results[c]['xout'] for c in range(NCORES)]
    out = np.concatenate(cur, 0)
    return np.ascontiguousarray(np.transpose(out, (0, 2, 1))).astype(np.float32)
```

```python
import contextlib
import math
import os

import numpy as np
import concourse.bass as bass
import concourse.mybir as mybir
from concourse.bass_utils import run_bass_kernel_spmd

F32 = mybir.dt.float32
BF16 = mybir.dt.bfloat16
AF = mybir.ActivationFunctionType
ALU = mybir.AluOpType

D = 1024
SEQ = 2048
DEPTH = 4
NH = 16
DFF = 2816
NFC = DFF // 128
NTILE = SEQ // 128
NCORES = 8
A_IN = 1864
B_IN = 1280
NEG_BIG = -1.0e30
REPL = -3.0e38

ENGS = ('pe', 'act', 'dve', 'pool', 'sp')
NDMASEM = 12
EPOCH = 3000


class Op:
    __slots__ = ('eng', 'fn', 'deps', 'signal', 'is_dma', 'dsem', 'dval', 'cnt', 'epoch')

    def __init__(self, eng, fn, is_dma):
        self.eng = eng
        self.fn = fn
        self.deps = ()
        self.signal = False
        self.is_dma = is_dma
        self.dsem = None
        self.dval = 0
        self.cnt = 0
        self.epoch = 0


class Sched:
    def __init__(self, nc):
        self.nc = nc
        self.ops = {e: [] for e in ENGS}
        self.res = {}
        self.dma_hist = {e: [] for e in ENGS}
        self.pending_barrier = {e: None for e in ENGS}
        self.dma_since_barrier = []

    def barrier(self):
        deps = set(self.dma_since_barrier)
        for e in ENGS:
            if self.ops[e]:
                last = self.ops[e][-1]
                deps.add(last)
        self.dma_since_barrier = []
        for e in ENGS:
            old = self.pending_barrier[e]
            self.pending_barrier[e] = set(deps) | (old or set())

    def add(self, eng, fn, reads=(), writes=(), dma=False):
        op = Op(eng, fn, dma)
        deps = set()
        res = self.res
        for k in reads:
            st = res.get(k)
            if st is not None and st[0] is not None:
                deps.add(st[0])
        for k in writes:
            st = res.get(k)
            if st is not None:
                if st[0] is not None:
                    deps.add(st[0])
                deps.update(st[1].values())
                deps.update(st[2])
        for k in reads:
            st = res.get(k)
            if st is None:
                st = res[k] = [None, {}, []]
            if dma:
                st[2].append(op)
            else:
                st[1][eng] = op
        for k in writes:
            res[k] = [op, {}, []]
        pb = self.pending_barrier[eng]
        if pb is not None:
            deps.update(pb)
            self.pending_barrier[eng] = None
        if dma:
            h = self.dma_hist[eng]
            k = len(h)
            op.dsem = k % NDMASEM
            op.dval = 16 * (k // NDMASEM + 1)
            if k >= NDMASEM:
                deps.add(h[k - NDMASEM])
            h.append(op)
            self.dma_since_barrier.append(op)
        deps.discard(op)
        op.deps = deps
        for p in deps:
            if not p.is_dma:
                if not (p.eng == 'pe' and eng == 'pe'):
                    p.signal = True
        self.ops[eng].append(op)
        return op

    def finalize(self, final_waits=()):
        nc = self.nc
        nepoch = {}
        for e in ENGS:
            c = 0
            ep = 0
            for op in self.ops[e]:
                if op.signal and not op.is_dma:
                    if c >= EPOCH:
                        ep += 1
                        c = 0
                    c += 1
                    op.cnt = c
                    op.epoch = ep
            nepoch[e] = ep + 1
        with contextlib.ExitStack() as es:
            csem = {}
            for e in ENGS:
                for ep in range(nepoch[e]):
                    csem[(e, ep)] = es.enter_context(nc.semaphore(f"c_{e}_{ep}"))
            dsem = {}
            for e in ENGS:
                if self.dma_hist[e]:
                    for i in range(NDMASEM):
                        dsem[(e, i)] = es.enter_context(nc.semaphore(f"d_{e}_{i}"))
            block = es.enter_context(nc.Block())
            getter = {'pe': block.tensor, 'act': block.scalar, 'dve': block.vector,
                      'pool': block.gpsimd, 'sp': block.sync}
            finals = list(final_waits)

            def make(e):
                def body(engobj):
                    waited = {}
                    for op in self.ops[e]:
                        for p in op.deps:
                            if p.is_dma:
                                key = ('d', p.eng, p.dsem)
                                if waited.get(key, 0) >= p.dval:
                                    continue
                                waited[key] = p.dval
                                engobj.wait_ge(dsem[(p.eng, p.dsem)], p.dval)
                            else:
                                if p.eng == 'pe' and e == 'pe':
                                    continue
                                key = ('c', p.eng)
                                val = (p.epoch, p.cnt)
                                if waited.get(key, (-1, 0)) >= val:
                                    continue
                                waited[key] = val
                                engobj.wait_ge(csem[(p.eng, p.epoch)], p.cnt)
                        ins = op.fn(engobj)
                        if op.is_dma:
                            ins.then_inc(dsem[(e, op.dsem)], 16)
                        elif op.signal:
                            ins.then_inc(csem[(e, op.epoch)], 1)
                    if e == 'sp':
                        for p in finals:
                            engobj.wait_ge(dsem[(p.eng, p.dsem)], p.dval)
                return body

            for e in ENGS:
                if self.ops[e] or e == 'sp':
                    getter[e](make(e))


class Arena:
    def __init__(self, tensor, nwords):
        self.t = tensor
        self.n = nwords
        self.top = 0
        self.peak = 0

    def alloc(self, free_shape, dtype):
        n = int(np.prod(free_shape))
        words = n if dtype == F32 else (n + 1) // 2
        words = (words + 7) // 8 * 8
        off = self.top
        self.top += words
        self.peak = max(self.peak, self.top)
        assert self.top <= self.n, f"arena overflow {self.top} > {self.n}"
        ap = self.t[:, off:off + words]
        if dtype != F32:
            ap = ap.bitcast(dtype)
        ap = ap[:, 0:n]
        if len(free_shape) == 2:
            ap = ap.rearrange("p (a b) -> p a b", a=free_shape[0])
        elif len(free_shape) == 3:
            ap = ap.rearrange("p (a b c) -> p a b c", a=free_shape[0], b=free_shape[1])
        elif len(free_shape) == 4:
            ap = ap.rearrange("p (a b c d) -> p a b c d", a=free_shape[0], b=free_shape[1], c=free_shape[2])
        return ap

    def mark(self):
        return self.top

    def release(self, m):
        self.top = m


def _smalls_layout(nseq):
    items = [('c', 8 * nseq), ('bada', 4 * 48), ('gmix', 32), ('gffn', 32), ('gfin', 8),
             ('kvg', 4), ('ikg', 2), ('ikb', 2), ('bq', 16), ('bk2', 4), ('bout', 16),
             ('sinks', 32), ('rb31', 16), ('bv2', 512)]
    off = {}
    o = 0
    for k, n in items:
        off[k] = (o, n)
        o += n
    return off, o


ARENA_WORDS = 53000


def build(layers, nseq, final_norm):
    DBG = os.environ.get('KDBG', '').split(',')
    nc = bass.Bass("TRN2", target_bir_lowering=False)
    SM, NS = _smalls_layout(nseq)
    dtn = nc.dram_tensor
    xin = dtn("xin", [nseq, D, SEQ], F32, kind="ExternalInput").ap()
    xout = dtn("xout", [nseq, D, SEQ], F32, kind="ExternalOutput").ap()
    smalls_d = dtn("smalls", [128, NS], F32, kind="ExternalInput").ap()
    consts_d = dtn("consts", [128, 5 * 128], F32, kind="ExternalInput").ap()
    tb_d = dtn("tbias", [2, 128, NH * 128], F32, kind="ExternalInput").ap()
    w_ada = dtn("w_ada", [DEPTH, D, 6 * D], F32, kind="ExternalInput").ap()
    a_w_in = dtn("a_w_in", [2, D, A_IN], F32, kind="ExternalInput").ap()
    a_w_ukT = dtn("a_w_ukT", [2, 128, 8 * 256], F32, kind="ExternalInput").ap()
    a_w_uv = dtn("a_w_uv", [2, NH, 256, 64], F32, kind="ExternalInput").ap()
    a_w_out = dtn("a_w_out", [2, D, D], F32, kind="ExternalInput").ap()
    b_w_in = dtn("b_w_in", [2, D, B_IN], F32, kind="ExternalInput").ap()
    b_w_out = dtn("b_w_out", [2, D, D], F32, kind="ExternalInput").ap()
    ffn_w1 = dtn("ffn_w1", [DEPTH, D, DFF], F32, kind="ExternalInput").ap()
    ffn_w3 = dtn("ffn_w3", [DEPTH, D, DFF], F32, kind="ExternalInput").ap()
    ffn_w2 = dtn("ffn_w2", [DEPTH, DFF, D], F32, kind="ExternalInput").ap()
    maskD = dtn("maskD", [NTILE, 128, NTILE * 128], BF16).ap()

    es = contextlib.ExitStack()
    arena_t = es.enter_context(nc.sbuf_tensor("arena", [128, ARENA_WORDS], F32))
    PSB = [es.enter_context(nc.psum_tensor(f"ps{i}", [128, 512], F32)) for i in range(8)]
    S = Sched(nc)
    AR = Arena(arena_t, ARENA_WORDS)

    def ps(i):
        return PSB[i][:, :]

    def psk(i):
        return ('ps', i)

    def MM(out, lhsT, rhs, start, stop, rd, wr):
        S.add('pe', lambda e: e.matmul(out, lhsT=lhsT, rhs=rhs, start=start, stop=stop), reads=rd, writes=wr)

    def ACT(out, in_, func, rd, wr, bias=None, scale=None):
        kw = {}
        if bias is not None:
            kw['bias'] = bias
        if scale is not None:
            kw['scale'] = scale
        S.add('act', lambda e: e.activation(out=out, in_=in_, func=func, **kw), reads=rd, writes=wr)

    def TS(out, in0, s1, s2, op0, op1, rd, wr, eng='dve'):
        if op1 is None:
            S.add(eng, lambda e: e.tensor_scalar(out=out, in0=in0, scalar1=s1, scalar2=None, op0=op0), reads=rd, writes=wr)
        else:
            S.add(eng, lambda e: e.tensor_scalar(out=out, in0=in0, scalar1=s1, scalar2=s2, op0=op0, op1=op1), reads=rd, writes=wr)

    def TT(out, in0, in1, op, rd, wr, eng='dve'):
        S.add(eng, lambda e: e.tensor_tensor(out=out, in0=in0, in1=in1, op=op), reads=rd, writes=wr)

    def STT(out, in0, scalar, in1, op0, op1, rd, wr):
        S.add('dve', lambda e: e.scalar_tensor_tensor(out=out, in0=in0, scalar=scalar, in1=in1, op0=op0, op1=op1), reads=rd, writes=wr)

    def CP(out, in_, rd, wr, eng='dve'):
        if eng == 'act':
            S.add(eng, lambda e: e.activation(out=out, in_=in_, func=AF.Identity), reads=rd, writes=wr)
        else:
            S.add(eng, lambda e: e.tensor_copy(out=out, in_=in_), reads=rd, writes=wr)

    def RECIP(out, in_, rd, wr):
        S.add('dve', lambda e: e.reciprocal(out=out, in_=in_), reads=rd, writes=wr)

    def MEMSET(ap, val, wr, eng='dve'):
        S.add(eng, lambda e: e.memset(ap, val), writes=wr)

    def DMA(q, out, in_, rd, wr):
        return S.add(q, lambda e: e.dma_start(out=out, in_=in_), reads=rd, writes=wr, dma=True)

    xT = AR.alloc((8, SEQ), F32)
    hT = AR.alloc((8, SEQ), BF16)
    smalls = AR.alloc((NS,), F32)
    cst = AR.alloc((5, 128), F32)
    ident_bf = AR.alloc((128,), BF16)
    ones_bf = AR.alloc((128,), BF16)
    ones_f = AR.alloc((128,), F32)
    Tcur = AR.alloc((NH, 128), F32)
    Tprev = AR.alloc((NH, 128), F32)
    modT = AR.alloc((DEPTH, 48, nseq), F32)
    aT = AR.alloc((DEPTH, 2, 8, nseq), F32)
    gbo = AR.alloc((DEPTH, 8, nseq), F32)
    esink = AR.alloc((2, NH), F32)
    cs_bf = AR.alloc((8, nseq), BF16)
    cols = AR.alloc((8,), F32)
    m8 = AR.alloc((8,), F32)
    PBASE = AR.mark()

    def sm(name):
        o, n = SM[name]
        return smalls[:, o:o + n]

    DMA('sp', smalls, smalls_d, [], ['smalls'])
    DMA('sp', cst, consts_d.rearrange("p (a b) -> p a b", a=5), [], ['cst'])
    DMA('sp', Tcur, tb_d[0].rearrange("p (a b) -> p a b", a=NH), [], ['Tcur'])
    DMA('sp', Tprev, tb_d[1].rearrange("p (a b) -> p a b", a=NH), [], ['Tprev'])
    MEMSET(ones_bf, 1.0, ['ones_bf'])
    MEMSET(ones_f, 1.0, ['ones_f'])
    MEMSET(cols[:, 0:1], 1e-6, ['cols'])
    CP(ident_bf, cst[:, 0, :], ['cst'], ['ident_bf'])
    ident_f = cst[:, 0, :]
    onesblk = cst[:, 1, :]
    causal_add = cst[:, 2, :]
    prev_add = cst[:, 3, :]
    negtri = cst[:, 4, :]
    eps_col = cols[:, 0:1]
    rb31 = sm('rb31')
    rb31_b = rb31.unsqueeze(2).to_broadcast([128, NH, 128])
    TT(Tcur, Tcur, rb31_b, ALU.subtract, ['Tcur', 'smalls'], ['Tcur'])
    TT(Tprev, Tprev, rb31_b, ALU.subtract, ['Tprev', 'smalls'], ['Tprev'])
    TT(Tcur, Tcur, causal_add.unsqueeze(1).to_broadcast([128, NH, 128]), ALU.add, ['Tcur', 'cst'], ['Tcur'])
    sinks = sm('sinks').rearrange("p (a b) -> p a b", a=2)
    TT(esink, sinks, rb31.unsqueeze(1).to_broadcast([128, 2, NH]), ALU.subtract, ['smalls'], ['esink'])
    ACT(esink, esink, AF.Exp, ['esink'], ['esink'])
    cview = sm('c').rearrange("p (a b) -> p a b", a=8)
    ACT(cs_bf, cview, AF.Silu, ['smalls'], ['cs_bf'])

    class Slots:
        def __init__(self, n, tag):
            self.bufs = [AR.alloc((8 * 512,), BF16) for _ in range(n)]
            self.n = n
            self.i = 0
            self.tag = tag

        def next(self):
            k = self.i % self.n
            self.i += 1
            return self.bufs[k], (self.tag, k)

    def load_w(slots, src, kc, ncol):
        buf, key = slots.next()
        v = buf[:, 0:kc * ncol].rearrange("p (a b) -> p a b", a=kc)
        DMA('pool', v, src, [], [key])
        return v, key

    def wrows(w2d):
        return w2d.rearrange("(c p) n -> p c n", p=128)

    S.barrier()
    m0 = AR.mark()
    sl = Slots(2, 'wsl')
    bada = sm('bada').rearrange("p (a b) -> p a b", a=4)
    gmix = sm('gmix').rearrange("p (a b) -> p a b", a=4)
    gffn = sm('gffn').rearrange("p (a b) -> p a b", a=4)
    bout = sm('bout').rearrange("p (a b) -> p a b", a=2)
    for l in layers:
        wv = wrows(w_ada[l])
        pm = PSB[0][:, 0:48 * nseq]
        for blk in range(12):
            v, key = load_w(sl, wv[:, :, blk * 512:(blk + 1) * 512], 8, 512)
            for fcl in range(4):
                ch = blk * 4 + fcl
                for kc in range(8):
                    MM(pm[:, ch * nseq:(ch + 1) * nseq], v[:, kc, fcl * 128:(fcl + 1) * 128], cs_bf[:, kc, :],
                       kc == 0, kc == 7, [key, 'cs_bf'], [psk(0)])
        TT(modT[:, l], pm.rearrange("p (a b) -> p a b", a=48), bada[:, l, :].unsqueeze(2).to_broadcast([128, 48, nseq]),
           ALU.add, [psk(0), 'smalls'], ['modT'])
        for which, (g, sc0) in enumerate(((gmix, 8), (gffn, 32))):
            TS(aT[:, l, which], modT[:, l, sc0:sc0 + 8, :], 1.0, None, ALU.add, None, ['modT'], ['aT'])
            TT(aT[:, l, which], aT[:, l, which], g[:, l, :].unsqueeze(2).to_broadcast([128, 8, nseq]), ALU.mult,
               ['aT', 'smalls'], ['aT'])
        if l % 2 == 1:
            TT(gbo[:, l], modT[:, l, 16:24, :], bout[:, l // 2, :].unsqueeze(2).to_broadcast([128, 8, nseq]), ALU.mult,
               ['modT', 'smalls'], ['gbo'])
    AR.release(m0)

    def norm_mod(a_of_c, b_of_c, out_fn):
        m = AR.mark()
        sq = [AR.alloc((512,), F32) for _ in range(2)]
        tmp = [AR.alloc((512,), F32) for _ in range(2)]
        sd = AR.alloc((512,), F32)
        rstd = AR.alloc((512,), F32)
        for tg in range(4):
            tsl = slice(tg * 512, (tg + 1) * 512)
            for c in range(8):
                ACT(sq[c % 2], xT[:, c, tsl], AF.Square, [('x', c, tg)], [('sq', c % 2)])
                MM(ps(0), ones_f, sq[c % 2], c == 0, c == 7, [('sq', c % 2), 'ones_f'], [psk(0)])
            ACT(sd, ps(0), AF.Sqrt, [psk(0), 'cols'], ['sd'], bias=eps_col, scale=1.0 / D)
            RECIP(rstd, sd, ['sd'], ['rstd'])
            for c in range(8):
                TT(tmp[c % 2], xT[:, c, tsl], rstd, ALU.mult, [('x', c, tg), 'rstd'], [('ntmp', c % 2)])
                out_fn(c, tg, tmp[c % 2], ('ntmp', c % 2))
        AR.release(m)

    def norm_to_h(l, which, s):
        sh0 = 0 if which == 0 else 24

        def out_fn(c, tg, t_ap, t_key):
            ACT(hT[:, c, tg * 512:(tg + 1) * 512], t_ap, AF.Identity, [t_key, 'aT', 'modT'], [('h', c, tg)],
                bias=modT[:, l, sh0 + c, s:s + 1], scale=aT[:, l, which, c, s:s + 1])
        norm_mod(None, None, out_fn)

    def ffn(l, s):
        S.barrier()
        m = AR.mark()
        uT = AR.alloc((NFC, 1024), BF16)
        sl = Slots(4, 'wsl')
        sg = [AR.alloc((512,), F32) for _ in range(2)]
        w1v = wrows(ffn_w1[l])
        w3v = wrows(ffn_w3[l])
        w2v = ffn_w2[l].rearrange("(f p) n -> p f n", p=128)
        cnt = 0
        for th in range(2):
            for fb in range(6):
                nfc = 4 if fb < 5 else 2
                ncol = nfc * 128
                va, ka = load_w(sl, w1v[:, :, fb * 512:fb * 512 + ncol], 8, ncol)
                vb, kb = load_w(sl, w3v[:, :, fb * 512:fb * 512 + ncol], 8, ncol)
                for fcl in range(nfc):
                    fc = fb * 4 + fcl
                    for t2 in range(2):
                        tg = th * 2 + t2
                        b1 = (cnt % 2) * 2
                        b3 = b1 + 1
                        for kc in range(8):
                            MM(ps(b1), va[:, kc, fcl * 128:(fcl + 1) * 128], hT[:, kc, tg * 512:(tg + 1) * 512],
                               kc == 0, kc == 7, [ka, ('h', kc, tg)], [psk(b1)])
                        for kc in range(8):
                            MM(ps(b3), vb[:, kc, fcl * 128:(fcl + 1) * 128], hT[:, kc, tg * 512:(tg + 1) * 512],
                               kc == 0, kc == 7, [kb, ('h', kc, tg)], [psk(b3)])
                        ACT(sg[cnt % 2], ps(b1), AF.Silu, [psk(b1)], [('sg', cnt % 2)])
                        TT(uT[:, fc, t2 * 512:(t2 + 1) * 512], sg[cnt % 2], ps(b3), ALU.mult,
                           [('sg', cnt % 2), psk(b3)], [('u', fc, t2)])
                        cnt += 1
            for t2 in range(2):
                tg = th * 2 + t2
                for fcb in range(6):
                    nfc = 4 if fcb < 5 else 2
                    buf, key = sl.next()
                    v = buf[:, 0:nfc * 1024].rearrange("p (a b) -> p a b", a=nfc)
                    DMA('pool', v, w2v[:, fcb * 4:fcb * 4 + nfc, :], [], [key])
                    for fcl in range(nfc):
                        fc = fcb * 4 + fcl
                        for fo in range(8):
                            MM(ps(fo), v[:, fcl, fo * 128:(fo + 1) * 128], uT[:, fc, t2 * 512:(t2 + 1) * 512],
                               fc == 0, fc == NFC - 1, [key, ('u', fc, t2)], [psk(fo)])
                for fo in range(8):
                    xs = xT[:, fo, tg * 512:(tg + 1) * 512]
                    STT(xs, ps(fo), modT[:, l, 40 + fo, s:s + 1], xs, ALU.mult, ALU.add,
                        [psk(fo), 'modT', ('x', fo, tg)], [('x', fo, tg)])
        AR.release(m)

    def out_proj(w_out2d, l, s, has_bias):
        S.barrier()
        m = AR.mark()
        sl = Slots(2, 'wsl')
        wv = wrows(w_out2d)
        for half in range(2):
            v, key = load_w(sl, wv[:, :, half * 512:(half + 1) * 512], 8, 512)
            for fl in range(4):
                fo = half * 4 + fl
                for tg in range(4):
                    b = (fl * 4 + tg) % 4
                    for kc in range(8):
                        MM(ps(b), v[:, kc, fl * 128:(fl + 1) * 128], hT[:, kc, tg * 512:(tg + 1) * 512],
                           kc == 0, kc == 7, [key, ('h', kc, tg)], [psk(b)])
                    xs = xT[:, fo, tg * 512:(tg + 1) * 512]
                    STT(xs, ps(b), modT[:, l, 16 + fo, s:s + 1], xs, ALU.mult, ALU.add,
                        [psk(b), 'modT', ('x', fo, tg)], [('x', fo, tg)])
                    if has_bias:
                        TS(xs, xs, gbo[:, l, fo, s:s + 1], None, ALU.add, None, [('x', fo, tg), 'gbo'], [('x', fo, tg)])
        AR.release(m)

    def mixer_b(l, s):
        j = l // 2
        S.barrier()
        m = AR.mark()
        qT = AR.alloc((8, SEQ), BF16)
        kT2 = AR.alloc((2, SEQ), BF16)
        v2 = AR.alloc((NTILE, 256), BF16)
        bq = sm('bq').rearrange("p (a b) -> p a b", a=2)
        bk2 = sm('bk2').rearrange("p (a b) -> p a b", a=2)
        bv2 = sm('bv2').rearrange("p (a b) -> p a b", a=2)
        m1 = AR.mark()
        sl = Slots(2, 'wsl')
        wv = wrows(b_w_in[j])
        for half in range(2):
            v, key = load_w(sl, wv[:, :, half * 512:(half + 1) * 512], 8, 512)
            for fl in range(4):
                c = half * 4 + fl
                for tg in range(4):
                    b = (fl * 4 + tg) % 4
                    for kc in range(8):
                        MM(ps(b), v[:, kc, fl * 128:(fl + 1) * 128], hT[:, kc, tg * 512:(tg + 1) * 512],
                           kc == 0, kc == 7, [key, ('h', kc, tg)], [psk(b)])
                    ACT(qT[:, c, tg * 512:(tg + 1) * 512], ps(b), AF.Identity, [psk(b), 'smalls'], [('q', c, tg)],
                        bias=bq[:, j, c:c + 1])
        buf, key = sl.next()
        v = buf[:, :].rearrange("p (a b) -> p a b", a=8)
        for kvh in range(2):
            for dup in range(2):
                DMA('pool', v[:, :, kvh * 128 + dup * 64:kvh * 128 + dup * 64 + 64],
                    wv[:, :, 1024 + kvh * 64:1024 + kvh * 64 + 64], [], [key])
                DMA('pool', v[:, :, 256 + kvh * 128 + dup * 64:256 + kvh * 128 + dup * 64 + 64],
                    wv[:, :, 1152 + kvh * 64:1152 + kvh * 64 + 64], [], [key])
        for kvh in range(2):
            for tg in range(4):
                b = tg % 4
                for kc in range(8):
                    MM(ps(b), v[:, kc, kvh * 128:(kvh + 1) * 128], hT[:, kc, tg * 512:(tg + 1) * 512],
                       kc == 0, kc == 7, [key, ('h', kc, tg)], [psk(b)])
                ACT(kT2[:, kvh, tg * 512:(tg + 1) * 512], ps(b), AF.Identity, [psk(b), 'smalls'], [('k2', kvh, tg)],
                    bias=bk2[:, j, kvh:kvh + 1])
        for st in range(NTILE):
            b = st % 4
            for kc in range(8):
                MM(PSB[b][:, 0:256], hT[:, kc, st * 128:(st + 1) * 128], v[:, kc, 256:512],
                   kc == 0, kc == 7, [key, ('h', kc, st // 4)], [psk(b)])
            TT(v2[:, st, :], PSB[b][:, 0:256], bv2[:, j, :], ALU.add, [psk(b), 'smalls'], [('v2', st)])
        S.barrier()
        AR.release(m1)
        tmpb = [AR.alloc((4, 128), F32) for _ in range(2)]
        pb = [AR.alloc((512,), BF16) for _ in range(4)]
        lnd = AR.alloc((512,), F32)
        rec = AR.alloc((4, 128), F32)
        it = 0
        pi = 0
        for n in range(NTILE):
            tsl = slice(n * 128, (n + 1) * 128)
            for kvh in range(2):
                for par in range(2):
                    psl = slice(par * 64, (par + 1) * 64)
                    kts = ([n - 1] if n > 0 else []) + [n]
                    plist = []
                    for kt in kts:
                        bl = it % 2
                        MM(ps(bl), kT2[psl, kvh, kt * 128:(kt + 1) * 128], qT[psl, 4 * kvh:4 * kvh + 4, tsl],
                           True, True, [('k2', kvh, kt // 4), ('q', 4 * kvh, n // 4), ('q', 4 * kvh + 1, n // 4),
                                        ('q', 4 * kvh + 2, n // 4), ('q', 4 * kvh + 3, n // 4)], [psk(bl)])
                        tb = tmpb[it % 2]
                        tkey = ('tmpb', it % 2)
                        T = Tcur if kt == n else Tprev
                        hs = 8 * kvh + par
                        Tsel = T[:, hs:hs + 7:2, :]
                        STT(tb, PSB[bl][:, :].rearrange("p (a b) -> p a b", a=4), 0.125, Tsel, ALU.mult, ALU.add,
                            [psk(bl), 'Tcur', 'Tprev'], [tkey])
                        if kt != n:
                            TT(tb, tb, prev_add.unsqueeze(1).to_broadcast([128, 4, 128]), ALU.add, [tkey, 'cst'], [tkey])
                        p_ap = pb[pi % 4]
                        pkey = ('pb', pi % 4)
                        pi += 1
                        ACT(p_ap, tb.rearrange("p a b -> p (a b)"), AF.Exp, [tkey], [pkey])
                        plist.append((p_ap, pkey, kt))
                        it += 1
                    for ii, (p_ap, pkey, kt) in enumerate(plist):
                        MM(ps(2), ones_bf, p_ap, ii == 0, ii == len(plist) - 1, [pkey, 'ones_bf'], [psk(2)])
                    for ii, (p_ap, pkey, kt) in enumerate(plist):
                        MM(ps(3), v2[:, kt, kvh * 128:(kvh + 1) * 128], p_ap, ii == 0, ii == len(plist) - 1,
                           [pkey, ('v2', kt)], [psk(3)])
                    hs = 8 * kvh + par
                    es_b = esink[:, j, hs:hs + 7:2].unsqueeze(2).to_broadcast([128, 4, 128])
                    TT(rec, PSB[2][:, :].rearrange("p (a b) -> p a b", a=4), es_b, ALU.add, [psk(2), 'esink'], ['rec'])
                    ACT(lnd, rec.rearrange("p a b -> p (a b)"), AF.Ln, ['rec'], ['lnd'])
                    ACT(rec.rearrange("p a b -> p (a b)"), lnd, AF.Exp, ['lnd'], ['rec'], scale=-1.0)
                    TT(hT[psl, 4 * kvh:4 * kvh + 4, tsl], PSB[3][psl, :].rearrange("p (a b) -> p a b", a=4), rec[psl],
                       ALU.mult, [psk(3), 'rec'], [('h', 4 * kvh + cc, n // 4) for cc in range(4)])
        AR.release(m)
        out_proj(b_w_out[j], l, s, True)

    def mixer_a(l, s):
        j = l // 2
        S.barrier()
        m = AR.mark()
        wv = wrows(a_w_in[j])
        kvg = sm('kvg').rearrange("p (a b) -> p a b", a=2)
        ikg = sm('ikg')
        ikb = sm('ikb')
        qiT = AR.alloc((4, SEQ), BF16)
        kiT = AR.alloc((SEQ,), BF16)
        wi = AR.alloc((NTILE, 8), F32)
        m1 = AR.mark()
        sl = Slots(2, 'wsl')
        v, key = load_w(sl, wv[:, :, 1280:1792], 8, 512)
        for c in range(4 if 'noqi' not in DBG else 0):
            for tg in range(4):
                b = tg % 4
                for kc in range(8):
                    MM(ps(b), v[:, kc, c * 128:(c + 1) * 128], hT[:, kc, tg * 512:(tg + 1) * 512],
                       kc == 0, kc == 7, [key, ('h', kc, tg)], [psk(b)])
                CP(qiT[:, c, tg * 512:(tg + 1) * 512], ps(b), [psk(b)], [('qi', c, tg)], eng='act' if tg % 2 else 'dve')
        buf, key = sl.next()
        v = buf[:, 0:8 * 256].rearrange("p (a b) -> p a b", a=8)
        DMA('pool', v[:, :, 0:64], wv[:, :, 1792:1856], [], [key])
        DMA('pool', v[:, :, 64:128], wv[:, :, 1792:1856], [], [key])
        DMA('pool', v[:, :, 128:192], wv[:, :, 1800:1864], [], [key])
        kraw = AR.alloc((512,), F32)
        kcen = AR.alloc((512,), F32)
        ksq = AR.alloc((512,), F32)
        ksd = AR.alloc((512,), F32)
        krs = AR.alloc((512,), F32)
        for tg in range(4 if 'noki' not in DBG else 0):
            tsl = slice(tg * 512, (tg + 1) * 512)
            for kc in range(8):
                MM(ps(0), v[:, kc, 0:128], hT[:, kc, tsl], kc == 0, kc == 7, [key, ('h', kc, tg)], [psk(0)])
            CP(kraw, ps(0), [psk(0)], ['kraw'])
            MM(ps(1), onesblk, kraw, True, True, ['kraw', 'cst'], [psk(1)])
            TT(kcen, kraw, ps(1), ALU.subtract, ['kraw', psk(1)], ['kcen'])
            ACT(ksq, kcen, AF.Square, ['kcen'], ['ksq'])
            MM(ps(1), onesblk, ksq, True, True, ['ksq', 'cst'], [psk(1)])
            ACT(ksd, ps(1), AF.Sqrt, [psk(1), 'cols'], ['ksd'], bias=eps_col)
            RECIP(krs, ksd, ['ksd'], ['krs'])
            TT(kcen, kcen, krs, ALU.mult, ['kcen', 'krs'], ['kcen'])
            ACT(kiT[:, tsl], kcen, AF.Identity, ['kcen', 'smalls'], [('ki', tg)],
                bias=ikb[:, j:j + 1], scale=ikg[:, j:j + 1])
        for st in range(NTILE if 'nowi' not in DBG else 0):
            b = 2 + st % 2
            for kc in range(8):
                MM(PSB[b][:, 0:8], hT[:, kc, st * 128:(st + 1) * 128], v[:, kc, 184:192],
                   kc == 0, kc == 7, [key, ('h', kc, st // 4)], [psk(b)])
            TS(wi[:, st, :], PSB[b][:, 0:8], 8 ** -0.5 * 64 ** -0.5, None, ALU.mult, None, [psk(b)], [('wi', st)])
        S.barrier()
        AR.release(m1)
        score = AR.alloc((SEQ,), F32)
        mask01 = AR.alloc((SEQ,), BF16)
        rl = [AR.alloc((512,), F32) for _ in range(2)]
        mst = [AR.alloc((NTILE, 128), BF16) for _ in range(2)]
        ri = 0
        tgi = 0
        for i in range(2, NTILE if 'nop1' not in DBG else 0):
            n = 128 * (i + 1)
            nkb = (n + 511) // 512
            for h in range(8):
                psl = slice((h % 2) * 64, (h % 2) * 64 + 64)
                for kb in range(nkb):
                    w = min(512, n - kb * 512)
                    bl = ri % 2
                    MM(PSB[bl][:, 0:w], qiT[psl, h // 2, i * 128:(i + 1) * 128], kiT[psl, kb * 512:kb * 512 + w],
                       True, True, [('qi', h // 2, i // 4), ('ki', kb)], [psk(bl)])
                    r_ap = rl[ri % 2][:, 0:w]
                    rkey = ('rl', ri % 2)
                    ACT(r_ap, PSB[bl][:, 0:w], AF.Relu, [psk(bl)], [rkey])
                    sc_ap = score[:, kb * 512:kb * 512 + w]
                    if h == 0:
                        TS(sc_ap, r_ap, wi[:, i, 0:1], None, ALU.mult, None, [rkey, ('wi', i)], [('score', kb)])
                    else:
                        STT(sc_ap, r_ap, wi[:, i, h:h + 1], sc_ap, ALU.mult, ALU.add,
                            [rkey, ('wi', i), ('score', kb)], [('score', kb)])
                    ri += 1
            allk = [('score', kb) for kb in range(nkb)]
            dsl = slice(i * 128, (i + 1) * 128)
            TT(score[:, dsl], score[:, dsl], negtri, ALU.add, allk + ['cst'], allk)
            for it in range(32 if 'notopk' not in DBG else 0):
                S.add('dve', lambda e, n=n: e.max(out=m8, in_=score[:, 0:n]), reads=allk, writes=['m8'])
                S.add('dve', lambda e, n=n: e.match_replace(out=score[:, 0:n], in_to_replace=m8, in_values=score[:, 0:n],
                                                            imm_value=REPL), reads=allk + ['m8'], writes=allk)
            TS(mask01[:, 0:n], score[:, 0:n], -2.0e38, None, ALU.is_le, None, allk, ['mask01'])
            ms = mst[i % 2]
            mkey = ('mst', i % 2)
            for g in range((i + 1 + 7) // 8):
                j0 = 8 * g
                ng = min(8, i + 1 - j0)
                bank = 6 + (tgi % 2)
                tgi += 1
                pbb = PSB[bank][:, :].bitcast(BF16)
                for jl in range(ng):
                    jj = j0 + jl
                    S.add('pe', lambda e, jj=jj, jl=jl, pbb=pbb: e.transpose(pbb[:, jl * 128:(jl + 1) * 128], mask01[:, jj * 128:(jj + 1) * 128], ident_bf),
                          reads=['mask01', 'ident_bf'], writes=[psk(bank)])
                CP(ms[:, j0:j0 + ng, :].rearrange("p a b -> p (a b)"), pbb[:, 0:ng * 128], [psk(bank)], [mkey], eng='act')
            DMA('sp', maskD[i][:, 0:n], ms[:, 0:i + 1, :].rearrange("p a b -> p (a b)"), [mkey], [('maskD', i)])
        S.barrier()
        AR.release(m)

        m = AR.mark()
        qT = AR.alloc((8, SEQ), BF16)
        ckvT = AR.alloc((2, SEQ), BF16)
        ckv = AR.alloc((NTILE, 256), BF16)
        m1 = AR.mark()
        sl = Slots(2, 'wsl')
        for half in range(2 if 'noq' not in DBG else 0):
            v, key = load_w(sl, wv[:, :, half * 512:(half + 1) * 512], 8, 512)
            for fl in range(4):
                c = half * 4 + fl
                for tg in range(4):
                    b = (fl * 4 + tg) % 4
                    for kc in range(8):
                        MM(ps(b), v[:, kc, fl * 128:(fl + 1) * 128], hT[:, kc, tg * 512:(tg + 1) * 512],
                           kc == 0, kc == 7, [key, ('h', kc, tg)], [psk(b)])
                    CP(qT[:, c, tg * 512:(tg + 1) * 512], ps(b), [psk(b)], [('q', c, tg)], eng='act' if tg % 2 else 'dve')
        v, key = load_w(sl, wv[:, :, 1024:1280], 8, 256)
        craw = AR.alloc((2, 512), F32)
        csq = AR.alloc((512,), F32)
        csd = AR.alloc((512,), F32)
        crs = AR.alloc((512,), F32)
        for tg in range(4 if 'nockv' not in DBG else 0):
            tsl = slice(tg * 512, (tg + 1) * 512)
            for rc in range(2):
                for kc in range(8):
                    MM(ps(rc), v[:, kc, rc * 128:(rc + 1) * 128], hT[:, kc, tsl], kc == 0, kc == 7,
                       [key, ('h', kc, tg)], [psk(rc)])
                CP(craw[:, rc, :], ps(rc), [psk(rc)], [('craw', rc)])
                ACT(csq, craw[:, rc, :], AF.Square, [('craw', rc)], ['csq'])
                MM(ps(2), ones_f, csq, rc == 0, rc == 1, ['csq', 'ones_f'], [psk(2)])
            ACT(csd, ps(2), AF.Sqrt, [psk(2), 'cols'], ['csd'], bias=eps_col, scale=1.0 / 256)
            RECIP(crs, csd, ['csd'], ['crs'])
            for rc in range(2):
                STT(ckvT[:, rc, tsl], craw[:, rc, :], kvg[:, j, rc:rc + 1], crs, ALU.mult, ALU.mult,
                    [('craw', rc), 'crs', 'smalls'], [('ckvT', rc, tg)])
        for sg4 in range(NTILE // 4 if 'notr' not in DBG else 0):
            bank = 6 + (sg4 % 2)
            pbb = PSB[bank][:, :].bitcast(BF16)
            for sl4 in range(4):
                st = sg4 * 4 + sl4
                for rc in range(2):
                    k = sl4 * 2 + rc
                    S.add('pe', lambda e, st=st, rc=rc, k=k, pbb=pbb: e.transpose(pbb[:, k * 128:(k + 1) * 128], ckvT[:, rc, st * 128:(st + 1) * 128], ident_bf),
                          reads=[('ckvT', rc, st // 4), 'ident_bf'], writes=[psk(bank)])
            CP(ckv[:, sg4 * 4:sg4 * 4 + 4, :].rearrange("p a b -> p (a b)"), pbb[:, :], [psk(bank)],
               [('ckv', sg4 * 4 + q) for q in range(4)], eng='act' if sg4 % 2 else 'dve')
        S.barrier()
        AR.release(m1)
        wuk = AR.alloc((8, 256), BF16)
        wuv = AR.alloc((2, NH, 128), BF16)
        if 'nouk' not in DBG:
            DMA('pool', wuk, a_w_ukT[j].rearrange("p (a b) -> p a b", a=8), [], ['wuk'])
        if 'nomemset' not in DBG:
            MEMSET(wuv, 0.0, ['wuv'])
        uvv = a_w_uv[j].rearrange("h (rc p) d -> p rc h d", p=128)
        for rc in range(2 if 'nouv' not in DBG else 0):
            for par in range(2):
                S.add('pool', lambda e, rc=rc, par=par: e.dma_start(out=wuv[:, rc, par:NH:2, par * 64:par * 64 + 64],
                                                                    in_=uvv[:, rc, par:NH:2, :]),
                      reads=[], writes=['wuv'], dma=True)
        qa = [AR.alloc((2, 512), BF16) for _ in range(2)]
        mT = [AR.alloc((NTILE, 128), BF16) for _ in range(1)]
        pb = [AR.alloc((4, 128), BF16) for _ in range(4)]
        olat = [AR.alloc((2, 512), BF16) for _ in range(1)]
        rec = AR.alloc((512,), F32)
        lnd = AR.alloc((512,), F32)
        tmpb = [AR.alloc((4, 128), F32) for _ in range(2)]
        pi = 0
        ti = 0
        gi = 0
        for i in range(NTILE if 'nop2' not in DBG else 0):
            tsl = slice(i * 128, (i + 1) * 128)
            mt = mT[0]
            mtk = ('mT', 0)
            if i >= 2 and 'nop1' not in DBG:
                DMA('sp', mt[:, 0:i + 1, :].rearrange("p a b -> p (a b)"), maskD[i][:, 0:128 * (i + 1)], [('maskD', i)], [mtk])
            for hg in range(4):
                qa_ap = qa[gi % 2]
                qak = ('qa', gi % 2)
                ol = olat[0]
                olk = ('olat', 0)
                gi += 1
                qa4 = qa_ap.rearrange("p r (a b) -> p r a b", a=4)
                for rc in range(2):
                    for hl in range(4):
                        h = 4 * hg + hl
                        psl = slice((h % 2) * 64, (h % 2) * 64 + 64)
                        bq_ = 5 + (hl % 2)
                        MM(PSB[bq_][:, (hl // 2) * 128:(hl // 2) * 128 + 128], wuk[psl, h // 2, rc * 128:(rc + 1) * 128],
                           qT[psl, h // 2, tsl], True, True, ['wuk', ('q', h // 2, i // 4)], [psk(bq_)])
                    CP(qa4[:, rc, 0:4:2, :], PSB[5][:, 0:256].rearrange("p (a b) -> p a b", a=2), [psk(5)], [qak], eng='dve')
                    CP(qa4[:, rc, 1:4:2, :], PSB[6][:, 0:256].rearrange("p (a b) -> p a b", a=2), [psk(6)], [qak], eng='act')
                for jj in range(i + 1):
                    bl = pi % 2
                    for rc in range(2):
                        MM(ps(bl), ckvT[:, rc, jj * 128:(jj + 1) * 128], qa_ap[:, rc, :], rc == 0, rc == 1,
                           [('ckvT', rc, jj // 4), qak], [psk(bl)])
                    p_ap = pb[pi % 4]
                    pkey = ('pb', pi % 4)
                    pi += 1
                    if jj >= i - 1:
                        T = Tcur if jj == i else Tprev
                        tb = tmpb[ti % 2]
                        tkey = ('tmpb', ti % 2)
                        ti += 1
                        STT(tb, PSB[bl][:, :].rearrange("p (a b) -> p a b", a=4), 0.125, T[:, 4 * hg:4 * hg + 4, :],
                            ALU.mult, ALU.add, [psk(bl), 'Tcur', 'Tprev'], [tkey])
                        ACT(p_ap, tb, AF.Exp, [tkey], [pkey])
                    else:
                        ACT(p_ap, PSB[bl][:, :].rearrange("p (a b) -> p a b", a=4), AF.Exp, [psk(bl)], [pkey], scale=0.125)
                    if i >= 2 and 'nop1' not in DBG:
                        TT(p_ap, p_ap, mt[:, jj, :].unsqueeze(1).to_broadcast([128, 4, 128]), ALU.mult,
                           [pkey, mtk], [pkey])
                    p2 = p_ap.rearrange("p a b -> p (a b)")
                    MM(ps(2), ones_bf, p2, jj == 0, jj == i, [pkey, 'ones_bf'], [psk(2)])
                    for rc in range(2):
                        MM(ps(3 + rc), ckv[:, jj, rc * 128:(rc + 1) * 128], p2, jj == 0, jj == i,
                           [pkey, ('ckv', jj)], [psk(3 + rc)])
                ACT(lnd, ps(2), AF.Ln, [psk(2)], ['lnd'])
                ACT(rec, lnd, AF.Exp, ['lnd'], ['rec'], scale=-1.0)
                for rc in range(2):
                    CP(ol[:, rc, :], ps(3 + rc), [psk(3 + rc)], [olk], eng='act' if rc else 'dve')
                for pr in range(2):
                    c = 2 * hg + pr
                    bo = 5 + pr
                    k = 0
                    for par in range(2):
                        hl = 2 * pr + par
                        h = 4 * hg + hl
                        for rc in range(2):
                            MM(PSB[bo][:, 0:128], wuv[:, rc, h, :], ol[:, rc, hl * 128:(hl + 1) * 128], k == 0, k == 3,
                               ['wuv', olk], [psk(bo)])
                            k += 1
                    for par in range(2):
                        hl = 2 * pr + par
                        psl = slice(par * 64, par * 64 + 64)
                        TT(hT[psl, c, tsl], PSB[bo][psl, 0:128], rec[psl, hl * 128:(hl + 1) * 128], ALU.mult,
                           [psk(bo), 'rec'], [('h', c, i // 4)])
        AR.release(m)
        out_proj(a_w_out[j], l, s, False)

    finals = []
    for s in range(nseq):
        S.barrier()
        for c in range(8):
            DMA('sp', xT[:, c, :], xin[s, c * 128:(c + 1) * 128, :], [], [('x', c, tg) for tg in range(4)])
        for l in layers:
            S.barrier()
            norm_to_h(l, 0, s)
            if 'noA' in DBG:
                pass
            elif l % 2 == 0:
                mixer_a(l, s)
            else:
                mixer_b(l, s)
            S.barrier()
            norm_to_h(l, 1, s)
            ffn(l, s)
        S.barrier()
        if final_norm:
            m = AR.mark()
            ost = [AR.alloc((512,), F32) for _ in range(2)]
            gfin = sm('gfin')
            cnt = [0]

            def out_fn(c, tg, t_ap, t_key):
                k = cnt[0] % 2
                cnt[0] += 1
                TS(ost[k], t_ap, gfin[:, c:c + 1], None, ALU.mult, None, [t_key, 'smalls'], [('ost', k)], eng='dve')
                finals.append(DMA('sp', xout[s, c * 128:(c + 1) * 128, tg * 512:(tg + 1) * 512], ost[k], [('ost', k)], []))
            norm_mod(None, None, out_fn)
            AR.release(m)
        else:
            for c in range(8):
                finals.append(DMA('sp', xout[s, c * 128:(c + 1) * 128, :], xT[:, c, :], [('x', c, tg) for tg in range(4)], []))
    S.finalize(final_waits=finals)
    es.close()
    return nc


def _t5_bucket_np(dist):
    d = np.maximum(dist, 0)
    large = 16 + (np.log(np.maximum(d, 1).astype(np.float32) / 16) / math.log(128 / 16) * 16).astype(np.int32)
    large = np.minimum(large, 31)
    return np.where(d < 16, d, large)


def _consts():
    c = np.zeros((128, 5, 128), np.float32)
    idx = np.arange(128)
    c[:, 0, :] = np.eye(128, dtype=np.float32)
    blk = (idx[:, None] // 64) == (idx[None, :] // 64)
    c[:, 1, :] = blk.astype(np.float32) / 64.0
    c[:, 2, :] = np.where(idx[:, None] <= idx[None, :], 0.0, -30000.0)
    c[:, 3, :] = np.where(idx[:, None] > idx[None, :], 0.0, -30000.0)
    c[:, 4, :] = np.where(idx[None, :] <= idx[:, None], 0.0, NEG_BIG)
    return c.reshape(128, 5 * 128)


def _tbias(rel_bias):
    s = np.arange(128)[:, None]
    t = np.arange(128)[None, :]
    out = np.zeros((2, 128, NH, 128), np.float32)
    for k, off in enumerate((0, 128)):
        b = _t5_bucket_np(t - s + off)
        g = rel_bias[b]
        out[k] = np.transpose(g, (0, 2, 1))
    return out.reshape(2, 128, NH * 128)


def _fm(v):
    v = np.asarray(v, np.float32)
    lead = v.shape[:-1]
    r = v.reshape(lead + (v.shape[-1] // 128, 128))
    return np.moveaxis(r, -1, 0)


def _smalls(inp, core, nseq):
    SM, NS = _smalls_layout(nseq)
    sm = np.zeros((128, NS), np.float32)

    def put(name, arr):
        o, n = SM[name]
        sm[:, o:o + n] = np.asarray(arr, np.float32).reshape(128, n)
    c = inp['c'][core * nseq:(core + 1) * nseq]
    put('c', np.transpose(_fm(c), (0, 2, 1)))
    put('bada', _fm(inp['b_ada']))
    put('gmix', _fm(inp['norm_mix_g']))
    put('gffn', _fm(inp['norm_ffn_g']))
    put('gfin', _fm(inp['norm_final_g']))
    put('kvg', _fm(inp['a_kv_norm_g']))
    put('ikg', np.concatenate([inp['a_idx_k_g'].T, inp['a_idx_k_g'].T], 0))
    put('ikb', np.concatenate([inp['a_idx_k_b'].T, inp['a_idx_k_b'].T], 0))
    bin_ = inp['b_b_in']
    put('bq', _fm(bin_[:, 0:1024]))
    bk = bin_[:, 1024:1152].reshape(2, 2, 64)
    put('bk2', np.concatenate([np.transpose(bk, (2, 0, 1))] * 2, 0))
    put('bout', _fm(inp['b_b_out']))
    put('sinks', np.broadcast_to(inp['b_sinks'].reshape(1, 32), (128, 32)))
    put('rb31', np.broadcast_to(inp['rel_bias'][31].reshape(1, 16), (128, 16)))
    bv = bin_[:, 1152:1280].reshape(2, 2, 1, 64)
    bv2 = np.broadcast_to(bv, (2, 2, 2, 64)).reshape(1, 512)
    put('bv2', np.broadcast_to(bv2, (128, 512)))
    return sm


_NC_CACHE = {}


def _get_nc(layers, nseq, final_norm):
    key = (tuple(layers), nseq, final_norm)
    if key not in _NC_CACHE:
        _NC_CACHE[key] = build(list(layers), nseq, final_norm)
    return _NC_CACHE[key]


FUSED = True


def kernel(**inp):
    inp = {k: np.asarray(v) for k, v in inp.items()}
    nseq = 2
    x = inp['x']
    xT = np.ascontiguousarray(np.transpose(x, (0, 2, 1)))
    consts = _consts()
    tb = _tbias(inp['rel_bias'])
    ukT = np.ascontiguousarray(
        np.transpose(inp['a_w_uk'].reshape(2, 8, 2, 256, 64), (0, 2, 4, 1, 3))).reshape(2, 128, 8 * 256)
    shared = {
        'consts': consts, 'tbias': tb, 'w_ada': inp['w_ada'], 'a_w_in': inp['a_w_in'], 'a_w_ukT': ukT,
        'a_w_uv': inp['a_w_uv'], 'a_w_out': inp['a_w_out'], 'b_w_in': inp['b_w_in'], 'b_w_out': inp['b_w_out'],
        'ffn_w1': inp['ffn_w1'], 'ffn_w3': inp['ffn_w3'], 'ffn_w2': inp['ffn_w2'],
    }
    smalls = [_smalls(inp, c, nseq) for c in range(NCORES)]
    cur = [xT[c * nseq:(c + 1) * nseq] for c in range(NCORES)]
    plan = [([0, 1, 2, 3], True)] if FUSED else [([0], False), ([1], False), ([2], False), ([3], True)]
    for layers, fin in plan:
        nc = _get_nc(layers, nseq, fin)
        in_maps = [dict(shared, xin=np.ascontiguousarray(cur[c]), smalls=smalls[c]) for c in range(NCORES)]
        res = run_bass_kernel_spmd(nc, in_maps, core_ids=list(range(NCORES)))
        cur = [res.results[c]['xout'] for c in range(NCORES)]
    out = np.concatenate(cur, 0)
    return np.ascontiguousarray(np.transpose(out, (0, 2, 1))).astype(np.float32)
```

```python
import contextlib
import math
import os

import numpy as np
import concourse.bass as bass
import concourse.mybir as mybir
from concourse.bass_utils import run_bass_kernel_spmd

F32 = mybir.dt.float32
BF16 = mybir.dt.bfloat16
AF = mybir.ActivationFunctionType
ALU = mybir.AluOpType

D = 1024
SEQ = 2048
DEPTH = 4
NH = 16
DFF = 2816
NFC = DFF // 128
NTILE = SEQ // 128
NCORES = 8
A_IN = 1864
B_IN = 1280
NEG_BIG = -1.0e30
REPL = -3.0e38

ENGS = ('pe', 'act', 'dve', 'pool', 'sp')
NDMASEM = 12
EPOCH = 3000


class Op:
    __slots__ = ('eng', 'fn', 'deps', 'signal', 'is_dma', 'dsem', 'dval', 'cnt', 'epoch')

    def __init__(self, eng, fn, is_dma):
        self.eng = eng
        self.fn = fn
        self.deps = ()
        self.signal = False
        self.is_dma = is_dma
        self.dsem = None
        self.dval = 0
        self.cnt = 0
        self.epoch = 0


class Sched:
    def __init__(self, nc):
        self.nc = nc
        self.ops = {e: [] for e in ENGS}
        self.res = {}
        self.dma_hist = {e: [] for e in ENGS}
        self.pending_barrier = {e: None for e in ENGS}
        self.dma_since_barrier = []

    def barrier(self):
        deps = set(self.dma_since_barrier)
        for e in ENGS:
            if self.ops[e]:
                last = self.ops[e][-1]
                deps.add(last)
        self.dma_since_barrier = []
        for e in ENGS:
            old = self.pending_barrier[e]
            self.pending_barrier[e] = set(deps) | (old or set())

    def add(self, eng, fn, reads=(), writes=(), dma=False):
        op = Op(eng, fn, dma)
        deps = set()
        res = self.res
        for k in reads:
            st = res.get(k)
            if st is not None and st[0] is not None:
                deps.add(st[0])
        for k in writes:
            st = res.get(k)
            if st is not None:
                if st[0] is not None:
                    deps.add(st[0])
                deps.update(st[1].values())
                deps.update(st[2])
        for k in reads:
            st = res.get(k)
            if st is None:
                st = res[k] = [None, {}, []]
            if dma:
                st[2].append(op)
            else:
                st[1][eng] = op
        for k in writes:
            res[k] = [op, {}, []]
        pb = self.pending_barrier[eng]
        if pb is not None:
            deps.update(pb)
            self.pending_barrier[eng] = None
        if dma:
            h = self.dma_hist[eng]
            k = len(h)
            op.dsem = k % NDMASEM
            op.dval = 16 * (k // NDMASEM + 1)
            if k >= NDMASEM:
                deps.add(h[k - NDMASEM])
            h.append(op)
            self.dma_since_barrier.append(op)
        deps.discard(op)
        op.deps = deps
        for p in deps:
            if not p.is_dma:
                if not (p.eng == 'pe' and eng == 'pe'):
                    p.signal = True
        self.ops[eng].append(op)
        return op

    def finalize(self, final_waits=()):
        nc = self.nc
        nepoch = {}
        for e in ENGS:
            c = 0
            ep = 0
            for op in self.ops[e]:
                if op.signal and not op.is_dma:
                    if c >= EPOCH:
                        ep += 1
                        c = 0
                    c += 1
                    op.cnt = c
                    op.epoch = ep
            nepoch[e] = ep + 1
        with contextlib.ExitStack() as es:
            csem = {}
            for e in ENGS:
                for ep in range(nepoch[e]):
                    csem[(e, ep)] = es.enter_context(nc.semaphore(f"c_{e}_{ep}"))
            dsem = {}
            for e in ENGS:
                if self.dma_hist[e]:
                    for i in range(NDMASEM):
                        dsem[(e, i)] = es.enter_context(nc.semaphore(f"d_{e}_{i}"))
            block = es.enter_context(nc.Block())
            getter = {'pe': block.tensor, 'act': block.scalar, 'dve': block.vector,
                      'pool': block.gpsimd, 'sp': block.sync}
            finals = list(final_waits)

            def make(e):
                def body(engobj):
                    waited = {}
                    for op in self.ops[e]:
                        for p in op.deps:
                            if p.is_dma:
                                key = ('d', p.eng, p.dsem)
                                if waited.get(key, 0) >= p.dval:
                                    continue
                                waited[key] = p.dval
                                engobj.wait_ge(dsem[(p.eng, p.dsem)], p.dval)
                            else:
                                if p.eng == 'pe' and e == 'pe':
                                    continue
                                key = ('c', p.eng)
                                val = (p.epoch, p.cnt)
                                if waited.get(key, (-1, 0)) >= val:
                                    continue
                                waited[key] = val
                                engobj.wait_ge(csem[(p.eng, p.epoch)], p.cnt)
                        ins = op.fn(engobj)
                        if op.is_dma:
                            ins.then_inc(dsem[(e, op.dsem)], 16)
                        elif op.signal:
                            ins.then_inc(csem[(e, op.epoch)], 1)
                    if e == 'sp':
                        for p in finals:
                            engobj.wait_ge(dsem[(p.eng, p.dsem)], p.dval)
                return body

            for e in ENGS:
                if self.ops[e] or e == 'sp':
                    getter[e](make(e))


class Arena:
    def __init__(self, tensor, nwords):
        self.t = tensor
        self.n = nwords
        self.top = 0
        self.peak = 0

    def alloc(self, free_shape, dtype):
        n = int(np.prod(free_shape))
        words = n if dtype == F32 else (n + 1) // 2
        words = (words + 7) // 8 * 8
        off = self.top
        self.top += words
        self.peak = max(self.peak, self.top)
        assert self.top <= self.n, f"arena overflow {self.top} > {self.n}"
        ap = self.t[:, off:off + words]
        if dtype != F32:
            ap = ap.bitcast(dtype)
        ap = ap[:, 0:n]
        if len(free_shape) == 2:
            ap = ap.rearrange("p (a b) -> p a b", a=free_shape[0])
        elif len(free_shape) == 3:
            ap = ap.rearrange("p (a b c) -> p a b c", a=free_shape[0], b=free_shape[1])
        elif len(free_shape) == 4:
            ap = ap.rearrange("p (a b c d) -> p a b c d", a=free_shape[0], b=free_shape[1], c=free_shape[2])
        return ap

    def mark(self):
        return self.top

    def release(self, m):
        self.top = m


def _smalls_layout(nseq):
    items = [('c', 8 * nseq), ('bada', 4 * 48), ('gmix', 32), ('gffn', 32), ('gfin', 8),
             ('kvg', 4), ('ikg', 2), ('ikb', 2), ('bq', 16), ('bk2', 4), ('bout', 16),
             ('sinks', 32), ('rb31', 16), ('bv2', 512)]
    off = {}
    o = 0
    for k, n in items:
        off[k] = (o, n)
        o += n
    return off, o


ARENA_WORDS = 53000


def build(layers, nseq, final_norm):
    DBG = os.environ.get('KDBG', '').split(',')
    nc = bass.Bass("TRN2", target_bir_lowering=False)
    SM, NS = _smalls_layout(nseq)
    dtn = nc.dram_tensor
    xin = dtn("xin", [nseq, D, SEQ], F32, kind="ExternalInput").ap()
    xout = dtn("xout", [nseq, D, SEQ], F32, kind="ExternalOutput").ap()
    smalls_d = dtn("smalls", [128, NS], F32, kind="ExternalInput").ap()
    consts_d = dtn("consts", [128, 5 * 128], F32, kind="ExternalInput").ap()
    tb_d = dtn("tbias", [2, 128, NH * 128], F32, kind="ExternalInput").ap()
    w_ada = dtn("w_ada", [DEPTH, D, 6 * D], F32, kind="ExternalInput").ap()
    a_w_in = dtn("a_w_in", [2, D, A_IN], F32, kind="ExternalInput").ap()
    a_w_ukT = dtn("a_w_ukT", [2, 128, 8 * 256], F32, kind="ExternalInput").ap()
    a_w_uv = dtn("a_w_uv", [2, NH, 256, 64], F32, kind="ExternalInput").ap()
    a_w_out = dtn("a_w_out", [2, D, D], F32, kind="ExternalInput").ap()
    b_w_in = dtn("b_w_in", [2, D, B_IN], F32, kind="ExternalInput").ap()
    b_w_out = dtn("b_w_out", [2, D, D], F32, kind="ExternalInput").ap()
    ffn_w1 = dtn("ffn_w1", [DEPTH, D, DFF], F32, kind="ExternalInput").ap()
    ffn_w3 = dtn("ffn_w3", [DEPTH, D, DFF], F32, kind="ExternalInput").ap()
    ffn_w2 = dtn("ffn_w2", [DEPTH, DFF, D], F32, kind="ExternalInput").ap()
    maskD = dtn("maskD", [NTILE, 128, NTILE * 128], BF16).ap()

    es = contextlib.ExitStack()
    arena_t = es.enter_context(nc.sbuf_tensor("arena", [128, ARENA_WORDS], F32))
    PSB = [es.enter_context(nc.psum_tensor(f"ps{i}", [128, 512], F32)) for i in range(8)]
    S = Sched(nc)
    AR = Arena(arena_t, ARENA_WORDS)

    def ps(i):
        return PSB[i][:, :]

    def psk(i):
        return ('ps', i)

    def MM(out, lhsT, rhs, start, stop, rd, wr):
        S.add('pe', lambda e: e.matmul(out, lhsT=lhsT, rhs=rhs, start=start, stop=stop), reads=rd, writes=wr)

    def ACT(out, in_, func, rd, wr, bias=None, scale=None):
        kw = {}
        if bias is not None:
            kw['bias'] = bias
        if scale is not None:
            kw['scale'] = scale
        S.add('act', lambda e: e.activation(out=out, in_=in_, func=func, **kw), reads=rd, writes=wr)

    def TS(out, in0, s1, s2, op0, op1, rd, wr, eng='dve'):
        if op1 is None:
            S.add(eng, lambda e: e.tensor_scalar(out=out, in0=in0, scalar1=s1, scalar2=None, op0=op0), reads=rd, writes=wr)
        else:
            S.add(eng, lambda e: e.tensor_scalar(out=out, in0=in0, scalar1=s1, scalar2=s2, op0=op0, op1=op1), reads=rd, writes=wr)

    def TT(out, in0, in1, op, rd, wr, eng='dve'):
        S.add(eng, lambda e: e.tensor_tensor(out=out, in0=in0, in1=in1, op=op), reads=rd, writes=wr)

    def STT(out, in0, scalar, in1, op0, op1, rd, wr):
        S.add('dve', lambda e: e.scalar_tensor_tensor(out=out, in0=in0, scalar=scalar, in1=in1, op0=op0, op1=op1), reads=rd, writes=wr)

    def CP(out, in_, rd, wr, eng='dve'):
        if eng == 'act':
            S.add(eng, lambda e: e.activation(out=out, in_=in_, func=AF.Identity), reads=rd, writes=wr)
        else:
            S.add(eng, lambda e: e.tensor_copy(out=out, in_=in_), reads=rd, writes=wr)

    def RECIP(out, in_, rd, wr):
        S.add('dve', lambda e: e.reciprocal(out=out, in_=in_), reads=rd, writes=wr)

    def MEMSET(ap, val, wr, eng='dve'):
        S.add(eng, lambda e: e.memset(ap, val), writes=wr)

    def DMA(q, out, in_, rd, wr):
        return S.add(q, lambda e: e.dma_start(out=out, in_=in_), reads=rd, writes=wr, dma=True)

    xT = AR.alloc((8, SEQ), F32)
    hT = AR.alloc((8, SEQ), BF16)
    smalls = AR.alloc((NS,), F32)
    cst = AR.alloc((5, 128), F32)
    ident_bf = AR.alloc((128,), BF16)
    ones_bf = AR.alloc((128,), BF16)
    ones_f = AR.alloc((128,), F32)
    Tcur = AR.alloc((NH, 128), F32)
    Tprev = AR.alloc((NH, 128), F32)
    modT = AR.alloc((DEPTH, 48, nseq), F32)
    aT = AR.alloc((DEPTH, 2, 8, nseq), F32)
    gbo = AR.alloc((DEPTH, 8, nseq), F32)
    esink = AR.alloc((2, NH), F32)
    cs_bf = AR.alloc((8, nseq), BF16)
    cols = AR.alloc((8,), F32)
    m8 = AR.alloc((8,), F32)
    PBASE = AR.mark()

    def sm(name):
        o, n = SM[name]
        return smalls[:, o:o + n]

    DMA('sp', smalls, smalls_d, [], ['smalls'])
    DMA('sp', cst, consts_d.rearrange("p (a b) -> p a b", a=5), [], ['cst'])
    DMA('sp', Tcur, tb_d[0].rearrange("p (a b) -> p a b", a=NH), [], ['Tcur'])
    DMA('sp', Tprev, tb_d[1].rearrange("p (a b) -> p a b", a=NH), [], ['Tprev'])
    MEMSET(ones_bf, 1.0, ['ones_bf'])
    MEMSET(ones_f, 1.0, ['ones_f'])
    MEMSET(cols[:, 0:1], 1e-6, ['cols'])
    CP(ident_bf, cst[:, 0, :], ['cst'], ['ident_bf'])
    ident_f = cst[:, 0, :]
    onesblk = cst[:, 1, :]
    causal_add = cst[:, 2, :]
    prev_add = cst[:, 3, :]
    negtri = cst[:, 4, :]
    eps_col = cols[:, 0:1]
    rb31 = sm('rb31')
    rb31_b = rb31.unsqueeze(2).to_broadcast([128, NH, 128])
    TT(Tcur, Tcur, rb31_b, ALU.subtract, ['Tcur', 'smalls'], ['Tcur'])
    TT(Tprev, Tprev, rb31_b, ALU.subtract, ['Tprev', 'smalls'], ['Tprev'])
    TT(Tcur, Tcur, causal_add.unsqueeze(1).to_broadcast([128, NH, 128]), ALU.add, ['Tcur', 'cst'], ['Tcur'])
    sinks = sm('sinks').rearrange("p (a b) -> p a b", a=2)
    TT(esink, sinks, rb31.unsqueeze(1).to_broadcast([128, 2, NH]), ALU.subtract, ['smalls'], ['esink'])
    ACT(esink, esink, AF.Exp, ['esink'], ['esink'])
    cview = sm('c').rearrange("p (a b) -> p a b", a=8)
    ACT(cs_bf, cview, AF.Silu, ['smalls'], ['cs_bf'])

    class Slots:
        def __init__(self, n, tag):
            self.bufs = [AR.alloc((8 * 512,), BF16) for _ in range(n)]
            self.n = n
            self.i = 0
            self.tag = tag

        def next(self):
            k = self.i % self.n
            self.i += 1
            return self.bufs[k], (self.tag, k)

    def load_w(slots, src, kc, ncol):
        buf, key = slots.next()
        v = buf[:, 0:kc * ncol].rearrange("p (a b) -> p a b", a=kc)
        DMA('pool', v, src, [], [key])
        return v, key

    def wrows(w2d):
        return w2d.rearrange("(c p) n -> p c n", p=128)

    S.barrier()
    m0 = AR.mark()
    sl = Slots(2, 'wsl')
    bada = sm('bada').rearrange("p (a b) -> p a b", a=4)
    gmix = sm('gmix').rearrange("p (a b) -> p a b", a=4)
    gffn = sm('gffn').rearrange("p (a b) -> p a b", a=4)
    bout = sm('bout').rearrange("p (a b) -> p a b", a=2)
    for l in layers:
        wv = wrows(w_ada[l])
        pm = PSB[0][:, 0:48 * nseq]
        for blk in range(12):
            v, key = load_w(sl, wv[:, :, blk * 512:(blk + 1) * 512], 8, 512)
            for fcl in range(4):
                ch = blk * 4 + fcl
                for kc in range(8):
                    MM(pm[:, ch * nseq:(ch + 1) * nseq], v[:, kc, fcl * 128:(fcl + 1) * 128], cs_bf[:, kc, :],
                       kc == 0, kc == 7, [key, 'cs_bf'], [psk(0)])
        TT(modT[:, l], pm.rearrange("p (a b) -> p a b", a=48), bada[:, l, :].unsqueeze(2).to_broadcast([128, 48, nseq]),
           ALU.add, [psk(0), 'smalls'], ['modT'])
        for which, (g, sc0) in enumerate(((gmix, 8), (gffn, 32))):
            TS(aT[:, l, which], modT[:, l, sc0:sc0 + 8, :], 1.0, None, ALU.add, None, ['modT'], ['aT'])
            TT(aT[:, l, which], aT[:, l, which], g[:, l, :].unsqueeze(2).to_broadcast([128, 8, nseq]), ALU.mult,
               ['aT', 'smalls'], ['aT'])
        if l % 2 == 1:
            TT(gbo[:, l], modT[:, l, 16:24, :], bout[:, l // 2, :].unsqueeze(2).to_broadcast([128, 8, nseq]), ALU.mult,
               ['modT', 'smalls'], ['gbo'])
    AR.release(m0)

    def norm_mod(a_of_c, b_of_c, out_fn):
        m = AR.mark()
        sq = [AR.alloc((512,), F32) for _ in range(2)]
        tmp = [AR.alloc((512,), F32) for _ in range(2)]
        sd = AR.alloc((512,), F32)
        rstd = AR.alloc((512,), F32)
        for tg in range(4):
            tsl = slice(tg * 512, (tg + 1) * 512)
            for c in range(8):
                ACT(sq[c % 2], xT[:, c, tsl], AF.Square, [('x', c, tg)], [('sq', c % 2)])
                MM(ps(0), ones_f, sq[c % 2], c == 0, c == 7, [('sq', c % 2), 'ones_f'], [psk(0)])
            ACT(sd, ps(0), AF.Sqrt, [psk(0), 'cols'], ['sd'], bias=eps_col, scale=1.0 / D)
            RECIP(rstd, sd, ['sd'], ['rstd'])
            for c in range(8):
                TT(tmp[c % 2], xT[:, c, tsl], rstd, ALU.mult, [('x', c, tg), 'rstd'], [('ntmp', c % 2)])
                out_fn(c, tg, tmp[c % 2], ('ntmp', c % 2))
        AR.release(m)

    def norm_to_h(l, which, s):
        sh0 = 0 if which == 0 else 24

        def out_fn(c, tg, t_ap, t_key):
            ACT(hT[:, c, tg * 512:(tg + 1) * 512], t_ap, AF.Identity, [t_key, 'aT', 'modT'], [('h', c, tg)],
                bias=modT[:, l, sh0 + c, s:s + 1], scale=aT[:, l, which, c, s:s + 1])
        norm_mod(None, None, out_fn)

    def ffn(l, s):
        S.barrier()
        m = AR.mark()
        uT = AR.alloc((NFC, 1024), BF16)
        sl = Slots(4, 'wsl')
        sg = [AR.alloc((512,), F32) for _ in range(2)]
        w1v = wrows(ffn_w1[l])
        w3v = wrows(ffn_w3[l])
        w2v = ffn_w2[l].rearrange("(f p) n -> p f n", p=128)
        cnt = 0
        for th in range(2):
            for fb in range(6):
                nfc = 4 if fb < 5 else 2
                ncol = nfc * 128
                va, ka = load_w(sl, w1v[:, :, fb * 512:fb * 512 + ncol], 8, ncol)
                vb, kb = load_w(sl, w3v[:, :, fb * 512:fb * 512 + ncol], 8, ncol)
                for fcl in range(nfc):
                    fc = fb * 4 + fcl
                    for t2 in range(2):
                        tg = th * 2 + t2
                        b1 = (cnt % 2) * 2
                        b3 = b1 + 1
                        for kc in range(8):
                            MM(ps(b1), va[:, kc, fcl * 128:(fcl + 1) * 128], hT[:, kc, tg * 512:(tg + 1) * 512],
                               kc == 0, kc == 7, [ka, ('h', kc, tg)], [psk(b1)])
                        for kc in range(8):
                            MM(ps(b3), vb[:, kc, fcl * 128:(fcl + 1) * 128], hT[:, kc, tg * 512:(tg + 1) * 512],
                               kc == 0, kc == 7, [kb, ('h', kc, tg)], [psk(b3)])
                        ACT(sg[cnt % 2], ps(b1), AF.Silu, [psk(b1)], [('sg', cnt % 2)])
                        TT(uT[:, fc, t2 * 512:(t2 + 1) * 512], sg[cnt % 2], ps(b3), ALU.mult,
                           [('sg', cnt % 2), psk(b3)], [('u', fc, t2)])
                        cnt += 1
            for t2 in range(2):
                tg = th * 2 + t2
                for fcb in range(6):
                    nfc = 4 if fcb < 5 else 2
                    buf, key = sl.next()
                    v = buf[:, 0:nfc * 1024].rearrange("p (a b) -> p a b", a=nfc)
                    DMA('pool', v, w2v[:, fcb * 4:fcb * 4 + nfc, :], [], [key])
                    for fcl in range(nfc):
                        fc = fcb * 4 + fcl
                        for fo in range(8):
                            MM(ps(fo), v[:, fcl, fo * 128:(fo + 1) * 128], uT[:, fc, t2 * 512:(t2 + 1) * 512],
                               fc == 0, fc == NFC - 1, [key, ('u', fc, t2)], [psk(fo)])
                for fo in range(8):
                    xs = xT[:, fo, tg * 512:(tg + 1) * 512]
                    STT(xs, ps(fo), modT[:, l, 40 + fo, s:s + 1], xs, ALU.mult, ALU.add,
                        [psk(fo), 'modT', ('x', fo, tg)], [('x', fo, tg)])
        AR.release(m)

    def out_proj(w_out2d, l, s, has_bias):
        S.barrier()
        m = AR.mark()
        sl = Slots(2, 'wsl')
        wv = wrows(w_out2d)
        for half in range(2):
            v, key = load_w(sl, wv[:, :, half * 512:(half + 1) * 512], 8, 512)
            for fl in range(4):
                fo = half * 4 + fl
                for tg in range(4):
                    b = (fl * 4 + tg) % 4
                    for kc in range(8):
                        MM(ps(b), v[:, kc, fl * 128:(fl + 1) * 128], hT[:, kc, tg * 512:(tg + 1) * 512],
                           kc == 0, kc == 7, [key, ('h', kc, tg)], [psk(b)])
                    xs = xT[:, fo, tg * 512:(tg + 1) * 512]
                    STT(xs, ps(b), modT[:, l, 16 + fo, s:s + 1], xs, ALU.mult, ALU.add,
                        [psk(b), 'modT', ('x', fo, tg)], [('x', fo, tg)])
                    if has_bias:
                        TS(xs, xs, gbo[:, l, fo, s:s + 1], None, ALU.add, None, [('x', fo, tg), 'gbo'], [('x', fo, tg)])
        AR.release(m)

    def mixer_b(l, s):
        j = l // 2
        S.barrier()
        m = AR.mark()
        qT = AR.alloc((8, SEQ), BF16)
        kT2 = AR.alloc((2, SEQ), BF16)
        v2 = AR.alloc((NTILE, 256), BF16)
        bq = sm('bq').rearrange("p (a b) -> p a b", a=2)
        bk2 = sm('bk2').rearrange("p (a b) -> p a b", a=2)
        bv2 = sm('bv2').rearrange("p (a b) -> p a b", a=2)
        m1 = AR.mark()
        sl = Slots(2, 'wsl')
        wv = wrows(b_w_in[j])
        for half in range(2):
            v, key = load_w(sl, wv[:, :, half * 512:(half + 1) * 512], 8, 512)
            for fl in range(4):
                c = half * 4 + fl
                for tg in range(4):
                    b = (fl * 4 + tg) % 4
                    for kc in range(8):
                        MM(ps(b), v[:, kc, fl * 128:(fl + 1) * 128], hT[:, kc, tg * 512:(tg + 1) * 512],
                           kc == 0, kc == 7, [key, ('h', kc, tg)], [psk(b)])
                    ACT(qT[:, c, tg * 512:(tg + 1) * 512], ps(b), AF.Identity, [psk(b), 'smalls'], [('q', c, tg)],
                        bias=bq[:, j, c:c + 1])
        buf, key = sl.next()
        v = buf[:, :].rearrange("p (a b) -> p a b", a=8)
        for kvh in range(2):
            for dup in range(2):
                DMA('pool', v[:, :, kvh * 128 + dup * 64:kvh * 128 + dup * 64 + 64],
                    wv[:, :, 1024 + kvh * 64:1024 + kvh * 64 + 64], [], [key])
                DMA('pool', v[:, :, 256 + kvh * 128 + dup * 64:256 + kvh * 128 + dup * 64 + 64],
                    wv[:, :, 1152 + kvh * 64:1152 + kvh * 64 + 64], [], [key])
        for kvh in range(2):
            for tg in range(4):
                b = tg % 4
                for kc in range(8):
                    MM(ps(b), v[:, kc, kvh * 128:(kvh + 1) * 128], hT[:, kc, tg * 512:(tg + 1) * 512],
                       kc == 0, kc == 7, [key, ('h', kc, tg)], [psk(b)])
                ACT(kT2[:, kvh, tg * 512:(tg + 1) * 512], ps(b), AF.Identity, [psk(b), 'smalls'], [('k2', kvh, tg)],
                    bias=bk2[:, j, kvh:kvh + 1])
        for st in range(NTILE):
            b = st % 4
            for kc in range(8):
                MM(PSB[b][:, 0:256], hT[:, kc, st * 128:(st + 1) * 128], v[:, kc, 256:512],
                   kc == 0, kc == 7, [key, ('h', kc, st // 4)], [psk(b)])
            TT(v2[:, st, :], PSB[b][:, 0:256], bv2[:, j, :], ALU.add, [psk(b), 'smalls'], [('v2', st)])
        S.barrier()
        AR.release(m1)
        tmpb = [AR.alloc((4, 128), F32) for _ in range(2)]
        pb = [AR.alloc((512,), BF16) for _ in range(4)]
        lnd = AR.alloc((512,), F32)
        rec = AR.alloc((4, 128), F32)
        it = 0
        pi = 0
        for n in range(NTILE):
            tsl = slice(n * 128, (n + 1) * 128)
            for kvh in range(2):
                for par in range(2):
                    psl = slice(par * 64, (par + 1) * 64)
                    kts = ([n - 1] if n > 0 else []) + [n]
                    plist = []
                    for kt in kts:
                        bl = it % 2
                        MM(ps(bl), kT2[psl, kvh, kt * 128:(kt + 1) * 128], qT[psl, 4 * kvh:4 * kvh + 4, tsl],
                           True, True, [('k2', kvh, kt // 4), ('q', 4 * kvh, n // 4), ('q', 4 * kvh + 1, n // 4),
                                        ('q', 4 * kvh + 2, n // 4), ('q', 4 * kvh + 3, n // 4)], [psk(bl)])
                        tb = tmpb[it % 2]
                        tkey = ('tmpb', it % 2)
                        T = Tcur if kt == n else Tprev
                        hs = 8 * kvh + par
                        Tsel = T[:, hs:hs + 7:2, :]
                        STT(tb, PSB[bl][:, :].rearrange("p (a b) -> p a b", a=4), 0.125, Tsel, ALU.mult, ALU.add,
                            [psk(bl), 'Tcur', 'Tprev'], [tkey])
                        if kt != n:
                            TT(tb, tb, prev_add.unsqueeze(1).to_broadcast([128, 4, 128]), ALU.add, [tkey, 'cst'], [tkey])
                        p_ap = pb[pi % 4]
                        pkey = ('pb', pi % 4)
                        pi += 1
                        ACT(p_ap, tb.rearrange("p a b -> p (a b)"), AF.Exp, [tkey], [pkey])
                        plist.append((p_ap, pkey, kt))
                        it += 1
                    for ii, (p_ap, pkey, kt) in enumerate(plist):
                        MM(ps(2), ones_bf, p_ap, ii == 0, ii == len(plist) - 1, [pkey, 'ones_bf'], [psk(2)])
                    for ii, (p_ap, pkey, kt) in enumerate(plist):
                        MM(ps(3), v2[:, kt, kvh * 128:(kvh + 1) * 128], p_ap, ii == 0, ii == len(plist) - 1,
                           [pkey, ('v2', kt)], [psk(3)])
                    hs = 8 * kvh + par
                    es_b = esink[:, j, hs:hs + 7:2].unsqueeze(2).to_broadcast([128, 4, 128])
                    TT(rec, PSB[2][:, :].rearrange("p (a b) -> p a b", a=4), es_b, ALU.add, [psk(2), 'esink'], ['rec'])
                    ACT(lnd, rec.rearrange("p a b -> p (a b)"), AF.Ln, ['rec'], ['lnd'])
                    ACT(rec.rearrange("p a b -> p (a b)"), lnd, AF.Exp, ['lnd'], ['rec'], scale=-1.0)
                    TT(hT[psl, 4 * kvh:4 * kvh + 4, tsl], PSB[3][psl, :].rearrange("p (a b) -> p a b", a=4), rec[psl],
                       ALU.mult, [psk(3), 'rec'], [('h', 4 * kvh + cc, n // 4) for cc in range(4)])
        AR.release(m)
        out_proj(b_w_out[j], l, s, True)

    def mixer_a(l, s):
        j = l // 2
        S.barrier()
        m = AR.mark()
        wv = wrows(a_w_in[j])
        kvg = sm('kvg').rearrange("p (a b) -> p a b", a=2)
        ikg = sm('ikg')
        ikb = sm('ikb')
        qiT = AR.alloc((4, SEQ), BF16)
        kiT = AR.alloc((SEQ,), BF16)
        wi = AR.alloc((NTILE, 8), F32)
        m1 = AR.mark()
        sl = Slots(2, 'wsl')
        v, key = load_w(sl, wv[:, :, 1280:1792], 8, 512)
        for c in range(4 if 'noqi' not in DBG else 0):
            for tg in range(4):
                b = tg % 4
                for kc in range(8):
                    MM(ps(b), v[:, kc, c * 128:(c + 1) * 128], hT[:, kc, tg * 512:(tg + 1) * 512],
                       kc == 0, kc == 7, [key, ('h', kc, tg)], [psk(b)])
                CP(qiT[:, c, tg * 512:(tg + 1) * 512], ps(b), [psk(b)], [('qi', c, tg)], eng='act' if tg % 2 else 'dve')
        buf, key = sl.next()
        v = buf[:, 0:8 * 256].rearrange("p (a b) -> p a b", a=8)
        DMA('pool', v[:, :, 0:64], wv[:, :, 1792:1856], [], [key])
        DMA('pool', v[:, :, 64:128], wv[:, :, 1792:1856], [], [key])
        DMA('pool', v[:, :, 128:192], wv[:, :, 1800:1864], [], [key])
        kraw = AR.alloc((512,), F32)
        kcen = AR.alloc((512,), F32)
        ksq = AR.alloc((512,), F32)
        ksd = AR.alloc((512,), F32)
        krs = AR.alloc((512,), F32)
        for tg in range(4 if 'noki' not in DBG else 0):
            tsl = slice(tg * 512, (tg + 1) * 512)
            for kc in range(8):
                MM(ps(0), v[:, kc, 0:128], hT[:, kc, tsl], kc == 0, kc == 7, [key, ('h', kc, tg)], [psk(0)])
            CP(kraw, ps(0), [psk(0)], ['kraw'])
            MM(ps(1), onesblk, kraw, True, True, ['kraw', 'cst'], [psk(1)])
            TT(kcen, kraw, ps(1), ALU.subtract, ['kraw', psk(1)], ['kcen'])
            ACT(ksq, kcen, AF.Square, ['kcen'], ['ksq'])
            MM(ps(1), onesblk, ksq, True, True, ['ksq', 'cst'], [psk(1)])
            ACT(ksd, ps(1), AF.Sqrt, [psk(1), 'cols'], ['ksd'], bias=eps_col)
            RECIP(krs, ksd, ['ksd'], ['krs'])
            TT(kcen, kcen, krs, ALU.mult, ['kcen', 'krs'], ['kcen'])
            ACT(kiT[:, tsl], kcen, AF.Identity, ['kcen', 'smalls'], [('ki', tg)],
                bias=ikb[:, j:j + 1], scale=ikg[:, j:j + 1])
        for st in range(NTILE if 'nowi' not in DBG else 0):
            b = 2 + st % 2
            for kc in range(8):
                MM(PSB[b][:, 0:8], hT[:, kc, st * 128:(st + 1) * 128], v[:, kc, 184:192],
                   kc == 0, kc == 7, [key, ('h', kc, st // 4)], [psk(b)])
            TS(wi[:, st, :], PSB[b][:, 0:8], 8 ** -0.5 * 64 ** -0.5, None, ALU.mult, None, [psk(b)], [('wi', st)])
        S.barrier()
        AR.release(m1)
        score = AR.alloc((SEQ,), F32)
        mask01 = AR.alloc((SEQ,), BF16)
        rl = [AR.alloc((512,), F32) for _ in range(2)]
        mst = [AR.alloc((NTILE, 128), BF16) for _ in range(2)]
        ri = 0
        tgi = 0
        for i in range(2, NTILE if 'nop1' not in DBG else 0):
            n = 128 * (i + 1)
            nkb = (n + 511) // 512
            for h in range(8):
                psl = slice((h % 2) * 64, (h % 2) * 64 + 64)
                for kb in range(nkb):
                    w = min(512, n - kb * 512)
                    bl = ri % 2
                    MM(PSB[bl][:, 0:w], qiT[psl, h // 2, i * 128:(i + 1) * 128], kiT[psl, kb * 512:kb * 512 + w],
                       True, True, [('qi', h // 2, i // 4), ('ki', kb)], [psk(bl)])
                    r_ap = rl[ri % 2][:, 0:w]
                    rkey = ('rl', ri % 2)
                    ACT(r_ap, PSB[bl][:, 0:w], AF.Relu, [psk(bl)], [rkey])
                    sc_ap = score[:, kb * 512:kb * 512 + w]
                    if h == 0:
                        TS(sc_ap, r_ap, wi[:, i, 0:1], None, ALU.mult, None, [rkey, ('wi', i)], [('score', kb)])
                    else:
                        STT(sc_ap, r_ap, wi[:, i, h:h + 1], sc_ap, ALU.mult, ALU.add,
                            [rkey, ('wi', i), ('score', kb)], [('score', kb)])
                    ri += 1
            allk = [('score', kb) for kb in range(nkb)]
            dsl = slice(i * 128, (i + 1) * 128)
            TT(score[:, dsl], score[:, dsl], negtri, ALU.add, allk + ['cst'], allk)
            for it in range(32 if 'notopk' not in DBG else 0):
                S.add('dve', lambda e, n=n: e.max(out=m8, in_=score[:, 0:n]), reads=allk, writes=['m8'])
                S.add('dve', lambda e, n=n: e.match_replace(out=score[:, 0:n], in_to_replace=m8, in_values=score[:, 0:n],
                                                            imm_value=REPL), reads=allk + ['m8'], writes=allk)
            TS(mask01[:, 0:n], score[:, 0:n], -2.0e38, None, ALU.is_le, None, allk, ['mask01'])
            ms = mst[i % 2]
            mkey = ('mst', i % 2)
            for g in range((i + 1 + 7) // 8):
                j0 = 8 * g
                ng = min(8, i + 1 - j0)
                bank = 6 + (tgi % 2)
                tgi += 1
                pbb = PSB[bank][:, :].bitcast(BF16)
                for jl in range(ng):
                    jj = j0 + jl
                    S.add('pe', lambda e, jj=jj, jl=jl, pbb=pbb: e.transpose(pbb[:, jl * 128:(jl + 1) * 128], mask01[:, jj * 128:(jj + 1) * 128], ident_bf),
                          reads=['mask01', 'ident_bf'], writes=[psk(bank)])
                CP(ms[:, j0:j0 + ng, :].rearrange("p a b -> p (a b)"), pbb[:, 0:ng * 128], [psk(bank)], [mkey], eng='act')
            DMA('sp', maskD[i][:, 0:n], ms[:, 0:i + 1, :].rearrange("p a b -> p (a b)"), [mkey], [('maskD', i)])
        S.barrier()
        AR.release(m)

        m = AR.mark()
        qT = AR.alloc((8, SEQ), BF16)
        ckvT = AR.alloc((2, SEQ), BF16)
        ckv = AR.alloc((NTILE, 256), BF16)
        m1 = AR.mark()
        sl = Slots(2, 'wsl')
        for half in range(2 if 'noq' not in DBG else 0):
            v, key = load_w(sl, wv[:, :, half * 512:(half + 1) * 512], 8, 512)
            for fl in range(4):
                c = half * 4 + fl
                for tg in range(4):
                    b = (fl * 4 + tg) % 4
                    for kc in range(8):
                        MM(ps(b), v[:, kc, fl * 128:(fl + 1) * 128], hT[:, kc, tg * 512:(tg + 1) * 512],
                           kc == 0, kc == 7, [key, ('h', kc, tg)], [psk(b)])
                    CP(qT[:, c, tg * 512:(tg + 1) * 512], ps(b), [psk(b)], [('q', c, tg)], eng='act' if tg % 2 else 'dve')
        v, key = load_w(sl, wv[:, :, 1024:1280], 8, 256)
        craw = AR.alloc((2, 512), F32)
        csq = AR.alloc((512,), F32)
        csd = AR.alloc((512,), F32)
        crs = AR.alloc((512,), F32)
        for tg in range(4 if 'nockv' not in DBG else 0):
            tsl = slice(tg * 512, (tg + 1) * 512)
            for rc in range(2):
                for kc in range(8):
                    MM(ps(rc), v[:, kc, rc * 128:(rc + 1) * 128], hT[:, kc, tsl], kc == 0, kc == 7,
                       [key, ('h', kc, tg)], [psk(rc)])
                CP(craw[:, rc, :], ps(rc), [psk(rc)], [('craw', rc)])
                ACT(csq, craw[:, rc, :], AF.Square, [('craw', rc)], ['csq'])
                MM(ps(2), ones_f, csq, rc == 0, rc == 1, ['csq', 'ones_f'], [psk(2)])
            ACT(csd, ps(2), AF.Sqrt, [psk(2), 'cols'], ['csd'], bias=eps_col, scale=1.0 / 256)
            RECIP(crs, csd, ['csd'], ['crs'])
            for rc in range(2):
                STT(ckvT[:, rc, tsl], craw[:, rc, :], kvg[:, j, rc:rc + 1], crs, ALU.mult, ALU.mult,
                    [('craw', rc), 'crs', 'smalls'], [('ckvT', rc, tg)])
        for sg4 in range(NTILE // 4 if 'notr' not in DBG else 0):
            bank = 6 + (sg4 % 2)
            pbb = PSB[bank][:, :].bitcast(BF16)
            for sl4 in range(4):
                st = sg4 * 4 + sl4
                for rc in range(2):
                    k = sl4 * 2 + rc
                    S.add('pe', lambda e, st=st, rc=rc, k=k, pbb=pbb: e.transpose(pbb[:, k * 128:(k + 1) * 128], ckvT[:, rc, st * 128:(st + 1) * 128], ident_bf),
                          reads=[('ckvT', rc, st // 4), 'ident_bf'], writes=[psk(bank)])
            CP(ckv[:, sg4 * 4:sg4 * 4 + 4, :].rearrange("p a b -> p (a b)"), pbb[:, :], [psk(bank)],
               [('ckv', sg4 * 4 + q) for q in range(4)], eng='act' if sg4 % 2 else 'dve')
        S.barrier()
        AR.release(m1)
        wuk = AR.alloc((8, 256), BF16)
        wuv = AR.alloc((2, NH, 128), BF16)
        if 'nouk' not in DBG:
            DMA('pool', wuk, a_w_ukT[j].rearrange("p (a b) -> p a b", a=8), [], ['wuk'])
        if 'nomemset' not in DBG:
            MEMSET(wuv, 0.0, ['wuv'])
        uvv = a_w_uv[j].rearrange("h (rc p) d -> p rc h d", p=128)
        for rc in range(2 if 'nouv' not in DBG else 0):
            for par in range(2):
                S.add('pool', lambda e, rc=rc, par=par: e.dma_start(out=wuv[:, rc, par:NH:2, par * 64:par * 64 + 64],
                                                                    in_=uvv[:, rc, par:NH:2, :]),
                      reads=[], writes=['wuv'], dma=True)
        qa = [AR.alloc((2, 512), BF16) for _ in range(2)]
        mT = [AR.alloc((NTILE, 128), BF16) for _ in range(1)]
        pb = [AR.alloc((4, 128), BF16) for _ in range(4)]
        olat = [AR.alloc((2, 512), BF16) for _ in range(1)]
        rec = AR.alloc((512,), F32)
        lnd = AR.alloc((512,), F32)
        tmpb = [AR.alloc((4, 128), F32) for _ in range(2)]
        use_mask = 'nop1' not in DBG
        mt = mT[0]
        mtk = ('mT', 0)
        ol = olat[0]
        olk = ('olat', 0)
        state = {'pi': 0, 'ti': 0}

        def emit_qabs(i, hg, gi):
            tsl = slice(i * 128, (i + 1) * 128)
            qa_ap = qa[gi % 2]
            qak = ('qa', gi % 2)
            qa4 = qa_ap.rearrange("p r (a b) -> p r a b", a=4)
            for rc in range(2):
                for hl in range(4):
                    h = 4 * hg + hl
                    psl = slice((h % 2) * 64, (h % 2) * 64 + 64)
                    bq_ = 5 + (hl % 2)
                    MM(PSB[bq_][:, (hl // 2) * 128:(hl // 2) * 128 + 128], wuk[psl, h // 2, rc * 128:(rc + 1) * 128],
                       qT[psl, h // 2, tsl], True, True, ['wuk', ('q', h // 2, i // 4)], [psk(bq_)])
                CP(qa4[:, rc, 0:4:2, :], PSB[5][:, 0:256].rearrange("p (a b) -> p a b", a=2), [psk(5)], [qak], eng='dve')
                CP(qa4[:, rc, 1:4:2, :], PSB[6][:, 0:256].rearrange("p (a b) -> p a b", a=2), [psk(6)], [qak], eng='act')

        def emit_qk(i, hg, gi, jj):
            qa_ap = qa[gi % 2]
            qak = ('qa', gi % 2)
            bl = state['pi'] % 2
            for rc in range(2):
                MM(ps(bl), ckvT[:, rc, jj * 128:(jj + 1) * 128], qa_ap[:, rc, :], rc == 0, rc == 1,
                   [('ckvT', rc, jj // 4), qak], [psk(bl)])
            p_ap = pb[state['pi'] % 4]
            pkey = ('pb', state['pi'] % 4)
            state['pi'] += 1
            return bl, p_ap, pkey

        def emit_soft(i, hg, jj, bl, p_ap, pkey):
            if jj >= i - 1:
                T = Tcur if jj == i else Tprev
                tb = tmpb[state['ti'] % 2]
                tkey = ('tmpb', state['ti'] % 2)
                state['ti'] += 1
                STT(tb, PSB[bl][:, :].rearrange("p (a b) -> p a b", a=4), 0.125, T[:, 4 * hg:4 * hg + 4, :],
                    ALU.mult, ALU.add, [psk(bl), 'Tcur', 'Tprev'], [tkey])
                ACT(p_ap, tb, AF.Exp, [tkey], [pkey])
            else:
                ACT(p_ap, PSB[bl][:, :].rearrange("p (a b) -> p a b", a=4), AF.Exp, [psk(bl)], [pkey], scale=0.125)
            if i >= 2 and use_mask:
                TT(p_ap, p_ap, mt[:, jj, :].unsqueeze(1).to_broadcast([128, 4, 128]), ALU.mult,
                   [pkey, mtk], [pkey])

        def emit_pv(i, jj, p_ap, pkey):
            p2 = p_ap.rearrange("p a b -> p (a b)")
            MM(ps(2), ones_bf, p2, jj == 0, jj == i, [pkey, 'ones_bf'], [psk(2)])
            for rc in range(2):
                MM(ps(3 + rc), ckv[:, jj, rc * 128:(rc + 1) * 128], p2, jj == 0, jj == i,
                   [pkey, ('ckv', jj)], [psk(3 + rc)])

        def emit_tail(i, hg):
            tsl = slice(i * 128, (i + 1) * 128)
            ACT(lnd, ps(2), AF.Ln, [psk(2)], ['lnd'])
            ACT(rec, lnd, AF.Exp, ['lnd'], ['rec'], scale=-1.0)
            for rc in range(2):
                CP(ol[:, rc, :], ps(3 + rc), [psk(3 + rc)], [olk], eng='act' if rc else 'dve')
            for pr in range(2):
                k = 0
                for par in range(2):
                    hl = 2 * pr + par
                    h = 4 * hg + hl
                    for rc in range(2):
                        MM(PSB[7][:, pr * 128:(pr + 1) * 128], wuv[:, rc, h, :], ol[:, rc, hl * 128:(hl + 1) * 128],
                           k == 0, k == 3, ['wuv', olk], [psk(7)])
                        k += 1
            for pr in range(2):
                c = 2 * hg + pr
                for par in range(2):
                    hl = 2 * pr + par
                    psl = slice(par * 64, par * 64 + 64)
                    TT(hT[psl, c, tsl], PSB[7][psl, pr * 128:(pr + 1) * 128], rec[psl, hl * 128:(hl + 1) * 128], ALU.mult,
                       [psk(7), 'rec'], [('h', c, i // 4)])

        groups = [(i, hg) for i in range(NTILE if 'nop2' not in DBG else 0) for hg in range(4)]
        pending_tail = None
        if groups:
            emit_qabs(groups[0][0], groups[0][1], 0)
        for gi, (i, hg) in enumerate(groups):
            if hg == 0 and i >= 2 and use_mask:
                DMA('sp', mt[:, 0:i + 1, :].rearrange("p a b -> p (a b)"), maskD[i][:, 0:128 * (i + 1)], [('maskD', i)], [mtk])
            cur = emit_qk(i, hg, gi, 0)
            if pending_tail is not None:
                emit_tail(*pending_tail)
                pending_tail = None
            for jj in range(i + 1):
                nxt = emit_qk(i, hg, gi, jj + 1) if jj + 1 <= i else None
                emit_soft(i, hg, jj, *cur)
                emit_pv(i, jj, cur[1], cur[2])
                cur = nxt
            if gi + 1 < len(groups):
                emit_qabs(groups[gi + 1][0], groups[gi + 1][1], gi + 1)
            pending_tail = (i, hg)
        if pending_tail is not None:
            emit_tail(*pending_tail)
        AR.release(m)
        out_proj(a_w_out[j], l, s, False)

    finals = []
    for s in range(nseq):
        S.barrier()
        for c in range(8):
            DMA('sp', xT[:, c, :], xin[s, c * 128:(c + 1) * 128, :], [], [('x', c, tg) for tg in range(4)])
        for l in layers:
            S.barrier()
            norm_to_h(l, 0, s)
            if 'noA' in DBG:
                pass
            elif l % 2 == 0:
                mixer_a(l, s)
            else:
                mixer_b(l, s)
            S.barrier()
            norm_to_h(l, 1, s)
            ffn(l, s)
        S.barrier()
        if final_norm:
            m = AR.mark()
            ost = [AR.alloc((512,), F32) for _ in range(2)]
            gfin = sm('gfin')
            cnt = [0]

            def out_fn(c, tg, t_ap, t_key):
                k = cnt[0] % 2
                cnt[0] += 1
                TS(ost[k], t_ap, gfin[:, c:c + 1], None, ALU.mult, None, [t_key, 'smalls'], [('ost', k)], eng='dve')
                finals.append(DMA('sp', xout[s, c * 128:(c + 1) * 128, tg * 512:(tg + 1) * 512], ost[k], [('ost', k)], []))
            norm_mod(None, None, out_fn)
            AR.release(m)
        else:
            for c in range(8):
                finals.append(DMA('sp', xout[s, c * 128:(c + 1) * 128, :], xT[:, c, :], [('x', c, tg) for tg in range(4)], []))
    S.finalize(final_waits=finals)
    es.close()
    return nc


def _t5_bucket_np(dist):
    d = np.maximum(dist, 0)
    large = 16 + (np.log(np.maximum(d, 1).astype(np.float32) / 16) / math.log(128 / 16) * 16).astype(np.int32)
    large = np.minimum(large, 31)
    return np.where(d < 16, d, large)


def _consts():
    c = np.zeros((128, 5, 128), np.float32)
    idx = np.arange(128)
    c[:, 0, :] = np.eye(128, dtype=np.float32)
    blk = (idx[:, None] // 64) == (idx[None, :] // 64)
    c[:, 1, :] = blk.astype(np.float32) / 64.0
    c[:, 2, :] = np.where(idx[:, None] <= idx[None, :], 0.0, -30000.0)
    c[:, 3, :] = np.where(idx[:, None] > idx[None, :], 0.0, -30000.0)
    c[:, 4, :] = np.where(idx[None, :] <= idx[:, None], 0.0, NEG_BIG)
    return c.reshape(128, 5 * 128)


def _tbias(rel_bias):
    s = np.arange(128)[:, None]
    t = np.arange(128)[None, :]
    out = np.zeros((2, 128, NH, 128), np.float32)
    for k, off in enumerate((0, 128)):
        b = _t5_bucket_np(t - s + off)
        g = rel_bias[b]
        out[k] = np.transpose(g, (0, 2, 1))
    return out.reshape(2, 128, NH * 128)


def _fm(v):
    v = np.asarray(v, np.float32)
    lead = v.shape[:-1]
    r = v.reshape(lead + (v.shape[-1] // 128, 128))
    return np.moveaxis(r, -1, 0)


def _smalls(inp, core, nseq):
    SM, NS = _smalls_layout(nseq)
    sm = np.zeros((128, NS), np.float32)

    def put(name, arr):
        o, n = SM[name]
        sm[:, o:o + n] = np.asarray(arr, np.float32).reshape(128, n)
    c = inp['c'][core * nseq:(core + 1) * nseq]
    put('c', np.transpose(_fm(c), (0, 2, 1)))
    put('bada', _fm(inp['b_ada']))
    put('gmix', _fm(inp['norm_mix_g']))
    put('gffn', _fm(inp['norm_ffn_g']))
    put('gfin', _fm(inp['norm_final_g']))
    put('kvg', _fm(inp['a_kv_norm_g']))
    put('ikg', np.concatenate([inp['a_idx_k_g'].T, inp['a_idx_k_g'].T], 0))
    put('ikb', np.concatenate([inp['a_idx_k_b'].T, inp['a_idx_k_b'].T], 0))
    bin_ = inp['b_b_in']
    put('bq', _fm(bin_[:, 0:1024]))
    bk = bin_[:, 1024:1152].reshape(2, 2, 64)
    put('bk2', np.concatenate([np.transpose(bk, (2, 0, 1))] * 2, 0))
    put('bout', _fm(inp['b_b_out']))
    put('sinks', np.broadcast_to(inp['b_sinks'].reshape(1, 32), (128, 32)))
    put('rb31', np.broadcast_to(inp['rel_bias'][31].reshape(1, 16), (128, 16)))
    bv = bin_[:, 1152:1280].reshape(2, 2, 1, 64)
    bv2 = np.broadcast_to(bv, (2, 2, 2, 64)).reshape(1, 512)
    put('bv2', np.broadcast_to(bv2, (128, 512)))
    return sm


_NC_CACHE = {}


def _get_nc(layers, nseq, final_norm):
    key = (tuple(layers), nseq, final_norm)
    if key not in _NC_CACHE:
        _NC_CACHE[key] = build(list(layers), nseq, final_norm)
    return _NC_CACHE[key]


FUSED = True


def kernel(**inp):
    inp = {k: np.asarray(v) for k, v in inp.items()}
    nseq = 2
    x = inp['x']
    xT = np.ascontiguousarray(np.transpose(x, (0, 2, 1)))
    consts = _consts()
    tb = _tbias(inp['rel_bias'])
    ukT = np.ascontiguousarray(
        np.transpose(inp['a_w_uk'].reshape(2, 8, 2, 256, 64), (0, 2, 4, 1, 3))).reshape(2, 128, 8 * 256)
    shared = {
        'consts': consts, 'tbias': tb, 'w_ada': inp['w_ada'], 'a_w_in': inp['a_w_in'], 'a_w_ukT': ukT,
        'a_w_uv': inp['a_w_uv'], 'a_w_out': inp['a_w_out'], 'b_w_in': inp['b_w_in'], 'b_w_out': inp['b_w_out'],
        'ffn_w1': inp['ffn_w1'], 'ffn_w3': inp['ffn_w3'], 'ffn_w2': inp['ffn_w2'],
    }
    smalls = [_smalls(inp, c, nseq) for c in range(NCORES)]
    cur = [xT[c * nseq:(c + 1) * nseq] for c in range(NCORES)]
    plan = [([0, 1, 2, 3], True)] if FUSED else [([0], False), ([1], False), ([2], False), ([3], True)]
    for layers, fin in plan:
        nc = _get_nc(layers, nseq, fin)
        in_maps = [dict(shared, xin=np.ascontiguousarray(cur[c]), smalls=smalls[c]) for c in range(NCORES)]
        res = run_bass_kernel_spmd(nc, in_maps, core_ids=list(range(NCORES)))
        cur = [res.results[c]['xout'] for c in range(NCORES)]
    out = np.concatenate(cur, 0)
    return np.ascontiguousarray(np.transpose(out, (0, 2, 1))).astype(np.float32)
```

```python
import contextlib
import math
import os

import numpy as np
import concourse.bass as bass
import concourse.mybir as mybir
from concourse.bass_utils import run_bass_kernel_spmd

F32 = mybir.dt.float32
BF16 = mybir.dt.bfloat16
AF = mybir.ActivationFunctionType
ALU = mybir.AluOpType

D = 1024
SEQ = 2048
DEPTH = 4
NH = 16
DFF = 2816
NFC = DFF // 128
NTILE = SEQ // 128
NCORES = 8
A_IN = 1864
B_IN = 1280
NEG_BIG = -1.0e30
REPL = -3.0e38

ENGS = ('pe', 'act', 'dve', 'pool', 'sp')
NDMASEM = 12
EPOCH = 3000


class Op:
    __slots__ = ('eng', 'fn', 'deps', 'signal', 'is_dma', 'dsem', 'dval', 'cnt', 'epoch')

    def __init__(self, eng, fn, is_dma):
        self.eng = eng
        self.fn = fn
        self.deps = ()
        self.signal = False
        self.is_dma = is_dma
        self.dsem = None
        self.dval = 0
        self.cnt = 0
        self.epoch = 0


class Sched:
    def __init__(self, nc):
        self.nc = nc
        self.ops = {e: [] for e in ENGS}
        self.res = {}
        self.dma_hist = {e: [] for e in ENGS}
        self.pending_barrier = {e: None for e in ENGS}
        self.dma_since_barrier = []

    def barrier(self):
        deps = set(self.dma_since_barrier)
        for e in ENGS:
            if self.ops[e]:
                last = self.ops[e][-1]
                deps.add(last)
        self.dma_since_barrier = []
        for e in ENGS:
            old = self.pending_barrier[e]
            self.pending_barrier[e] = set(deps) | (old or set())

    def add(self, eng, fn, reads=(), writes=(), dma=False):
        op = Op(eng, fn, dma)
        deps = set()
        res = self.res
        for k in reads:
            st = res.get(k)
            if st is not None and st[0] is not None:
                deps.add(st[0])
        for k in writes:
            st = res.get(k)
            if st is not None:
                if st[0] is not None:
                    deps.add(st[0])
                deps.update(st[1].values())
                deps.update(st[2])
        for k in reads:
            st = res.get(k)
            if st is None:
                st = res[k] = [None, {}, []]
            if dma:
                st[2].append(op)
            else:
                st[1][eng] = op
        for k in writes:
            res[k] = [op, {}, []]
        pb = self.pending_barrier[eng]
        if pb is not None:
            deps.update(pb)
            self.pending_barrier[eng] = None
        if dma:
            h = self.dma_hist[eng]
            k = len(h)
            op.dsem = k % NDMASEM
            op.dval = 16 * (k // NDMASEM + 1)
            if k >= NDMASEM:
                deps.add(h[k - NDMASEM])
            h.append(op)
            self.dma_since_barrier.append(op)
        deps.discard(op)
        op.deps = deps
        for p in deps:
            if not p.is_dma:
                if not (p.eng == 'pe' and eng == 'pe'):
                    p.signal = True
        self.ops[eng].append(op)
        return op

    def finalize(self, final_waits=()):
        nc = self.nc
        nepoch = {}
        for e in ENGS:
            c = 0
            ep = 0
            for op in self.ops[e]:
                if op.signal and not op.is_dma:
                    if c >= EPOCH:
                        ep += 1
                        c = 0
                    c += 1
                    op.cnt = c
                    op.epoch = ep
            nepoch[e] = ep + 1
        with contextlib.ExitStack() as es:
            csem = {}
            for e in ENGS:
                for ep in range(nepoch[e]):
                    csem[(e, ep)] = es.enter_context(nc.semaphore(f"c_{e}_{ep}"))
            dsem = {}
            for e in ENGS:
                if self.dma_hist[e]:
                    for i in range(NDMASEM):
                        dsem[(e, i)] = es.enter_context(nc.semaphore(f"d_{e}_{i}"))
            block = es.enter_context(nc.Block())
            getter = {'pe': block.tensor, 'act': block.scalar, 'dve': block.vector,
                      'pool': block.gpsimd, 'sp': block.sync}
            finals = list(final_waits)

            def make(e):
                def body(engobj):
                    waited = {}
                    for op in self.ops[e]:
                        for p in op.deps:
                            if p.is_dma:
                                key = ('d', p.eng, p.dsem)
                                if waited.get(key, 0) >= p.dval:
                                    continue
                                waited[key] = p.dval
                                engobj.wait_ge(dsem[(p.eng, p.dsem)], p.dval)
                            else:
                                if p.eng == 'pe' and e == 'pe':
                                    continue
                                key = ('c', p.eng)
                                val = (p.epoch, p.cnt)
                                if waited.get(key, (-1, 0)) >= val:
                                    continue
                                waited[key] = val
                                engobj.wait_ge(csem[(p.eng, p.epoch)], p.cnt)
                        ins = op.fn(engobj)
                        if op.is_dma:
                            ins.then_inc(dsem[(e, op.dsem)], 16)
                        elif op.signal:
                            ins.then_inc(csem[(e, op.epoch)], 1)
                    if e == 'sp':
                        for p in finals:
                            engobj.wait_ge(dsem[(p.eng, p.dsem)], p.dval)
                return body

            for e in ENGS:
                if self.ops[e] or e == 'sp':
                    getter[e](make(e))


class Arena:
    def __init__(self, tensor, nwords):
        self.t = tensor
        self.n = nwords
        self.top = 0
        self.peak = 0

    def alloc(self, free_shape, dtype):
        n = int(np.prod(free_shape))
        words = n if dtype == F32 else (n + 1) // 2
        words = (words + 7) // 8 * 8
        off = self.top
        self.top += words
        self.peak = max(self.peak, self.top)
        assert self.top <= self.n, f"arena overflow {self.top} > {self.n}"
        ap = self.t[:, off:off + words]
        if dtype != F32:
            ap = ap.bitcast(dtype)
        ap = ap[:, 0:n]
        if len(free_shape) == 2:
            ap = ap.rearrange("p (a b) -> p a b", a=free_shape[0])
        elif len(free_shape) == 3:
            ap = ap.rearrange("p (a b c) -> p a b c", a=free_shape[0], b=free_shape[1])
        elif len(free_shape) == 4:
            ap = ap.rearrange("p (a b c d) -> p a b c d", a=free_shape[0], b=free_shape[1], c=free_shape[2])
        return ap

    def mark(self):
        return self.top

    def release(self, m):
        self.top = m


def _smalls_layout(nseq):
    items = [('c', 8 * nseq), ('bada', 4 * 48), ('gmix', 32), ('gffn', 32), ('gfin', 8),
             ('kvg', 4), ('ikg', 2), ('ikb', 2), ('bq', 16), ('bk2', 4), ('bout', 16),
             ('sinks', 32), ('rb31', 16), ('bv2', 512)]
    off = {}
    o = 0
    for k, n in items:
        off[k] = (o, n)
        o += n
    return off, o


ARENA_WORDS = 53000


def build(layers, nseq, final_norm):
    DBG = os.environ.get('KDBG', '').split(',')
    nc = bass.Bass("TRN2", target_bir_lowering=False)
    SM, NS = _smalls_layout(nseq)
    dtn = nc.dram_tensor
    xin = dtn("xin", [nseq, D, SEQ], F32, kind="ExternalInput").ap()
    xout = dtn("xout", [nseq, D, SEQ], F32, kind="ExternalOutput").ap()
    smalls_d = dtn("smalls", [128, NS], F32, kind="ExternalInput").ap()
    consts_d = dtn("consts", [128, 5 * 128], F32, kind="ExternalInput").ap()
    tb_d = dtn("tbias", [2, 128, NH * 128], F32, kind="ExternalInput").ap()
    w_ada = dtn("w_ada", [DEPTH, D, 6 * D], F32, kind="ExternalInput").ap()
    a_w_in = dtn("a_w_in", [2, D, A_IN], F32, kind="ExternalInput").ap()
    a_w_ukT = dtn("a_w_ukT", [2, 128, 8 * 256], F32, kind="ExternalInput").ap()
    a_w_uv = dtn("a_w_uv", [2, NH, 256, 64], F32, kind="ExternalInput").ap()
    a_w_out = dtn("a_w_out", [2, D, D], F32, kind="ExternalInput").ap()
    b_w_in = dtn("b_w_in", [2, D, B_IN], F32, kind="ExternalInput").ap()
    b_w_out = dtn("b_w_out", [2, D, D], F32, kind="ExternalInput").ap()
    ffn_w1 = dtn("ffn_w1", [DEPTH, D, DFF], F32, kind="ExternalInput").ap()
    ffn_w3 = dtn("ffn_w3", [DEPTH, D, DFF], F32, kind="ExternalInput").ap()
    ffn_w2 = dtn("ffn_w2", [DEPTH, DFF, D], F32, kind="ExternalInput").ap()
    maskD = dtn("maskD", [NTILE, 128, NTILE * 128], BF16).ap()

    es = contextlib.ExitStack()
    arena_t = es.enter_context(nc.sbuf_tensor("arena", [128, ARENA_WORDS], F32))
    PSB = [es.enter_context(nc.psum_tensor(f"ps{i}", [128, 512], F32)) for i in range(8)]
    S = Sched(nc)
    AR = Arena(arena_t, ARENA_WORDS)

    def ps(i):
        return PSB[i][:, :]

    def psk(i):
        return ('ps', i)

    def MM(out, lhsT, rhs, start, stop, rd, wr):
        S.add('pe', lambda e: e.matmul(out, lhsT=lhsT, rhs=rhs, start=start, stop=stop), reads=rd, writes=wr)

    def ACT(out, in_, func, rd, wr, bias=None, scale=None):
        kw = {}
        if bias is not None:
            kw['bias'] = bias
        if scale is not None:
            kw['scale'] = scale
        S.add('act', lambda e: e.activation(out=out, in_=in_, func=func, **kw), reads=rd, writes=wr)

    def TS(out, in0, s1, s2, op0, op1, rd, wr, eng='dve'):
        if op1 is None:
            S.add(eng, lambda e: e.tensor_scalar(out=out, in0=in0, scalar1=s1, scalar2=None, op0=op0), reads=rd, writes=wr)
        else:
            S.add(eng, lambda e: e.tensor_scalar(out=out, in0=in0, scalar1=s1, scalar2=s2, op0=op0, op1=op1), reads=rd, writes=wr)

    def TT(out, in0, in1, op, rd, wr, eng='dve'):
        S.add(eng, lambda e: e.tensor_tensor(out=out, in0=in0, in1=in1, op=op), reads=rd, writes=wr)

    def STT(out, in0, scalar, in1, op0, op1, rd, wr):
        S.add('dve', lambda e: e.scalar_tensor_tensor(out=out, in0=in0, scalar=scalar, in1=in1, op0=op0, op1=op1), reads=rd, writes=wr)

    def CP(out, in_, rd, wr, eng='dve'):
        if eng == 'act':
            S.add(eng, lambda e: e.activation(out=out, in_=in_, func=AF.Identity), reads=rd, writes=wr)
        else:
            S.add(eng, lambda e: e.tensor_copy(out=out, in_=in_), reads=rd, writes=wr)

    def RECIP(out, in_, rd, wr):
        S.add('dve', lambda e: e.reciprocal(out=out, in_=in_), reads=rd, writes=wr)

    def MEMSET(ap, val, wr, eng='dve'):
        S.add(eng, lambda e: e.memset(ap, val), writes=wr)

    def DMA(q, out, in_, rd, wr):
        return S.add(q, lambda e: e.dma_start(out=out, in_=in_), reads=rd, writes=wr, dma=True)

    xT = AR.alloc((8, SEQ), F32)
    hT = AR.alloc((8, SEQ), BF16)
    smalls = AR.alloc((NS,), F32)
    cst = AR.alloc((5, 128), F32)
    ident_bf = AR.alloc((128,), BF16)
    ones_bf = AR.alloc((128,), BF16)
    ones_f = AR.alloc((128,), F32)
    Tcur = AR.alloc((NH, 128), F32)
    Tprev = AR.alloc((NH, 128), F32)
    modT = AR.alloc((DEPTH, 48, nseq), F32)
    aT = AR.alloc((DEPTH, 2, 8, nseq), F32)
    gbo = AR.alloc((DEPTH, 8, nseq), F32)
    esink = AR.alloc((2, NH), F32)
    cs_bf = AR.alloc((8, nseq), BF16)
    cols = AR.alloc((8,), F32)
    m8 = AR.alloc((8,), F32)
    PBASE = AR.mark()

    def sm(name):
        o, n = SM[name]
        return smalls[:, o:o + n]

    DMA('sp', smalls, smalls_d, [], ['smalls'])
    DMA('sp', cst, consts_d.rearrange("p (a b) -> p a b", a=5), [], ['cst'])
    DMA('sp', Tcur, tb_d[0].rearrange("p (a b) -> p a b", a=NH), [], ['Tcur'])
    DMA('sp', Tprev, tb_d[1].rearrange("p (a b) -> p a b", a=NH), [], ['Tprev'])
    MEMSET(ones_bf, 1.0, ['ones_bf'])
    MEMSET(ones_f, 1.0, ['ones_f'])
    MEMSET(cols[:, 0:1], 1e-6, ['cols'])
    CP(ident_bf, cst[:, 0, :], ['cst'], ['ident_bf'])
    ident_f = cst[:, 0, :]
    onesblk = cst[:, 1, :]
    causal_add = cst[:, 2, :]
    prev_add = cst[:, 3, :]
    negtri = cst[:, 4, :]
    eps_col = cols[:, 0:1]
    rb31 = sm('rb31')
    rb31_b = rb31.unsqueeze(2).to_broadcast([128, NH, 128])
    TT(Tcur, Tcur, rb31_b, ALU.subtract, ['Tcur', 'smalls'], ['Tcur'])
    TT(Tprev, Tprev, rb31_b, ALU.subtract, ['Tprev', 'smalls'], ['Tprev'])
    TT(Tcur, Tcur, causal_add.unsqueeze(1).to_broadcast([128, NH, 128]), ALU.add, ['Tcur', 'cst'], ['Tcur'])
    sinks = sm('sinks').rearrange("p (a b) -> p a b", a=2)
    TT(esink, sinks, rb31.unsqueeze(1).to_broadcast([128, 2, NH]), ALU.subtract, ['smalls'], ['esink'])
    ACT(esink, esink, AF.Exp, ['esink'], ['esink'])
    cview = sm('c').rearrange("p (a b) -> p a b", a=8)
    ACT(cs_bf, cview, AF.Silu, ['smalls'], ['cs_bf'])

    class Slots:
        def __init__(self, n, tag):
            self.bufs = [AR.alloc((8 * 512,), BF16) for _ in range(n)]
            self.n = n
            self.i = 0
            self.tag = tag

        def next(self):
            k = self.i % self.n
            self.i += 1
            return self.bufs[k], (self.tag, k)

    def load_w(slots, src, kc, ncol):
        buf, key = slots.next()
        v = buf[:, 0:kc * ncol].rearrange("p (a b) -> p a b", a=kc)
        DMA('pool', v, src, [], [key])
        return v, key

    def wrows(w2d):
        return w2d.rearrange("(c p) n -> p c n", p=128)

    S.barrier()
    m0 = AR.mark()
    sl = Slots(2, 'wsl')
    bada = sm('bada').rearrange("p (a b) -> p a b", a=4)
    gmix = sm('gmix').rearrange("p (a b) -> p a b", a=4)
    gffn = sm('gffn').rearrange("p (a b) -> p a b", a=4)
    bout = sm('bout').rearrange("p (a b) -> p a b", a=2)
    for l in layers:
        wv = wrows(w_ada[l])
        pm = PSB[0][:, 0:48 * nseq]
        for blk in range(12):
            v, key = load_w(sl, wv[:, :, blk * 512:(blk + 1) * 512], 8, 512)
            for fcl in range(4):
                ch = blk * 4 + fcl
                for kc in range(8):
                    MM(pm[:, ch * nseq:(ch + 1) * nseq], v[:, kc, fcl * 128:(fcl + 1) * 128], cs_bf[:, kc, :],
                       kc == 0, kc == 7, [key, 'cs_bf'], [psk(0)])
        TT(modT[:, l], pm.rearrange("p (a b) -> p a b", a=48), bada[:, l, :].unsqueeze(2).to_broadcast([128, 48, nseq]),
           ALU.add, [psk(0), 'smalls'], ['modT'])
        for which, (g, sc0) in enumerate(((gmix, 8), (gffn, 32))):
            TS(aT[:, l, which], modT[:, l, sc0:sc0 + 8, :], 1.0, None, ALU.add, None, ['modT'], ['aT'])
            TT(aT[:, l, which], aT[:, l, which], g[:, l, :].unsqueeze(2).to_broadcast([128, 8, nseq]), ALU.mult,
               ['aT', 'smalls'], ['aT'])
        if l % 2 == 1:
            TT(gbo[:, l], modT[:, l, 16:24, :], bout[:, l // 2, :].unsqueeze(2).to_broadcast([128, 8, nseq]), ALU.mult,
               ['modT', 'smalls'], ['gbo'])
    AR.release(m0)

    def norm_mod(a_of_c, b_of_c, out_fn):
        m = AR.mark()
        sq = [AR.alloc((512,), F32) for _ in range(2)]
        tmp = [AR.alloc((512,), F32) for _ in range(2)]
        sd = AR.alloc((512,), F32)
        rstd = AR.alloc((512,), F32)
        for tg in range(4):
            tsl = slice(tg * 512, (tg + 1) * 512)
            for c in range(8):
                ACT(sq[c % 2], xT[:, c, tsl], AF.Square, [('x', c, tg)], [('sq', c % 2)])
                MM(ps(0), ones_f, sq[c % 2], c == 0, c == 7, [('sq', c % 2), 'ones_f'], [psk(0)])
            ACT(sd, ps(0), AF.Sqrt, [psk(0), 'cols'], ['sd'], bias=eps_col, scale=1.0 / D)
            RECIP(rstd, sd, ['sd'], ['rstd'])
            for c in range(8):
                TT(tmp[c % 2], xT[:, c, tsl], rstd, ALU.mult, [('x', c, tg), 'rstd'], [('ntmp', c % 2)])
                out_fn(c, tg, tmp[c % 2], ('ntmp', c % 2))
        AR.release(m)

    def norm_to_h(l, which, s):
        sh0 = 0 if which == 0 else 24

        def out_fn(c, tg, t_ap, t_key):
            ACT(hT[:, c, tg * 512:(tg + 1) * 512], t_ap, AF.Identity, [t_key, 'aT', 'modT'], [('h', c, tg)],
                bias=modT[:, l, sh0 + c, s:s + 1], scale=aT[:, l, which, c, s:s + 1])
        norm_mod(None, None, out_fn)

    def ffn(l, s):
        S.barrier()
        m = AR.mark()
        uT = AR.alloc((NFC, 1024), BF16)
        sl = Slots(4, 'wsl')
        sg = [AR.alloc((512,), F32) for _ in range(2)]
        w1v = wrows(ffn_w1[l])
        w3v = wrows(ffn_w3[l])
        w2v = ffn_w2[l].rearrange("(f p) n -> p f n", p=128)
        cnt = 0
        for th in range(2):
            for fb in range(6):
                nfc = 4 if fb < 5 else 2
                ncol = nfc * 128
                va, ka = load_w(sl, w1v[:, :, fb * 512:fb * 512 + ncol], 8, ncol)
                vb, kb = load_w(sl, w3v[:, :, fb * 512:fb * 512 + ncol], 8, ncol)
                for fcl in range(nfc):
                    fc = fb * 4 + fcl
                    for t2 in range(2):
                        tg = th * 2 + t2
                        b1 = (cnt % 2) * 2
                        b3 = b1 + 1
                        for kc in range(8):
                            MM(ps(b1), va[:, kc, fcl * 128:(fcl + 1) * 128], hT[:, kc, tg * 512:(tg + 1) * 512],
                               kc == 0, kc == 7, [ka, ('h', kc, tg)], [psk(b1)])
                        for kc in range(8):
                            MM(ps(b3), vb[:, kc, fcl * 128:(fcl + 1) * 128], hT[:, kc, tg * 512:(tg + 1) * 512],
                               kc == 0, kc == 7, [kb, ('h', kc, tg)], [psk(b3)])
                        ACT(sg[cnt % 2], ps(b1), AF.Silu, [psk(b1)], [('sg', cnt % 2)])
                        TT(uT[:, fc, t2 * 512:(t2 + 1) * 512], sg[cnt % 2], ps(b3), ALU.mult,
                           [('sg', cnt % 2), psk(b3)], [('u', fc, t2)])
                        cnt += 1
            for t2 in range(2):
                tg = th * 2 + t2
                for fcb in range(6):
                    nfc = 4 if fcb < 5 else 2
                    buf, key = sl.next()
                    v = buf[:, 0:nfc * 1024].rearrange("p (a b) -> p a b", a=nfc)
                    DMA('pool', v, w2v[:, fcb * 4:fcb * 4 + nfc, :], [], [key])
                    for fcl in range(nfc):
                        fc = fcb * 4 + fcl
                        for fo in range(8):
                            MM(ps(fo), v[:, fcl, fo * 128:(fo + 1) * 128], uT[:, fc, t2 * 512:(t2 + 1) * 512],
                               fc == 0, fc == NFC - 1, [key, ('u', fc, t2)], [psk(fo)])
                for fo in range(8):
                    xs = xT[:, fo, tg * 512:(tg + 1) * 512]
                    STT(xs, ps(fo), modT[:, l, 40 + fo, s:s + 1], xs, ALU.mult, ALU.add,
                        [psk(fo), 'modT', ('x', fo, tg)], [('x', fo, tg)])
        AR.release(m)

    def out_proj(w_out2d, l, s, has_bias):
        S.barrier()
        m = AR.mark()
        sl = Slots(2, 'wsl')
        wv = wrows(w_out2d)
        for half in range(2):
            v, key = load_w(sl, wv[:, :, half * 512:(half + 1) * 512], 8, 512)
            for fl in range(4):
                fo = half * 4 + fl
                for tg in range(4):
                    b = (fl * 4 + tg) % 4
                    for kc in range(8):
                        MM(ps(b), v[:, kc, fl * 128:(fl + 1) * 128], hT[:, kc, tg * 512:(tg + 1) * 512],
                           kc == 0, kc == 7, [key, ('h', kc, tg)], [psk(b)])
                    xs = xT[:, fo, tg * 512:(tg + 1) * 512]
                    STT(xs, ps(b), modT[:, l, 16 + fo, s:s + 1], xs, ALU.mult, ALU.add,
                        [psk(b), 'modT', ('x', fo, tg)], [('x', fo, tg)])
                    if has_bias:
                        TS(xs, xs, gbo[:, l, fo, s:s + 1], None, ALU.add, None, [('x', fo, tg), 'gbo'], [('x', fo, tg)])
        AR.release(m)

    def mixer_b(l, s):
        j = l // 2
        S.barrier()
        m = AR.mark()
        qT = AR.alloc((8, SEQ), BF16)
        kT2 = AR.alloc((2, SEQ), BF16)
        v2 = AR.alloc((NTILE, 256), BF16)
        bq = sm('bq').rearrange("p (a b) -> p a b", a=2)
        bk2 = sm('bk2').rearrange("p (a b) -> p a b", a=2)
        bv2 = sm('bv2').rearrange("p (a b) -> p a b", a=2)
        m1 = AR.mark()
        sl = Slots(2, 'wsl')
        wv = wrows(b_w_in[j])
        for half in range(2):
            v, key = load_w(sl, wv[:, :, half * 512:(half + 1) * 512], 8, 512)
            for fl in range(4):
                c = half * 4 + fl
                for tg in range(4):
                    b = (fl * 4 + tg) % 4
                    for kc in range(8):
                        MM(ps(b), v[:, kc, fl * 128:(fl + 1) * 128], hT[:, kc, tg * 512:(tg + 1) * 512],
                           kc == 0, kc == 7, [key, ('h', kc, tg)], [psk(b)])
                    ACT(qT[:, c, tg * 512:(tg + 1) * 512], ps(b), AF.Identity, [psk(b), 'smalls'], [('q', c, tg)],
                        bias=bq[:, j, c:c + 1])
        buf, key = sl.next()
        v = buf[:, :].rearrange("p (a b) -> p a b", a=8)
        for kvh in range(2):
            for dup in range(2):
                DMA('pool', v[:, :, kvh * 128 + dup * 64:kvh * 128 + dup * 64 + 64],
                    wv[:, :, 1024 + kvh * 64:1024 + kvh * 64 + 64], [], [key])
                DMA('pool', v[:, :, 256 + kvh * 128 + dup * 64:256 + kvh * 128 + dup * 64 + 64],
                    wv[:, :, 1152 + kvh * 64:1152 + kvh * 64 + 64], [], [key])
        for kvh in range(2):
            for tg in range(4):
                b = tg % 4
                for kc in range(8):
                    MM(ps(b), v[:, kc, kvh * 128:(kvh + 1) * 128], hT[:, kc, tg * 512:(tg + 1) * 512],
                       kc == 0, kc == 7, [key, ('h', kc, tg)], [psk(b)])
                ACT(kT2[:, kvh, tg * 512:(tg + 1) * 512], ps(b), AF.Identity, [psk(b), 'smalls'], [('k2', kvh, tg)],
                    bias=bk2[:, j, kvh:kvh + 1])
        for st in range(NTILE):
            b = st % 4
            for kc in range(8):
                MM(PSB[b][:, 0:256], hT[:, kc, st * 128:(st + 1) * 128], v[:, kc, 256:512],
                   kc == 0, kc == 7, [key, ('h', kc, st // 4)], [psk(b)])
            TT(v2[:, st, :], PSB[b][:, 0:256], bv2[:, j, :], ALU.add, [psk(b), 'smalls'], [('v2', st)])
        S.barrier()
        AR.release(m1)
        tmpb = [AR.alloc((4, 128), F32) for _ in range(2)]
        pb = [AR.alloc((512,), BF16) for _ in range(4)]
        lnd = AR.alloc((512,), F32)
        rec = AR.alloc((4, 128), F32)
        it = 0
        pi = 0
        for n in range(NTILE):
            tsl = slice(n * 128, (n + 1) * 128)
            for kvh in range(2):
                for par in range(2):
                    psl = slice(par * 64, (par + 1) * 64)
                    kts = ([n - 1] if n > 0 else []) + [n]
                    plist = []
                    for kt in kts:
                        bl = it % 2
                        MM(ps(bl), kT2[psl, kvh, kt * 128:(kt + 1) * 128], qT[psl, 4 * kvh:4 * kvh + 4, tsl],
                           True, True, [('k2', kvh, kt // 4), ('q', 4 * kvh, n // 4), ('q', 4 * kvh + 1, n // 4),
                                        ('q', 4 * kvh + 2, n // 4), ('q', 4 * kvh + 3, n // 4)], [psk(bl)])
                        tb = tmpb[it % 2]
                        tkey = ('tmpb', it % 2)
                        T = Tcur if kt == n else Tprev
                        hs = 8 * kvh + par
                        Tsel = T[:, hs:hs + 7:2, :]
                        STT(tb, PSB[bl][:, :].rearrange("p (a b) -> p a b", a=4), 0.125, Tsel, ALU.mult, ALU.add,
                            [psk(bl), 'Tcur', 'Tprev'], [tkey])
                        if kt != n:
                            TT(tb, tb, prev_add.unsqueeze(1).to_broadcast([128, 4, 128]), ALU.add, [tkey, 'cst'], [tkey])
                        p_ap = pb[pi % 4]
                        pkey = ('pb', pi % 4)
                        pi += 1
                        ACT(p_ap, tb.rearrange("p a b -> p (a b)"), AF.Exp, [tkey], [pkey])
                        plist.append((p_ap, pkey, kt))
                        it += 1
                    for ii, (p_ap, pkey, kt) in enumerate(plist):
                        MM(ps(2), ones_bf, p_ap, ii == 0, ii == len(plist) - 1, [pkey, 'ones_bf'], [psk(2)])
                    for ii, (p_ap, pkey, kt) in enumerate(plist):
                        MM(ps(3), v2[:, kt, kvh * 128:(kvh + 1) * 128], p_ap, ii == 0, ii == len(plist) - 1,
                           [pkey, ('v2', kt)], [psk(3)])
                    hs = 8 * kvh + par
                    es_b = esink[:, j, hs:hs + 7:2].unsqueeze(2).to_broadcast([128, 4, 128])
                    TT(rec, PSB[2][:, :].rearrange("p (a b) -> p a b", a=4), es_b, ALU.add, [psk(2), 'esink'], ['rec'])
                    ACT(lnd, rec.rearrange("p a b -> p (a b)"), AF.Ln, ['rec'], ['lnd'])
                    ACT(rec.rearrange("p a b -> p (a b)"), lnd, AF.Exp, ['lnd'], ['rec'], scale=-1.0)
                    TT(hT[psl, 4 * kvh:4 * kvh + 4, tsl], PSB[3][psl, :].rearrange("p (a b) -> p a b", a=4), rec[psl],
                       ALU.mult, [psk(3), 'rec'], [('h', 4 * kvh + cc, n // 4) for cc in range(4)])
        AR.release(m)
        out_proj(b_w_out[j], l, s, True)

    def mixer_a(l, s):
        j = l // 2
        S.barrier()
        m = AR.mark()
        wv = wrows(a_w_in[j])
        kvg = sm('kvg').rearrange("p (a b) -> p a b", a=2)
        ikg = sm('ikg')
        ikb = sm('ikb')
        qiT = AR.alloc((4, SEQ), BF16)
        kiT = AR.alloc((SEQ,), BF16)
        wi = AR.alloc((NTILE, 8), F32)
        m1 = AR.mark()
        sl = Slots(2, 'wsl')
        v, key = load_w(sl, wv[:, :, 1280:1792], 8, 512)
        for c in range(4 if 'noqi' not in DBG else 0):
            for tg in range(4):
                b = tg % 4
                for kc in range(8):
                    MM(ps(b), v[:, kc, c * 128:(c + 1) * 128], hT[:, kc, tg * 512:(tg + 1) * 512],
                       kc == 0, kc == 7, [key, ('h', kc, tg)], [psk(b)])
                CP(qiT[:, c, tg * 512:(tg + 1) * 512], ps(b), [psk(b)], [('qi', c, tg)], eng='act' if tg % 2 else 'dve')
        buf, key = sl.next()
        v = buf[:, 0:8 * 256].rearrange("p (a b) -> p a b", a=8)
        DMA('pool', v[:, :, 0:64], wv[:, :, 1792:1856], [], [key])
        DMA('pool', v[:, :, 64:128], wv[:, :, 1792:1856], [], [key])
        DMA('pool', v[:, :, 128:192], wv[:, :, 1800:1864], [], [key])
        kraw = AR.alloc((512,), F32)
        kcen = AR.alloc((512,), F32)
        ksq = AR.alloc((512,), F32)
        ksd = AR.alloc((512,), F32)
        krs = AR.alloc((512,), F32)
        for tg in range(4 if 'noki' not in DBG else 0):
            tsl = slice(tg * 512, (tg + 1) * 512)
            for kc in range(8):
                MM(ps(0), v[:, kc, 0:128], hT[:, kc, tsl], kc == 0, kc == 7, [key, ('h', kc, tg)], [psk(0)])
            CP(kraw, ps(0), [psk(0)], ['kraw'])
            MM(ps(1), onesblk, kraw, True, True, ['kraw', 'cst'], [psk(1)])
            TT(kcen, kraw, ps(1), ALU.subtract, ['kraw', psk(1)], ['kcen'])
            ACT(ksq, kcen, AF.Square, ['kcen'], ['ksq'])
            MM(ps(1), onesblk, ksq, True, True, ['ksq', 'cst'], [psk(1)])
            ACT(ksd, ps(1), AF.Sqrt, [psk(1), 'cols'], ['ksd'], bias=eps_col)
            RECIP(krs, ksd, ['ksd'], ['krs'])
            TT(kcen, kcen, krs, ALU.mult, ['kcen', 'krs'], ['kcen'])
            ACT(kiT[:, tsl], kcen, AF.Identity, ['kcen', 'smalls'], [('ki', tg)],
                bias=ikb[:, j:j + 1], scale=ikg[:, j:j + 1])
        for st in range(NTILE if 'nowi' not in DBG else 0):
            b = 2 + st % 2
            for kc in range(8):
                MM(PSB[b][:, 0:8], hT[:, kc, st * 128:(st + 1) * 128], v[:, kc, 184:192],
                   kc == 0, kc == 7, [key, ('h', kc, st // 4)], [psk(b)])
            TS(wi[:, st, :], PSB[b][:, 0:8], 8 ** -0.5 * 64 ** -0.5, None, ALU.mult, None, [psk(b)], [('wi', st)])
        S.barrier()
        AR.release(m1)
        scoreb = [AR.alloc((SEQ,), F32) for _ in range(2)]
        m8b = [m8, AR.alloc((8,), F32)]
        mask01 = AR.alloc((SEQ,), BF16)
        rl = [AR.alloc((512,), F32) for _ in range(2)]
        mst = [AR.alloc((NTILE, 128), BF16) for _ in range(2)]
        ri = 0
        tgi = 0
        tiles = list(range(2, NTILE if 'nop1' not in DBG else 0))
        for pair in [tiles[k:k + 2] for k in range(0, len(tiles), 2)]:
            info = []
            for idx, i in enumerate(pair):
                score = scoreb[idx]
                n = 128 * (i + 1)
                nkb = (n + 511) // 512
                for h in range(8):
                    psl = slice((h % 2) * 64, (h % 2) * 64 + 64)
                    for kb in range(nkb):
                        w = min(512, n - kb * 512)
                        bl = ri % 2
                        MM(PSB[bl][:, 0:w], qiT[psl, h // 2, i * 128:(i + 1) * 128], kiT[psl, kb * 512:kb * 512 + w],
                           True, True, [('qi', h // 2, i // 4), ('ki', kb)], [psk(bl)])
                        r_ap = rl[ri % 2][:, 0:w]
                        rkey = ('rl', ri % 2)
                        ACT(r_ap, PSB[bl][:, 0:w], AF.Relu, [psk(bl)], [rkey])
                        sc_ap = score[:, kb * 512:kb * 512 + w]
                        if h == 0:
                            TS(sc_ap, r_ap, wi[:, i, 0:1], None, ALU.mult, None, [rkey, ('wi', i)], [('score', idx, kb)])
                        else:
                            STT(sc_ap, r_ap, wi[:, i, h:h + 1], sc_ap, ALU.mult, ALU.add,
                                [rkey, ('wi', i), ('score', idx, kb)], [('score', idx, kb)])
                        ri += 1
                allk = [('score', idx, kb) for kb in range(nkb)]
                dsl = slice(i * 128, (i + 1) * 128)
                TT(score[:, dsl], score[:, dsl], negtri, ALU.add, allk + ['cst'], allk)
                info.append((idx, i, n, score, allk))
            for it in range(32 if 'notopk' not in DBG else 0):
                for idx, i, n, score, allk in info:
                    S.add('dve', lambda e, n=n, score=score, mm=m8b[idx]: e.max(out=mm, in_=score[:, 0:n]),
                          reads=allk, writes=[('m8', idx)])
                for idx, i, n, score, allk in info:
                    S.add('dve', lambda e, n=n, score=score, mm=m8b[idx]: e.match_replace(
                        out=score[:, 0:n], in_to_replace=mm, in_values=score[:, 0:n], imm_value=REPL),
                        reads=allk + [('m8', idx)], writes=allk)
            for idx, i, n, score, allk in info:
                TS(mask01[:, 0:n], score[:, 0:n], -2.0e38, None, ALU.is_le, None, allk, ['mask01'])
                ms = mst[i % 2]
                mkey = ('mst', i % 2)
                for g in range((i + 1 + 7) // 8):
                    j0 = 8 * g
                    ng = min(8, i + 1 - j0)
                    bank = 6 + (tgi % 2)
                    tgi += 1
                    pbb = PSB[bank][:, :].bitcast(BF16)
                    for jl in range(ng):
                        jj = j0 + jl
                        S.add('pe', lambda e, jj=jj, jl=jl, pbb=pbb: e.transpose(pbb[:, jl * 128:(jl + 1) * 128], mask01[:, jj * 128:(jj + 1) * 128], ident_bf),
                              reads=['mask01', 'ident_bf'], writes=[psk(bank)])
                    CP(ms[:, j0:j0 + ng, :].rearrange("p a b -> p (a b)"), pbb[:, 0:ng * 128], [psk(bank)], [mkey], eng='act')
                DMA('sp', maskD[i][:, 0:n], ms[:, 0:i + 1, :].rearrange("p a b -> p (a b)"), [mkey], [('maskD', i)])
        S.barrier()
        AR.release(m)

        m = AR.mark()
        qT = AR.alloc((8, SEQ), BF16)
        ckvT = AR.alloc((2, SEQ), BF16)
        ckv = AR.alloc((NTILE, 256), BF16)
        m1 = AR.mark()
        sl = Slots(2, 'wsl')
        for half in range(2 if 'noq' not in DBG else 0):
            v, key = load_w(sl, wv[:, :, half * 512:(half + 1) * 512], 8, 512)
            for fl in range(4):
                c = half * 4 + fl
                for tg in range(4):
                    b = (fl * 4 + tg) % 4
                    for kc in range(8):
                        MM(ps(b), v[:, kc, fl * 128:(fl + 1) * 128], hT[:, kc, tg * 512:(tg + 1) * 512],
                           kc == 0, kc == 7, [key, ('h', kc, tg)], [psk(b)])
                    CP(qT[:, c, tg * 512:(tg + 1) * 512], ps(b), [psk(b)], [('q', c, tg)], eng='act' if tg % 2 else 'dve')
        v, key = load_w(sl, wv[:, :, 1024:1280], 8, 256)
        craw = AR.alloc((2, 512), F32)
        csq = AR.alloc((512,), F32)
        csd = AR.alloc((512,), F32)
        crs = AR.alloc((512,), F32)
        for tg in range(4 if 'nockv' not in DBG else 0):
            tsl = slice(tg * 512, (tg + 1) * 512)
            for rc in range(2):
                for kc in range(8):
                    MM(ps(rc), v[:, kc, rc * 128:(rc + 1) * 128], hT[:, kc, tsl], kc == 0, kc == 7,
                       [key, ('h', kc, tg)], [psk(rc)])
                CP(craw[:, rc, :], ps(rc), [psk(rc)], [('craw', rc)])
                ACT(csq, craw[:, rc, :], AF.Square, [('craw', rc)], ['csq'])
                MM(ps(2), ones_f, csq, rc == 0, rc == 1, ['csq', 'ones_f'], [psk(2)])
            ACT(csd, ps(2), AF.Sqrt, [psk(2), 'cols'], ['csd'], bias=eps_col, scale=1.0 / 256)
            RECIP(crs, csd, ['csd'], ['crs'])
            for rc in range(2):
                STT(ckvT[:, rc, tsl], craw[:, rc, :], kvg[:, j, rc:rc + 1], crs, ALU.mult, ALU.mult,
                    [('craw', rc), 'crs', 'smalls'], [('ckvT', rc, tg)])
        for sg4 in range(NTILE // 4 if 'notr' not in DBG else 0):
            bank = 6 + (sg4 % 2)
            pbb = PSB[bank][:, :].bitcast(BF16)
            for sl4 in range(4):
                st = sg4 * 4 + sl4
                for rc in range(2):
                    k = sl4 * 2 + rc
                    S.add('pe', lambda e, st=st, rc=rc, k=k, pbb=pbb: e.transpose(pbb[:, k * 128:(k + 1) * 128], ckvT[:, rc, st * 128:(st + 1) * 128], ident_bf),
                          reads=[('ckvT', rc, st // 4), 'ident_bf'], writes=[psk(bank)])
            CP(ckv[:, sg4 * 4:sg4 * 4 + 4, :].rearrange("p a b -> p (a b)"), pbb[:, :], [psk(bank)],
               [('ckv', sg4 * 4 + q) for q in range(4)], eng='act' if sg4 % 2 else 'dve')
        S.barrier()
        AR.release(m1)
        wuk = AR.alloc((8, 256), BF16)
        wuv = AR.alloc((2, NH, 128), BF16)
        if 'nouk' not in DBG:
            DMA('pool', wuk, a_w_ukT[j].rearrange("p (a b) -> p a b", a=8), [], ['wuk'])
        if 'nomemset' not in DBG:
            MEMSET(wuv, 0.0, ['wuv'])
        uvv = a_w_uv[j].rearrange("h (rc p) d -> p rc h d", p=128)
        for rc in range(2 if 'nouv' not in DBG else 0):
            for par in range(2):
                S.add('pool', lambda e, rc=rc, par=par: e.dma_start(out=wuv[:, rc, par:NH:2, par * 64:par * 64 + 64],
                                                                    in_=uvv[:, rc, par:NH:2, :]),
                      reads=[], writes=['wuv'], dma=True)
        qa = [AR.alloc((2, 512), BF16) for _ in range(2)]
        mT = [AR.alloc((NTILE, 128), BF16) for _ in range(1)]
        pb = [AR.alloc((4, 128), BF16) for _ in range(4)]
        olat = [AR.alloc((2, 512), BF16) for _ in range(1)]
        rec = AR.alloc((512,), F32)
        lnd = AR.alloc((512,), F32)
        tmpb = [AR.alloc((4, 128), F32) for _ in range(2)]
        use_mask = 'nop1' not in DBG
        mt = mT[0]
        mtk = ('mT', 0)
        ol = olat[0]
        olk = ('olat', 0)
        state = {'pi': 0, 'ti': 0}

        def emit_qabs(i, hg, gi):
            tsl = slice(i * 128, (i + 1) * 128)
            qa_ap = qa[gi % 2]
            qak = ('qa', gi % 2)
            qa4 = qa_ap.rearrange("p r (a b) -> p r a b", a=4)
            for rc in range(2):
                for hl in range(4):
                    h = 4 * hg + hl
                    psl = slice((h % 2) * 64, (h % 2) * 64 + 64)
                    bq_ = 5 + (hl % 2)
                    MM(PSB[bq_][:, (hl // 2) * 128:(hl // 2) * 128 + 128], wuk[psl, h // 2, rc * 128:(rc + 1) * 128],
                       qT[psl, h // 2, tsl], True, True, ['wuk', ('q', h // 2, i // 4)], [psk(bq_)])
                CP(qa4[:, rc, 0:4:2, :], PSB[5][:, 0:256].rearrange("p (a b) -> p a b", a=2), [psk(5)], [qak], eng='dve')
                CP(qa4[:, rc, 1:4:2, :], PSB[6][:, 0:256].rearrange("p (a b) -> p a b", a=2), [psk(6)], [qak], eng='act')

        def emit_qk(i, hg, gi, jj):
            qa_ap = qa[gi % 2]
            qak = ('qa', gi % 2)
            bl = state['pi'] % 2
            for rc in range(2):
                MM(ps(bl), ckvT[:, rc, jj * 128:(jj + 1) * 128], qa_ap[:, rc, :], rc == 0, rc == 1,
                   [('ckvT', rc, jj // 4), qak], [psk(bl)])
            p_ap = pb[state['pi'] % 4]
            pkey = ('pb', state['pi'] % 4)
            state['pi'] += 1
            return bl, p_ap, pkey

        def emit_soft(i, hg, jj, bl, p_ap, pkey):
            if jj >= i - 1:
                T = Tcur if jj == i else Tprev
                tb = tmpb[state['ti'] % 2]
                tkey = ('tmpb', state['ti'] % 2)
                state['ti'] += 1
                STT(tb, PSB[bl][:, :].rearrange("p (a b) -> p a b", a=4), 0.125, T[:, 4 * hg:4 * hg + 4, :],
                    ALU.mult, ALU.add, [psk(bl), 'Tcur', 'Tprev'], [tkey])
                ACT(p_ap, tb, AF.Exp, [tkey], [pkey])
            else:
                ACT(p_ap, PSB[bl][:, :].rearrange("p (a b) -> p a b", a=4), AF.Exp, [psk(bl)], [pkey], scale=0.125)
            if i >= 2 and use_mask:
                TT(p_ap, p_ap, mt[:, jj, :].unsqueeze(1).to_broadcast([128, 4, 128]), ALU.mult,
                   [pkey, mtk], [pkey])

        def emit_pv(i, jj, p_ap, pkey):
            p2 = p_ap.rearrange("p a b -> p (a b)")
            MM(ps(2), ones_bf, p2, jj == 0, jj == i, [pkey, 'ones_bf'], [psk(2)])
            for rc in range(2):
                MM(ps(3 + rc), ckv[:, jj, rc * 128:(rc + 1) * 128], p2, jj == 0, jj == i,
                   [pkey, ('ckv', jj)], [psk(3 + rc)])

        def emit_tail(i, hg):
            tsl = slice(i * 128, (i + 1) * 128)
            ACT(lnd, ps(2), AF.Ln, [psk(2)], ['lnd'])
            ACT(rec, lnd, AF.Exp, ['lnd'], ['rec'], scale=-1.0)
            for rc in range(2):
                CP(ol[:, rc, :], ps(3 + rc), [psk(3 + rc)], [olk], eng='act' if rc else 'dve')
            for pr in range(2):
                k = 0
                for par in range(2):
                    hl = 2 * pr + par
                    h = 4 * hg + hl
                    for rc in range(2):
                        MM(PSB[7][:, pr * 128:(pr + 1) * 128], wuv[:, rc, h, :], ol[:, rc, hl * 128:(hl + 1) * 128],
                           k == 0, k == 3, ['wuv', olk], [psk(7)])
                        k += 1
            for pr in range(2):
                c = 2 * hg + pr
                for par in range(2):
                    hl = 2 * pr + par
                    psl = slice(par * 64, par * 64 + 64)
                    TT(hT[psl, c, tsl], PSB[7][psl, pr * 128:(pr + 1) * 128], rec[psl, hl * 128:(hl + 1) * 128], ALU.mult,
                       [psk(7), 'rec'], [('h', c, i // 4)])

        groups = [(i, hg) for i in range(NTILE if 'nop2' not in DBG else 0) for hg in range(4)]
        pending_tail = None
        if groups:
            emit_qabs(groups[0][0], groups[0][1], 0)
        for gi, (i, hg) in enumerate(groups):
            if hg == 0 and i >= 2 and use_mask:
                DMA('sp', mt[:, 0:i + 1, :].rearrange("p a b -> p (a b)"), maskD[i][:, 0:128 * (i + 1)], [('maskD', i)], [mtk])
            cur = emit_qk(i, hg, gi, 0)
            if pending_tail is not None:
                emit_tail(*pending_tail)
                pending_tail = None
            for jj in range(i + 1):
                nxt = emit_qk(i, hg, gi, jj + 1) if jj + 1 <= i else None
                emit_soft(i, hg, jj, *cur)
                emit_pv(i, jj, cur[1], cur[2])
                cur = nxt
            if gi + 1 < len(groups):
                emit_qabs(groups[gi + 1][0], groups[gi + 1][1], gi + 1)
            pending_tail = (i, hg)
        if pending_tail is not None:
            emit_tail(*pending_tail)
        AR.release(m)
        out_proj(a_w_out[j], l, s, False)

    finals = []
    for s in range(nseq):
        S.barrier()
        for c in range(8):
            DMA('sp', xT[:, c, :], xin[s, c * 128:(c + 1) * 128, :], [], [('x', c, tg) for tg in range(4)])
        for l in layers:
            S.barrier()
            norm_to_h(l, 0, s)
            if 'noA' in DBG:
                pass
            elif l % 2 == 0:
                mixer_a(l, s)
            else:
                mixer_b(l, s)
            S.barrier()
            norm_to_h(l, 1, s)
            ffn(l, s)
        S.barrier()
        if final_norm:
            m = AR.mark()
            ost = [AR.alloc((512,), F32) for _ in range(2)]
            gfin = sm('gfin')
            cnt = [0]

            def out_fn(c, tg, t_ap, t_key):
                k = cnt[0] % 2
                cnt[0] += 1
                TS(ost[k], t_ap, gfin[:, c:c + 1], None, ALU.mult, None, [t_key, 'smalls'], [('ost', k)], eng='dve')
                finals.append(DMA('sp', xout[s, c * 128:(c + 1) * 128, tg * 512:(tg + 1) * 512], ost[k], [('ost', k)], []))
            norm_mod(None, None, out_fn)
            AR.release(m)
        else:
            for c in range(8):
                finals.append(DMA('sp', xout[s, c * 128:(c + 1) * 128, :], xT[:, c, :], [('x', c, tg) for tg in range(4)], []))
    S.finalize(final_waits=finals)
    es.close()
    return nc


def _t5_bucket_np(dist):
    d = np.maximum(dist, 0)
    large = 16 + (np.log(np.maximum(d, 1).astype(np.float32) / 16) / math.log(128 / 16) * 16).astype(np.int32)
    large = np.minimum(large, 31)
    return np.where(d < 16, d, large)


def _consts():
    c = np.zeros((128, 5, 128), np.float32)
    idx = np.arange(128)
    c[:, 0, :] = np.eye(128, dtype=np.float32)
    blk = (idx[:, None] // 64) == (idx[None, :] // 64)
    c[:, 1, :] = blk.astype(np.float32) / 64.0
    c[:, 2, :] = np.where(idx[:, None] <= idx[None, :], 0.0, -30000.0)
    c[:, 3, :] = np.where(idx[:, None] > idx[None, :], 0.0, -30000.0)
    c[:, 4, :] = np.where(idx[None, :] <= idx[:, None], 0.0, NEG_BIG)
    return c.reshape(128, 5 * 128)


def _tbias(rel_bias):
    s = np.arange(128)[:, None]
    t = np.arange(128)[None, :]
    out = np.zeros((2, 128, NH, 128), np.float32)
    for k, off in enumerate((0, 128)):
        b = _t5_bucket_np(t - s + off)
        g = rel_bias[b]
        out[k] = np.transpose(g, (0, 2, 1))
    return out.reshape(2, 128, NH * 128)


def _fm(v):
    v = np.asarray(v, np.float32)
    lead = v.shape[:-1]
    r = v.reshape(lead + (v.shape[-1] // 128, 128))
    return np.moveaxis(r, -1, 0)


def _smalls(inp, core, nseq):
    SM, NS = _smalls_layout(nseq)
    sm = np.zeros((128, NS), np.float32)

    def put(name, arr):
        o, n = SM[name]
        sm[:, o:o + n] = np.asarray(arr, np.float32).reshape(128, n)
    c = inp['c'][core * nseq:(core + 1) * nseq]
    put('c', np.transpose(_fm(c), (0, 2, 1)))
    put('bada', _fm(inp['b_ada']))
    put('gmix', _fm(inp['norm_mix_g']))
    put('gffn', _fm(inp['norm_ffn_g']))
    put('gfin', _fm(inp['norm_final_g']))
    put('kvg', _fm(inp['a_kv_norm_g']))
    put('ikg', np.concatenate([inp['a_idx_k_g'].T, inp['a_idx_k_g'].T], 0))
    put('ikb', np.concatenate([inp['a_idx_k_b'].T, inp['a_idx_k_b'].T], 0))
    bin_ = inp['b_b_in']
    put('bq', _fm(bin_[:, 0:1024]))
    bk = bin_[:, 1024:1152].reshape(2, 2, 64)
    put('bk2', np.concatenate([np.transpose(bk, (2, 0, 1))] * 2, 0))
    put('bout', _fm(inp['b_b_out']))
    put('sinks', np.broadcast_to(inp['b_sinks'].reshape(1, 32), (128, 32)))
    put('rb31', np.broadcast_to(inp['rel_bias'][31].reshape(1, 16), (128, 16)))
    bv = bin_[:, 1152:1280].reshape(2, 2, 1, 64)
    bv2 = np.broadcast_to(bv, (2, 2, 2, 64)).reshape(1, 512)
    put('bv2', np.broadcast_to(bv2, (128, 512)))
    return sm


_NC_CACHE = {}


def _get_nc(layers, nseq, final_norm):
    key = (tuple(layers), nseq, final_norm)
    if key not in _NC_CACHE:
        _NC_CACHE[key] = build(list(layers), nseq, final_norm)
    return _NC_CACHE[key]


FUSED = True


def kernel(**inp):
    inp = {k: np.asarray(v) for k, v in inp.items()}
    nseq = 2
    x = inp['x']
    xT = np.ascontiguousarray(np.transpose(x, (0, 2, 1)))
    consts = _consts()
    tb = _tbias(inp['rel_bias'])
    ukT = np.ascontiguousarray(
        np.transpose(inp['a_w_uk'].reshape(2, 8, 2, 256, 64), (0, 2, 4, 1, 3))).reshape(2, 128, 8 * 256)
    shared = {
        'consts': consts, 'tbias': tb, 'w_ada': inp['w_ada'], 'a_w_in': inp['a_w_in'], 'a_w_ukT': ukT,
        'a_w_uv': inp['a_w_uv'], 'a_w_out': inp['a_w_out'], 'b_w_in': inp['b_w_in'], 'b_w_out': inp['b_w_out'],
        'ffn_w1': inp['ffn_w1'], 'ffn_w3': inp['ffn_w3'], 'ffn_w2': inp['ffn_w2'],
    }
    smalls = [_smalls(inp, c, nseq) for c in range(NCORES)]
    cur = [xT[c * nseq:(c + 1) * nseq] for c in range(NCORES)]
    plan = [([0, 1, 2, 3], True)] if FUSED else [([0], False), ([1], False), ([2], False), ([3], True)]
    for layers, fin in plan:
        nc = _get_nc(layers, nseq, fin)
        in_maps = [dict(shared, xin=np.ascontiguousarray(cur[c]), smalls=smalls[c]) for c in range(NCORES)]
        res = run_bass_kernel_spmd(nc, in_maps, core_ids=list(range(NCORES)))
        cur = [res.results[c]['xout'] for c in range(NCORES)]
    out = np.concatenate(cur, 0)
    return np.ascontiguousarray(np.transpose(out, (0, 2, 1))).astype(np.float32)
```
